# Optimizing a Trainium2 kernel written in Bass

```python
import jax, jax.numpy as jnp
from jax import lax
import numpy as np

D_MODEL = 4096
BATCH = 2
SEQ = 4096
DEPTH = 2

CTX_LEN = 256
GRID_W = 64
N_MOD = 6
NORM_EPS = 1e-6

LRU_WIDTH = D_MODEL // 2
LRU_BLOCKS = 16
LRU_BLOCK = LRU_WIDTH // LRU_BLOCKS
CONV_WIDTH = 4
CONV_PAD_LEFT = 2
LRU_C = 8.0
LRU_MIN_RAD = 0.9
LRU_MAX_RAD = 0.999

RWKV_WIDTH = D_MODEL // 2
RWKV_HEAD = 64
RWKV_HEADS = RWKV_WIDTH // RWKV_HEAD
DECAY_LORA = 128
ICL_LORA = 128
GATE_LORA = 480
RWKV_GN_EPS = 64e-5
EVEN_IN = 2 * LRU_WIDTH + 3 * RWKV_WIDTH + DECAY_LORA + ICL_LORA + GATE_LORA

GLA_HEADS = 8
GLA_KEY = D_MODEL // 2
GLA_VAL = D_MODEL
GLA_DK = GLA_KEY // GLA_HEADS
GLA_DV = GLA_VAL // GLA_HEADS
GLA_GATE_LORA = 16
GLA_GATE_NORM = 16.0
GLA_CLAMP_MIN = -1.0
GLA_CHUNK = 64
GLA_EPS = 1e-5
ODD_IN = 2 * GLA_KEY + 2 * GLA_VAL + 2 * GLA_GATE_LORA

N_EXPERTS = 32
TOP_K = 4
D_EXPERT = D_MODEL // 8
SWIGLU_LIMIT = 7.0
SWIGLU_ALPHA = 1.702
MOE_BLOCK = 128

kernel_name = 'hybrid_lru_rwkv7_gla_moe_dit'


def rms_norm(z, gain):
    z32 = z.astype(jnp.float32)
    z32 = z32 * lax.rsqrt(jnp.mean(z32 * z32, axis=-1, keepdims=True) + NORM_EPS)
    return (z32 * gain.astype(jnp.float32)).astype(z.dtype)


def modulate(z, shift, scale):
    return z * (1.0 + scale) + shift


def per_segment(fn, z, n_ctx):
    return jnp.concatenate([fn(z[:, :n_ctx]), fn(z[:, n_ctx:])], axis=1)


def seg_reverse(z, n_ctx):
    return per_segment(lambda s: jnp.flip(s, axis=1), z, n_ctx)


def dir_pair(z, n_ctx):
    return jnp.stack([z, seg_reverse(z, n_ctx)], axis=0)


def dir_align(z2, n_ctx):
    return jnp.stack([z2[0], seg_reverse(z2[1], n_ctx)], axis=0)


def to_column_major(z, rows):
    b, _, d = z.shape
    return z.reshape(b, rows, GRID_W, d).transpose(0, 2, 1, 3).reshape(b, rows * GRID_W, d)


def from_column_major(z, rows):
    b, _, d = z.shape
    return z.reshape(b, GRID_W, rows, d).transpose(0, 2, 1, 3).reshape(b, rows * GRID_W, d)


def depthwise_conv(z, w, pad_left):
    width, ch = w.shape
    return lax.conv_general_dilated(z, w[:, None, :], window_strides=(1,),
                                    padding=[(pad_left, width - 1 - pad_left)],
                                    dimension_numbers=('NWC', 'WIO', 'NWC'),
                                    feature_group_count=ch)


def centred_token_shift(z, mu):
    zp = jnp.pad(z, ((0, 0), (1, 1), (0, 0)))
    return z + (0.5 * (zp[:, :-2] + zp[:, 2:]) - z) * mu


def linear_scan(a, b, axis):
    def combine(lhs, rhs):
        return lhs[0] * rhs[0], rhs[0] * lhs[1] + rhs[1]
    return lax.associative_scan(combine, (a, b), axis=axis)[1]


def rwkv7_scan(r, w, k, v, a, b):
    def step(state, inp):
        r_t, w_t, k_t, v_t, a_t, b_t = inp
        sa = jnp.einsum('dbhij,dbhj->dbhi', state, a_t)
        state = (state * w_t[..., None, :] + sa[..., :, None] * b_t[..., None, :]
                 + v_t[..., :, None] * k_t[..., None, :])
        return state, jnp.einsum('dbhij,dbhj->dbhi', state, r_t)
    s0 = jnp.zeros(r.shape[1:] + (r.shape[-1],), jnp.float32)
    _, y = lax.scan(step, s0, (r, w, k, v, a, b))
    return y


def gla_chunked(q, k, v, log_alpha):
    nd, bsz, t, nh, dk = q.shape
    dv = v.shape[-1]
    nc = t // GLA_CHUNK
    chunk = lambda z: z.reshape(nd, bsz, nc, GLA_CHUNK, nh, z.shape[-1])
    q, k, v, log_alpha = chunk(q), chunk(k), chunk(v), chunk(log_alpha)
    cum = jnp.cumsum(log_alpha, axis=3)
    cum_last = cum[:, :, :, -1]
    q_dec = q * jnp.exp(cum)
    k_inv = k * jnp.exp(-cum)
    k_end = k * jnp.exp(cum_last[:, :, :, None] - cum)
    lower = jnp.tril(jnp.ones((GLA_CHUNK, GLA_CHUNK), dtype=bool))
    scores = jnp.where(lower, jnp.einsum('dbnihk,dbnjhk->dbnhij', q_dec, k_inv), 0.0)
    o_intra = jnp.einsum('dbnhij,dbnjhv->dbnihv', scores, v)

    def step(state, inp):
        q_n, k_n, v_n, dec_n = inp
        o_n = jnp.einsum('dbihk,dbhkv->dbihv', q_n, state)
        state = state * dec_n[..., None] + jnp.einsum('dbjhk,dbjhv->dbhkv', k_n, v_n)
        return state, o_n
    s0 = jnp.zeros((nd, bsz, nh, dk, dv), jnp.float32)
    xs = tuple(jnp.moveaxis(z, 2, 0) for z in (q_dec, k_end, v, jnp.exp(cum_last)))
    _, o_inter = lax.scan(step, s0, xs)
    o = o_intra + jnp.moveaxis(o_inter, 0, 2)
    return o.reshape(nd, bsz, t, nh, dv)


def rwkv_heads(z):
    return z.reshape(z.shape[:-1] + (RWKV_HEADS, RWKV_HEAD))


def even_mixer(h, n_ctx, w_in, w_out, conv_w, conv_b, ga_w, ga_b, gx_w, gx_b, lam,
               mu, w0, w2, a0, a2, g2, k_k, k_a, r_k, ln_w, ln_b):
    bsz, t, _ = h.shape
    f32 = jnp.float32
    proj = h @ w_in
    gate_br, xa, rwkv_in = jnp.split(proj, [LRU_WIDTH, 2 * LRU_WIDTH], axis=-1)

    xa = per_segment(lambda z: depthwise_conv(z, conv_w, CONV_PAD_LEFT), xa, n_ctx) + conv_b
    xa2 = dir_pair(xa, n_ctx)
    blocks = xa2.reshape(2, bsz, t, LRU_BLOCKS, LRU_BLOCK)
    r_gate = jax.nn.sigmoid(jnp.einsum('dbtnk,dnkj->dbtnj', blocks, ga_w).reshape(xa2.shape) + ga_b[:, None, None])
    i_gate = jax.nn.sigmoid(jnp.einsum('dbtnk,dnkj->dbtnj', blocks, gx_w).reshape(xa2.shape) + gx_b[:, None, None])
    log_a = -LRU_C * r_gate.astype(f32) * jax.nn.softplus(-lam.astype(f32))[:, None, None]
    lru_in = jnp.sqrt(-jnp.expm1(2.0 * log_a)) * (i_gate * xa2).astype(f32)
    hs = dir_align(linear_scan(jnp.exp(log_a), lru_in, axis=2), n_ctx)
    y_a = (hs[0] + hs[1]).astype(h.dtype) * jax.nn.gelu(gate_br)

    rwkv_in = per_segment(lambda z: centred_token_shift(z, mu), rwkv_in, n_ctx)
    r, k, v, w_lo, a_lo, g_lo = jnp.split(
        rwkv_in, [RWKV_WIDTH, 2 * RWKV_WIDTH, 3 * RWKV_WIDTH,
                  3 * RWKV_WIDTH + DECAY_LORA, 3 * RWKV_WIDTH + DECAY_LORA + ICL_LORA], axis=-1)
    g = jax.nn.sigmoid(g_lo) @ g2
    kk = rwkv_heads((k * k_k).astype(f32))
    kk = kk / jnp.maximum(jnp.linalg.norm(kk, axis=-1, keepdims=True), 1e-12)
    w_log = -jax.nn.softplus(-(w0[:, None, None] + jnp.einsum('btr,drc->dbtc', jnp.tanh(w_lo), w2)).astype(f32)) - 0.5
    a_icl = jax.nn.sigmoid((a0[:, None, None] + jnp.einsum('btr,drc->dbtc', a_lo, a2)).astype(f32))
    k_dir = rwkv_heads(k.astype(f32) * (1.0 + (a_icl - 1.0) * k_a.astype(f32)))
    r_h = rwkv_heads(r.astype(f32))
    v_h = rwkv_heads(v.astype(f32))
    scan_in = (dir_pair(r_h, n_ctx),
               dir_align(rwkv_heads(jnp.exp(-jnp.exp(w_log))), n_ctx),
               dir_align(k_dir, n_ctx),
               dir_pair(v_h, n_ctx),
               dir_pair(-kk, n_ctx),
               dir_align(kk[None] * rwkv_heads(a_icl), n_ctx))
    y = rwkv7_scan(*[jnp.moveaxis(z, 2, 0) for z in scan_in])
    y = dir_align(jnp.moveaxis(y, 0, 2), n_ctx)
    y = y[0] + y[1]
    mean = jnp.mean(y, axis=-1, keepdims=True)
    var = jnp.mean(jnp.square(y - mean), axis=-1, keepdims=True)
    yn = ((y - mean) * lax.rsqrt(var + RWKV_GN_EPS)).reshape(bsz, t, RWKV_WIDTH) * ln_w + ln_b
    bonus = jnp.sum(jnp.sum(r_h * k_dir * r_k, axis=-1, keepdims=True), axis=0) * v_h
    y_b = (yn + bonus.reshape(bsz, t, RWKV_WIDTH)).astype(h.dtype) * g

    return jnp.concatenate([y_a, y_b], axis=-1) @ w_out


def odd_mixer(h, n_ctx, w_in, w_out, gk_w2, gk_b, norm_w):
    bsz, t, _ = h.shape
    f32 = jnp.float32
    proj = h @ w_in
    q, k, v, gate, gk_lo = jnp.split(
        proj, [GLA_KEY, 2 * GLA_KEY, 2 * GLA_KEY + GLA_VAL, 2 * GLA_KEY + 2 * GLA_VAL], axis=-1)
    gk = jnp.einsum('btdr,drc->dbtc', gk_lo.reshape(bsz, t, 2, GLA_GATE_LORA), gk_w2) + gk_b[:, None, None]
    log_alpha = jnp.maximum(jax.nn.log_sigmoid(gk.astype(f32)) / GLA_GATE_NORM, GLA_CLAMP_MIN)
    gla_heads = lambda z: z.reshape(z.shape[:-1] + (GLA_HEADS, -1)).astype(f32)
    o = gla_chunked(dir_pair(gla_heads(q) * GLA_DK ** -0.5, n_ctx),
                    dir_pair(gla_heads(k), n_ctx),
                    dir_pair(gla_heads(v), n_ctx),
                    dir_align(gla_heads(log_alpha), n_ctx))
    o = dir_align(o, n_ctx)
    o = o[0] + o[1]
    o = o * lax.rsqrt(jnp.mean(o * o, axis=-1, keepdims=True) + GLA_EPS) * norm_w
    o = o.reshape(bsz, t, GLA_VAL).astype(h.dtype) * jax.nn.silu(gate)
    return o @ w_out


def moe_ffn(h, router_w, router_b, w_gu, b_gu, w_down, b_down):
    n, d = h.shape
    logits = (h @ router_w + router_b).astype(jnp.float32)
    top_val, top_idx = lax.top_k(logits, TOP_K)
    gates = jax.nn.softmax(top_val, axis=-1)
    flat_e = top_idx.reshape(-1)
    flat_tok = jnp.repeat(jnp.arange(n, dtype=jnp.int32), TOP_K)
    order = jnp.argsort(flat_e)
    e_sorted = flat_e[order]
    counts = jnp.zeros((N_EXPERTS,), jnp.int32).at[flat_e].add(1)
    padded = (counts + MOE_BLOCK - 1) // MOE_BLOCK * MOE_BLOCK
    pad_end = jnp.cumsum(padded)
    raw_start = jnp.cumsum(counts) - counts
    dest = (pad_end - padded)[e_sorted] + jnp.arange(n * TOP_K, dtype=jnp.int32) - raw_start[e_sorted]
    n_blocks = -(-(n * TOP_K) // MOE_BLOCK) + N_EXPERTS
    row_tok = jnp.full((n_blocks * MOE_BLOCK,), n, jnp.int32).at[dest].set(flat_tok[order])
    row_gate = jnp.zeros((n_blocks * MOE_BLOCK,), jnp.float32).at[dest].set(gates.reshape(-1)[order])
    block_start = jnp.arange(n_blocks, dtype=jnp.int32) * MOE_BLOCK
    block_e = jnp.minimum(jnp.sum(block_start[:, None] >= pad_end[None, :], axis=1), N_EXPERTS - 1)

    def expert_block(args):
        tok, e, gate = args
        xb = jnp.take(h, tok, axis=0, mode='fill', fill_value=0)
        glu, lin = jnp.split(xb @ w_gu[e] + b_gu[e], 2, axis=-1)
        glu = jnp.minimum(glu, SWIGLU_LIMIT)
        lin = jnp.clip(lin, -SWIGLU_LIMIT, SWIGLU_LIMIT)
        act = glu * jax.nn.sigmoid(SWIGLU_ALPHA * glu) * (lin + 1.0)
        return (act @ w_down[e] + b_down[e]) * gate[:, None].astype(h.dtype)

    y = lax.map(expert_block, (row_tok.reshape(n_blocks, MOE_BLOCK), block_e,
                               row_gate.reshape(n_blocks, MOE_BLOCK)))
    return jax.ops.segment_sum(y.reshape(-1, d), row_tok, num_segments=n)


def setup_inputs(seed: int = 0) -> dict:
    key = jax.random.key(seed)
    ks = iter(jax.random.split(key, 48))
    f32 = jnp.float32
    n_even = (DEPTH + 1) // 2
    n_odd = DEPTH // 2

    def nrm(shape, scale):
        return jax.random.normal(next(ks), shape, f32) * scale

    def unif(shape, lo, hi):
        return jax.random.uniform(next(ks), shape, f32, lo, hi)

    def gain(shape):
        return 1.0 + nrm(shape, 0.05)

    def lam_init(shape):
        u = unif(shape, LRU_MIN_RAD, LRU_MAX_RAD)
        return jnp.log(u) - jnp.log1p(-u)

    return {
        'x': nrm((BATCH, SEQ, D_MODEL), 1.0),
        'c': nrm((BATCH, D_MODEL), 1.0),
        'ctx': nrm((BATCH, CTX_LEN, D_MODEL), 1.0),
        'c_ctx': nrm((D_MODEL,), 1.0),
        'ada_w': nrm((DEPTH, D_MODEL, N_MOD * D_MODEL), 0.5 * D_MODEL ** -0.5),
        'ada_b': nrm((DEPTH, N_MOD * D_MODEL), 0.02),
        'norm_mix': gain((DEPTH, D_MODEL)),
        'norm_ffn': gain((DEPTH, D_MODEL)),
        'norm_final': gain((D_MODEL,)),
        'ev_w_in': nrm((n_even, D_MODEL, EVEN_IN), D_MODEL ** -0.5),
        'ev_w_out': nrm((n_even, LRU_WIDTH + RWKV_WIDTH, D_MODEL), (LRU_WIDTH + RWKV_WIDTH) ** -0.5),
        'lru_conv_w': nrm((n_even, CONV_WIDTH, LRU_WIDTH), CONV_WIDTH ** -0.5),
        'lru_conv_b': nrm((n_even, LRU_WIDTH), 0.02),
        'lru_gate_a_w': nrm((n_even, 2, LRU_BLOCKS, LRU_BLOCK, LRU_BLOCK), LRU_BLOCK ** -0.5),
        'lru_gate_a_b': nrm((n_even, 2, LRU_WIDTH), 0.1),
        'lru_gate_x_w': nrm((n_even, 2, LRU_BLOCKS, LRU_BLOCK, LRU_BLOCK), LRU_BLOCK ** -0.5),
        'lru_gate_x_b': nrm((n_even, 2, LRU_WIDTH), 0.1),
        'lru_lambda': lam_init((n_even, 2, LRU_WIDTH)),
        'rwkv_mu': unif((n_even, EVEN_IN - 2 * LRU_WIDTH), 0.0, 1.0),
        'rwkv_w0': unif((n_even, 2, RWKV_WIDTH), -6.0, -1.0),
        'rwkv_w2': nrm((n_even, 2, DECAY_LORA, RWKV_WIDTH), 0.5 * DECAY_LORA ** -0.5),
        'rwkv_a0': nrm((n_even, 2, RWKV_WIDTH), 0.1),
        'rwkv_a2': nrm((n_even, 2, ICL_LORA, RWKV_WIDTH), 0.5 * ICL_LORA ** -0.5),
        'rwkv_g2': nrm((n_even, GATE_LORA, RWKV_WIDTH), GATE_LORA ** -0.5),
        'rwkv_k_k': 0.85 + nrm((n_even, RWKV_WIDTH), 0.05),
        'rwkv_k_a': 1.0 + nrm((n_even, RWKV_WIDTH), 0.05),
        'rwkv_r_k': nrm((n_even, RWKV_HEADS, RWKV_HEAD), 0.1),
        'rwkv_ln_w': gain((n_even, RWKV_WIDTH)),
        'rwkv_ln_b': nrm((n_even, RWKV_WIDTH), 0.02),
        'od_w_in': nrm((n_odd, D_MODEL, ODD_IN), D_MODEL ** -0.5),
        'od_w_out': nrm((n_odd, GLA_VAL, D_MODEL), GLA_VAL ** -0.5),
        'gla_gk_w2': nrm((n_odd, 2, GLA_GATE_LORA, GLA_KEY), GLA_GATE_LORA ** -0.5),
        'gla_gk_b': nrm((n_odd, 2, GLA_KEY), 0.1),
        'gla_norm_w': gain((n_odd, GLA_DV)),
        'router_w': nrm((DEPTH, D_MODEL, N_EXPERTS), D_MODEL ** -0.5),
        'router_b': nrm((DEPTH, N_EXPERTS), 0.01),
        'exp_w_gu': nrm((DEPTH, N_EXPERTS, D_MODEL, 2 * D_EXPERT), D_MODEL ** -0.5),
        'exp_b_gu': nrm((DEPTH, N_EXPERTS, 2 * D_EXPERT), 0.02),
        'exp_w_down': nrm((DEPTH, N_EXPERTS, D_EXPERT, D_MODEL), D_EXPERT ** -0.5),
        'exp_b_down': nrm((DEPTH, N_EXPERTS, D_MODEL), 0.02),
    }


def reference(x, c, ctx, c_ctx, ada_w, ada_b, norm_mix, norm_ffn, norm_final,
              ev_w_in, ev_w_out, lru_conv_w, lru_conv_b, lru_gate_a_w, lru_gate_a_b,
              lru_gate_x_w, lru_gate_x_b, lru_lambda,
              rwkv_mu, rwkv_w0, rwkv_w2, rwkv_a0, rwkv_a2, rwkv_g2, rwkv_k_k, rwkv_k_a,
              rwkv_r_k, rwkv_ln_w, rwkv_ln_b,
              od_w_in, od_w_out, gla_gk_w2, gla_gk_b, gla_norm_w,
              router_w, router_b, exp_w_gu, exp_b_gu, exp_w_down, exp_b_down):
    bsz, seq, d = x.shape
    n_ctx = ctx.shape[1]
    rows = seq // GRID_W
    cond_lat = jax.nn.silu(c)
    cond_ctx = jax.nn.silu(c_ctx)
    for i in range(DEPTH):
        last = i == DEPTH - 1
        j = i // 2
        mod_lat = jnp.split((cond_lat @ ada_w[i] + ada_b[i])[:, None, :], N_MOD, axis=-1)
        mod_ctx = jnp.split(cond_ctx @ ada_w[i] + ada_b[i], N_MOD, axis=-1)

        h_lat = modulate(rms_norm(x, norm_mix[i]), mod_lat[0], mod_lat[1])
        h_ctx = modulate(rms_norm(ctx, norm_mix[i]), mod_ctx[0], mod_ctx[1])
        if i % 2 == 0:
            y = even_mixer(jnp.concatenate([h_ctx, h_lat], axis=1), n_ctx,
                           ev_w_in[j], ev_w_out[j], lru_conv_w[j], lru_conv_b[j],
                           lru_gate_a_w[j], lru_gate_a_b[j], lru_gate_x_w[j], lru_gate_x_b[j],
                           lru_lambda[j], rwkv_mu[j], rwkv_w0[j], rwkv_w2[j], rwkv_a0[j],
                           rwkv_a2[j], rwkv_g2[j], rwkv_k_k[j], rwkv_k_a[j], rwkv_r_k[j],
                           rwkv_ln_w[j], rwkv_ln_b[j])
            y_ctx, y_lat = y[:, :n_ctx], y[:, n_ctx:]
        else:
            y = odd_mixer(jnp.concatenate([h_ctx, to_column_major(h_lat, rows)], axis=1), n_ctx,
                          od_w_in[j], od_w_out[j], gla_gk_w2[j], gla_gk_b[j], gla_norm_w[j])
            y_ctx, y_lat = y[:, :n_ctx], from_column_major(y[:, n_ctx:], rows)
        x = x + mod_lat[2] * y_lat

        f_lat = modulate(rms_norm(x, norm_ffn[i]), mod_lat[3], mod_lat[4])
        moe_args = (router_w[i], router_b[i], exp_w_gu[i], exp_b_gu[i], exp_w_down[i], exp_b_down[i])
        if last:
            f = moe_ffn(f_lat.reshape(-1, d), *moe_args).reshape(bsz, seq, d)
            x = x + mod_lat[5] * f
        else:
            ctx = ctx + mod_ctx[2] * y_ctx
            f_ctx = modulate(rms_norm(ctx, norm_ffn[i]), mod_ctx[3], mod_ctx[4])
            f = moe_ffn(jnp.concatenate([f_ctx, f_lat], axis=1).reshape(-1, d), *moe_args)
            f = f.reshape(bsz, n_ctx + seq, d)
            ctx = ctx + mod_ctx[5] * f[:, :n_ctx]
            x = x + mod_lat[5] * f[:, n_ctx:]
    return rms_norm(x, norm_final)
```

```python
import numpy as np
import concourse.bass as bass
import concourse.mybir as mybir
from concourse.bass_utils import run_bass_kernel_spmd

F32 = mybir.dt.float32
BF16 = mybir.dt.bfloat16
I32 = mybir.dt.int32
U32 = mybir.dt.uint32
AF = mybir.ActivationFunctionType
ALU = mybir.AluOpType
AX = mybir.AxisListType

D = 4096
KC = 32
NTOK = 1088
NT = 9
TSEQ = 4352
NCTX = 256
EVEN_IN = 10976
ODD_IN = 12320
EPS = 1e-6


class Tok:
    __slots__ = ("name", "w", "r", "chan", "ccount")

    def __init__(self, name=""):
        self.name = name
        self.w = {}
        self.r = {}
        self.chan = None
        self.ccount = 0


class K:
    def __init__(self):
        self.nc = bass.Bass("TRN2", target_bir_lowering=False)
        nc = self.nc
        self.eng = {"pe": nc.tensor, "dve": nc.vector, "act": nc.scalar, "pool": nc.gpsimd, "sp": nc.sync}
        self.sem = {e: nc.alloc_semaphore("c_" + e) for e in self.eng}
        self.cnt = {e: 0 for e in self.eng}
        self.waited = {}
        self.pending = {}
        self.ninst = 0
        self._uid = 0

    def uid(self, p):
        self._uid += 1
        return f"{p}{self._uid}"

    def sb(self, shape, dt, name=None):
        return self.nc.alloc_sbuf_tensor(name or self.uid("sb"), list(shape), dt)

    def ps(self, shape, dt=F32, name=None):
        return self.nc.alloc_psum_tensor(name or self.uid("ps"), list(shape), dt)

    def dram(self, name, shape, dt, kind="Internal"):
        return self.nc.dram_tensor(name, list(shape), dt, kind=kind)

    def inp(self, name, shape, dt=F32):
        return self.dram(name, shape, dt, kind="ExternalInput").ap()

    def outp(self, name, shape, dt=F32):
        return self.dram(name, shape, dt, kind="ExternalOutput").ap()

    def tok(self, name=""):
        return Tok(name)

    def toks(self, n):
        return [Tok() for _ in range(n)]

    def _wait(self, e, ev):
        for sid, (sem, val) in ev.items():
            if e == "pe" and sem is self.sem["pe"]:
                continue
            key = (e, sid)
            if self.waited.get(key, 0) >= val:
                continue
            self.waited[key] = val
            self.eng[e].wait_ge(sem, val)
            self.ninst += 1

    @staticmethod
    def _merge(d, ev):
        for sid, (sem, val) in ev.items():
            if sid not in d or d[sid][1] < val:
                d[sid] = (sem, val)

    def _deps(self, e, reads, writes, acc):
        for t in reads:
            self._wait(e, t.w)
        for t in writes:
            self._wait(e, t.w)
            self._wait(e, t.r)
        for t in acc:
            self._wait(e, t.r)

    def _commit(self, ev, reads, writes, acc):
        for t in reads:
            self._merge(t.r, ev)
        for t in writes:
            t.w = dict(ev)
            t.r = {}
        for t in acc:
            self._merge(t.w, ev)
            t.r = {}

    def op(self, e, fn, reads=(), writes=(), acc=(), inc=True):
        self._deps(e, reads, writes, acc)
        ins = fn(self.eng[e])
        self.ninst += 1
        pend = self.pending.setdefault(e, [])
        pend.append((tuple(reads), tuple(writes), tuple(acc)))
        if not inc:
            return None
        self.cnt[e] += 1
        ins.then_inc(self.sem[e], 1)
        ev = {id(self.sem[e]): (self.sem[e], self.cnt[e])}
        for (r, w, a) in pend:
            self._commit(ev, r, w, a)
        self.pending[e] = []
        return ev

    def dma(self, e, pairs, owner, reads=(), writes=(), acc=(), **kw):
        if owner.chan is None:
            owner.chan = self.nc.alloc_semaphore(self.uid("d"))
        self._deps(e, reads, writes, acc)
        for (o, i) in pairs:
            self.eng[e].dma_start(out=o, in_=i, **kw).then_inc(owner.chan, 16)
            owner.ccount += 16
            self.ninst += 1
        ev = {id(owner.chan): (owner.chan, owner.ccount)}
        self._commit(ev, reads, writes, acc)
        return ev

    def finish(self, toks, e="sp"):
        for t in toks:
            self._wait(e, t.w)
            self._wait(e, t.r)


def make_ident(k, dt=F32):
    idf = k.sb([128, 128], F32)
    t = k.tok()
    k.op("pool", lambda g: g.memset(idf[:], 0.0), writes=[t])
    k.op("pool", lambda g: g.affine_select(out=idf[:], in_=idf[:], compare_op=ALU.not_equal, fill=1.0,
                                            base=0, pattern=[[-1, 128]], channel_multiplier=1),
         reads=[t], writes=[t])
    if dt == F32:
        return idf, t
    idb = k.sb([128, 128], dt)
    t2 = k.tok()
    k.op("dve", lambda v: v.tensor_copy(idb[:], idf[:]), reads=[t], writes=[t2])
    return idb, t2


def tile_rows(i):
    return 128 if i < 8 else 64


def build_mods():
    k = K()
    condT = k.inp("condT", [128, KC, 3])
    aw = k.inp("aw", [2, D, 3072])
    ab = k.inp("ab", [2, 3, 3072])
    out = k.outp("mod", [2, 3, 3072])
    ct = k.sb([128, KC, 3], F32); t_c = k.tok()
    sc = k.sb([128, KC, 3], F32); t_s = k.tok()
    abt = k.sb([3, 2, 3072], F32); t_ab = k.tok()
    ot = k.sb([3, 2, 3072], F32); t_o = k.tok()
    k.dma("sp", [(ct[:], condT)], t_c, writes=[t_c])
    k.dma("sp", [(abt[:, l, :], ab[l]) for l in range(2)], t_ab, writes=[t_ab])
    k.op("act", lambda a: a.activation(sc[:], ct[:], AF.Silu), reads=[t_c], writes=[t_s])
    wb = [k.sb([128, KC, 512], F32) for _ in range(2)]
    t_w = k.toks(2)
    pss = [k.ps([3, 512], F32) for _ in range(2)]
    t_p = k.toks(2)
    it = 0
    for l in range(2):
        for nb in range(6):
            s = it % 2
            src = aw[l, :, nb * 512:(nb + 1) * 512].rearrange("(kc p) n -> p kc n", p=128)
            k.dma("sp", [(wb[s][:, 0:16, :], src[:, 0:16, :]), (wb[s][:, 16:32, :], src[:, 16:32, :])],
                  t_w[s], writes=[t_w[s]])
            for kc in range(KC):
                k.op("pe", lambda p, kc=kc, s=s: p.matmul(pss[s][:], sc[:, kc, :], wb[s][:, kc, :],
                                                           start=(kc == 0), stop=(kc == KC - 1)),
                     reads=[t_s, t_w[s]], writes=[t_p[s]] if kc == 0 else (), acc=[t_p[s]] if kc else ())
            k.op("dve", lambda v, s=s, l=l, nb=nb: v.tensor_tensor(ot[:, l, nb * 512:(nb + 1) * 512], pss[s][:],
                                                                   abt[:, l, nb * 512:(nb + 1) * 512], ALU.add),
                 reads=[t_p[s], t_ab], acc=[t_o])
            it += 1
    t_out = k.tok()
    k.dma("sp", [(out[l], ot[:, l, :]) for l in range(2)], t_o, reads=[t_o], writes=[t_out])
    k.finish([t_out])
    return k


class FrontBufs:
    def __init__(self, k, nbuf=2):
        self.nbuf = nbuf
        self.xt = [k.sb([128, D], F32) for _ in range(nbuf)]
        self.t_xt = k.toks(nbuf)
        self.junk = k.sb([128, D], BF16)
        self.t_junk = k.tok()
        self.ss = k.sb([128, NT], F32)
        self.den = k.sb([128, NT], F32)
        self.rstd = k.sb([128, NT], F32)
        self.t_ss = k.toks(NT)
        self.tp = [k.ps([128, 4, 128], F32) for _ in range(2)]
        self.t_tp = k.toks(2)
        self.t_ser = k.toks(2)
        self.ident, self.t_id = make_ident(k, F32)
        self.n = 0


def front_end(k, fb, x_src, A_of, B_of, t_vec, hT, t_hT, f32T_cb=None, tiles=None, rows_of=tile_rows, tile_done=None, t_src=None):
    for i in (tiles if tiles is not None else range(NT)):
        rows = rows_of(i)
        s = fb.n % fb.nbuf
        fb.n += 1
        xt = fb.xt[s]
        t_x = fb.t_xt[s]
        k.dma("sp", [(xt[:rows, 0:2048], x_src[i * 128:i * 128 + rows, 0:2048]),
                     (xt[:rows, 2048:4096], x_src[i * 128:i * 128 + rows, 2048:4096])], t_x,
              reads=[t_src] if t_src is not None else (), writes=[t_x])
        k.op("act", lambda a: a.activation(fb.junk[:rows, :], xt[:rows, :], AF.Square, accum_out=fb.ss[:rows, i:i + 1]),
             reads=[t_x], writes=[fb.t_junk, fb.t_ss[i]])
        k.op("act", lambda a: a.activation(fb.den[:rows, i:i + 1], fb.ss[:rows, i:i + 1], AF.Sqrt, bias=EPS, scale=1.0 / D),
             reads=[fb.t_ss[i]], writes=[fb.t_ss[i]])
        k.op("dve", lambda v: v.reciprocal(fb.rstd[:rows, i:i + 1], fb.den[:rows, i:i + 1]),
             reads=[fb.t_ss[i]], writes=[fb.t_ss[i]])
        k.op("act", lambda a: a.activation(xt[:rows, :], xt[:rows, :], AF.Copy, scale=fb.rstd[:rows, i:i + 1]),
             reads=[fb.t_ss[i]], writes=[t_x])
        A = A_of(i)
        B = B_of(i)
        for g in range(8):
            ps = fb.tp[g % 2]
            t_ps = fb.t_tp[g % 2]
            for j in range(4):
                kc = g * 4 + j
                k.op("pe", lambda p, kc=kc, j=j: p.transpose(ps[:, j, :rows], xt[:rows, kc * 128:(kc + 1) * 128],
                                                              fb.ident[:rows, :rows]),
                     reads=[t_x, fb.t_id], writes=[t_ps] if j == 0 else (), acc=[t_ps] if j else (), inc=(j == 3))
            for j in range(4):
                kc = g * 4 + j
                dst = hT[:, kc, i * 128:i * 128 + rows]
                if j % 2 == 0:
                    k.op("dve", lambda v, kc=kc, j=j, dst=dst: v.tensor_scalar(dst, ps[:, j, :rows], A[:, kc:kc + 1], B[:, kc:kc + 1],
                                                                               ALU.mult, ALU.add),
                         reads=[t_ps, t_vec], writes=[t_hT[i], fb.t_ser[g % 2]])
                else:
                    k.op("act", lambda a, kc=kc, j=j, dst=dst: a.activation(dst, ps[:, j, :rows], AF.Identity,
                                                                            bias=B[:, kc:kc + 1], scale=A[:, kc:kc + 1]),
                         reads=[t_ps, t_vec], writes=[t_hT[i], fb.t_ser[g % 2]])
                if f32T_cb is not None:
                    f32T_cb(i, kc, ps[:, j, :rows], t_ps, A, B, fb.t_ser[g % 2])
        if tile_done is not None:
            tile_done(i)


def make_AB(k, vec, t_vec, AB, t_AB, gi, sci, shi, oi):
    k.op("dve", lambda v: v.tensor_scalar(AB[:, oi, :], vec[:, sci, :], 1.0, None, ALU.add), reads=[t_vec], writes=[t_AB])
    k.op("dve", lambda v: v.tensor_tensor(AB[:, oi, :], AB[:, oi, :], vec[:, gi, :], ALU.mult), reads=[t_vec, t_AB], writes=[t_AB])
    k.op("dve", lambda v: v.tensor_copy(AB[:, oi + 1, :], vec[:, shi, :]), reads=[t_vec, t_AB], writes=[t_AB])


NBLK = [(0, 512), (512, 512), (1024, 64)]


class GemmBufs:
    def __init__(self, k, cb=256, nwb=3):
        self.cb = cb
        self.nwb = nwb
        self.wb = [k.sb([128, KC, cb], BF16) for _ in range(nwb)]
        self.t_wb = k.toks(nwb)
        self.ps = [[k.ps([128, 512], F32) for _ in range(3)] for _ in range(2)]
        self.t_ps = [k.toks(3) for _ in range(2)]
        self.osb = [k.sb([128, NTOK], F32) for _ in range(2)]
        self.t_osb = k.toks(2)
        self.nw = 0
        self.nc_ = 0


def gemm_f(k, gb, W, ncols, hT, t_hT, outT, t_out, epilogue=None):
    cb = gb.cb
    nblocks = (ncols + cb - 1) // cb
    for b in range(nblocks):
        c0 = b * cb
        cw = min(cb, ncols - c0)
        s = gb.nw % gb.nwb
        gb.nw += 1
        wb = gb.wb[s]
        t_w = gb.t_wb[s]
        src = W[:, c0:c0 + cw].rearrange("(kc p) n -> p kc n", p=128)
        k.dma("pool", [(wb[:, 0:16, :cw], src[:, 0:16, :]), (wb[:, 16:32, :cw], src[:, 16:32, :])], t_w, writes=[t_w])
        for sub in range((cw + 127) // 128):
            m0 = sub * 128
            m = min(128, cw - m0)
            pi = gb.nc_ % 2
            gb.nc_ += 1
            for nt, (t0, n) in enumerate(NBLK):
                ps = gb.ps[pi][nt]
                t_p = gb.t_ps[pi][nt]
                rd = [t_w] + [t_hT[i] for i in range(t0 // 128, (t0 + n + 127) // 128)]
                for kc in range(KC):
                    k.op("pe", lambda p, kc=kc, ps=ps, t0=t0, n=n, m0=m0, m=m: p.matmul(
                        ps[:m, :n], wb[:, kc, m0:m0 + m], hT[:, kc, t0:t0 + n], start=(kc == 0), stop=(kc == KC - 1)),
                        reads=rd if kc == 0 else [t_w], writes=[t_p] if kc == 0 else (), acc=[t_p] if kc else (),
                        inc=(kc == KC - 1))
            if epilogue is not None:
                epilogue(c0 + m0, m, gb.ps[pi], gb.t_ps[pi])
                continue
            osb = gb.osb[pi]
            t_o = gb.t_osb[pi]
            for nt, (t0, n) in enumerate(NBLK):
                ps = gb.ps[pi][nt]
                t_p = gb.t_ps[pi][nt]
                if nt == 1:
                    k.op("dve", lambda v, ps=ps, t0=t0, n=n: v.tensor_copy(osb[:m, t0:t0 + n], ps[:m, :n]),
                         reads=[t_p], writes=[t_o])
                else:
                    k.op("act", lambda a, ps=ps, t0=t0, n=n: a.activation(osb[:m, t0:t0 + n], ps[:m, :n], AF.Copy),
                         reads=[t_p], writes=[t_o])
            k.dma("sp", [(outT[c0 + m0:c0 + m0 + m, :], osb[:m, :])], t_o, reads=[t_o], acc=[t_out])


def build_inproj(ncols):
    k = K()
    x = k.inp("x", [NTOK, D])
    vec_d = k.inp("vec", [128, 8, KC])
    W = k.inp("w", [D, ncols])
    outT = k.outp("projT", [ncols, NTOK])
    vec = k.sb([128, 8, KC], F32); t_vec = k.tok()
    AB = k.sb([128, 4, KC], F32); t_AB = k.tok()
    k.dma("sp", [(vec[:], vec_d)], t_vec, writes=[t_vec])
    make_AB(k, vec, t_vec, AB, t_AB, 0, 1, 2, 0)
    make_AB(k, vec, t_vec, AB, t_AB, 0, 3, 4, 2)
    hT = k.sb([128, KC, NTOK], BF16)
    t_hT = k.toks(NT)
    fb = FrontBufs(k)
    front_end(k, fb, x, lambda i: AB[:, 0 if i < 2 else 2, :], lambda i: AB[:, 1 if i < 2 else 3, :], t_AB, hT, t_hT)
    gb = GemmBufs(k)
    t_out = k.tok()
    gemm_f(k, gb, W, ncols, hT, t_hT, outT, t_out)
    k.finish([t_out])
    return k


def build_final_norm():
    k = K()
    x = k.inp("x", [NTOK, D])
    g_d = k.inp("g", [128, D])
    out = k.outp("o", [NTOK, D])
    g = k.sb([128, D], F32); t_g = k.tok()
    k.dma("sp", [(g[:], g_d)], t_g, writes=[t_g])
    xt = [k.sb([128, D], F32) for _ in range(2)]; t_x = k.toks(2)
    junk = k.sb([128, D], BF16); t_j = k.tok()
    st = k.sb([128, NT, 3], F32); t_s = k.toks(NT)
    t_out = k.tok()
    for i in range(NT):
        rows = tile_rows(i)
        s = i % 2
        r0 = i * 128
        k.dma("sp", [(xt[s][:rows, 0:2048], x[r0:r0 + rows, 0:2048]), (xt[s][:rows, 2048:4096], x[r0:r0 + rows, 2048:4096])],
              t_x[s], writes=[t_x[s]])
        k.op("act", lambda a: a.activation(junk[:rows, :], xt[s][:rows, :], AF.Square, accum_out=st[:rows, i, 0:1]),
             reads=[t_x[s]], writes=[t_j, t_s[i]])
        k.op("act", lambda a: a.activation(st[:rows, i, 1:2], st[:rows, i, 0:1], AF.Sqrt, bias=EPS, scale=1.0 / D),
             reads=[t_s[i]], writes=[t_s[i]])
        k.op("dve", lambda v: v.reciprocal(st[:rows, i, 2:3], st[:rows, i, 1:2]), reads=[t_s[i]], writes=[t_s[i]])
        k.op("act", lambda a: a.activation(xt[s][:rows, :], xt[s][:rows, :], AF.Copy, scale=st[:rows, i, 2:3]),
             reads=[t_s[i]], writes=[t_x[s]])
        k.op("dve", lambda v: v.tensor_tensor(xt[s][:rows, :], xt[s][:rows, :], g[:rows, :], ALU.mult),
             reads=[t_g], writes=[t_x[s]])
        k.dma("sp", [(out[r0:r0 + rows, :], xt[s][:rows, :])], t_x[s], reads=[t_x[s]], acc=[t_out])
    k.finish([t_out])
    return k


def build_outproj():
    k = K()
    yT_d = k.inp("yT", [D, NTOK])
    x = k.inp("x", [NTOK, D])
    g_d = k.inp("g2", [128, 2, D])
    W = k.inp("w", [D, D])
    out = k.outp("x1", [NTOK, D])
    yT = k.sb([128, KC, NTOK], BF16); t_y = k.tok()
    src = yT_d.rearrange("(kc p) t -> p kc t", p=128)
    k.dma("pool", [(yT[:, 8 * j:8 * j + 8, :], src[:, 8 * j:8 * j + 8, :]) for j in range(4)], t_y, writes=[t_y])
    g2 = k.sb([128, 2, D], F32); t_g = k.tok()
    k.dma("sp", [(g2[:, c, :], g_d[:, c, :]) for c in range(2)], t_g, writes=[t_g])
    wb = [k.sb([128, KC, 512], BF16) for _ in range(2)]; t_w = k.toks(2)
    ps = [k.ps([128, 512], F32) for _ in range(2)]; t_p = k.toks(2)
    xt = [k.sb([128, 512], F32) for _ in range(2)]; t_x = k.toks(2)
    tm = [k.sb([128, 512], F32) for _ in range(2)]; t_t = k.toks(2)
    t_out = k.tok()
    it = 0
    for cg in range(8):
        c0 = cg * 512
        s = cg % 2
        wsrc = W[:, c0:c0 + 512].rearrange("(kc p) n -> p kc n", p=128)
        k.dma("pool", [(wb[s][:, 0:16, :], wsrc[:, 0:16, :]), (wb[s][:, 16:32, :], wsrc[:, 16:32, :])], t_w[s], writes=[t_w[s]])
        for i in range(NT):
            rows = tile_rows(i)
            r0 = i * 128
            b = it % 2
            it += 1
            cls = 0 if i < 2 else 1
            k.dma("sp", [(xt[b][:rows, :], x[r0:r0 + rows, c0:c0 + 512])], t_x[b], writes=[t_x[b]])
            for kc in range(KC):
                k.op("pe", lambda p, kc=kc: p.matmul(ps[b][:rows, :], yT[:, kc, r0:r0 + rows], wb[s][:, kc, :],
                                                      start=(kc == 0), stop=(kc == KC - 1)),
                     reads=[t_y, t_w[s]] if kc == 0 else (), writes=[t_p[b]] if kc == 0 else (), acc=[t_p[b]] if kc else (),
                     inc=(kc == KC - 1))
            k.op("dve", lambda v: v.tensor_tensor(tm[b][:rows, :], ps[b][:rows, :], g2[:rows, cls, c0:c0 + 512], ALU.mult),
                 reads=[t_p[b], t_g], writes=[t_t[b]])
            k.op("dve", lambda v: v.tensor_tensor(tm[b][:rows, :], tm[b][:rows, :], xt[b][:rows, :], ALU.add),
                 reads=[t_x[b]], writes=[t_t[b]])
            k.dma("sp", [(out[r0:r0 + rows, c0:c0 + 512], tm[b][:rows, :])], t_t[b], reads=[t_t[b]], acc=[t_out])
    k.finish([t_out])
    return k


def emit_combine(k, parts, x1, g5, t_g5, out, t_out, nparts=8, bufs=None, nld=1):
    if bufs is not None:
        acc, t_a = bufs
    else:
        acc = [k.sb([128, D], F32) for _ in range(2)]; t_a = k.toks(2)
    ld = [k.sb([128, D], F32) for _ in range(nld)]; t_l = k.toks(nld)
    n = 0
    for i in range(NT):
        rows = tile_rows(i)
        r0 = i * 128
        a = i % 2
        cls = 0 if i < 2 else 1
        k.dma("sp", [(acc[a][:rows, :], parts[0, r0:r0 + rows, :])], t_a[a], writes=[t_a[a]])
        for c in range(1, nparts + 1):
            b = n % nld
            n += 1
            srcap = parts[c, r0:r0 + rows, :] if c < nparts else x1[r0:r0 + rows, :]
            k.dma("sp", [(ld[b][:rows, :], srcap)], t_l[b], writes=[t_l[b]])
            if c == nparts:
                k.op("dve", lambda v: v.tensor_tensor(acc[a][:rows, :], acc[a][:rows, :], g5[:rows, cls, :], ALU.mult),
                     reads=[t_g5], writes=[t_a[a]])
            k.op("dve", lambda v: v.tensor_tensor(acc[a][:rows, :], acc[a][:rows, :], ld[b][:rows, :], ALU.add),
                 reads=[t_l[b]], writes=[t_a[a]])
        k.dma("sp", [(out[r0:r0 + rows, :], acc[a][:rows, :])], t_a[a], reads=[t_a[a]], acc=[t_out])


NEXP = 8
MOE_BLOCKS = [(i * 512, 512) for i in range(8)] + [(4096, 256)]


def build_moe(nblocks=len(MOE_BLOCKS), stage=3):
    k = K()
    x1 = k.inp("x1", [TSEQ, D])
    vec_d = k.inp("vec", [128, 8, KC])
    rw_d = k.inp("rw", [D, 32])
    rb_d = k.inp("rb", [128, 32])
    wgu = k.inp("wgu", [NEXP, D, 1024])
    bgu_d = k.inp("bgu", [128, NEXP, 8])
    wd = k.inp("wd", [NEXP, 512, D])
    bd = k.inp("bd", [NEXP, D])
    out = k.outp("ypart", [TSEQ, D])

    vec = k.sb([128, 8, KC], F32); t_vec = k.tok()
    AB = k.sb([128, 4, KC], F32); t_AB = k.tok()
    k.dma("sp", [(vec[:], vec_d)], t_vec, writes=[t_vec])
    for c in range(2):
        make_AB(k, vec, t_vec, AB, t_AB, 0, 1 + 2 * c, 2 + 2 * c, 2 * c)
    rw = k.sb([128, KC, 32], F32); t_rw = k.tok()
    k.dma("sp", [(rw[:], rw_d.rearrange("(kc p) e -> p kc e", p=128))], t_rw, writes=[t_rw])
    rb = k.sb([128, 32], F32)
    bgu = k.sb([128, NEXP, 8], F32)
    ones = k.sb([1, 128], F32)
    t_c = k.tok()
    k.dma("sp", [(rb[:], rb_d), (bgu[:], bgu_d)], t_c, writes=[t_c])
    t_one = k.tok()
    k.op("dve", lambda v: v.memset(ones[:], 1.0), writes=[t_one])

    hT = k.sb([128, KC, 512], BF16); t_hT = k.toks(4)
    fb = FrontBufs(k, nbuf=1)
    f32s = k.sb([128, KC, 128], F32); t_f32 = k.toks(KC)
    lg_ps = k.ps([128, 32], F32); t_lg = k.tok()
    lg = k.sb([128, 4, 32], F32)
    m8 = k.sb([128, 4, 8], F32)
    sm = k.sb([128, 4, 4], F32)
    ex = k.sb([128, 4, 32], F32)
    G = k.sb([128, 4, 32], F32)
    t_G = k.toks(4)
    wb = [k.sb([128, KC, 256], BF16) for _ in range(2)]; t_wb = k.toks(2)
    ups = [k.ps([128, 512], F32) for _ in range(2)]; t_up = k.toks(2)
    g1 = k.sb([128, 512], F32); sg = k.sb([128, 512], F32); l1 = k.sb([128, 512], F32)
    t_g1 = k.tok(); t_sg = k.tok(); t_l1 = k.tok()
    actT = k.sb([128, NEXP, 4, 512], BF16); t_act = [k.toks(4) for _ in range(NEXP)]
    wdb = [k.sb([128, 4, 4, 512], BF16) for _ in range(2)]; t_wd = k.toks(2)
    bdt = [k.sb([1, 4, 512], F32) for _ in range(2)]; t_bd = k.toks(2)
    dps = [k.ps([128, 512], F32) for _ in range(2)]; t_dp = k.toks(2)
    acc = k.sb([128, 4, 512], F32); t_acc = k.toks(4)
    t_out = k.tok()
    nwb = 0
    nwd = 0
    ndp = 0

    for blk in range(nblocks):
        tok0, bn = MOE_BLOCKS[blk]
        nti = bn // 128

        def f32_cb(i, kc, ps_ap, t_ps, A, B, t_ser):
            k.op("dve", lambda v: v.tensor_scalar(f32s[:, kc, :], ps_ap, A[:, kc:kc + 1], B[:, kc:kc + 1], ALU.mult, ALU.add),
                 reads=[t_ps, t_AB], writes=[t_f32[kc], t_ser])
            k.op("pe", lambda p: p.matmul(lg_ps[:, :], f32s[:, kc, :], rw[:, kc, :], start=(kc == 0), stop=(kc == KC - 1)),
                 reads=[t_f32[kc], t_rw], writes=[t_lg] if kc == 0 else (), acc=[t_lg] if kc else (), inc=(kc == KC - 1))

        def tile_done(i):
            tg = t_G[i]
            k.op("dve", lambda v: v.tensor_tensor(lg[:, i, :], lg_ps[:, :], rb[:, :], ALU.add), reads=[t_lg, t_c], writes=[tg])
            k.op("dve", lambda v: v.max(m8[:, i, :], lg[:, i, :]), reads=[tg], writes=[tg])
            k.op("dve", lambda v: v.tensor_scalar(sm[:, i, 0:1], m8[:, i, 0:1], -1.0, None, ALU.mult), reads=[tg], writes=[tg])
            k.op("act", lambda a: a.activation(ex[:, i, :], lg[:, i, :], AF.Exp, bias=sm[:, i, 0:1], scale=1.0), reads=[tg], writes=[tg])
            k.op("dve", lambda v: v.tensor_scalar(G[:, i, :], lg[:, i, :], m8[:, i, 3:4], None, ALU.is_ge), reads=[tg], writes=[tg])
            k.op("dve", lambda v: v.tensor_tensor(ex[:, i, :], ex[:, i, :], G[:, i, :], ALU.mult), reads=[tg], writes=[tg])
            k.op("dve", lambda v: v.reduce_sum(sm[:, i, 1:2], ex[:, i, :], axis=AX.X), reads=[tg], writes=[tg])
            k.op("dve", lambda v: v.reciprocal(sm[:, i, 2:3], sm[:, i, 1:2]), reads=[tg], writes=[tg])
            k.op("dve", lambda v: v.tensor_scalar(G[:, i, :], ex[:, i, :], sm[:, i, 2:3], None, ALU.mult), reads=[tg], writes=[tg])

        def cls(i):
            return 0 if (tok0 // 128 + i) < 2 else 1
        front_end(k, fb, x1[tok0:tok0 + bn, :], lambda i: AB[:, 2 * cls(i), :], lambda i: AB[:, 2 * cls(i) + 1, :],
                  t_AB, hT, t_hT, f32T_cb=f32_cb, tiles=range(nti), rows_of=lambda i: 128, tile_done=tile_done)
        if stage == 1:
            for i in range(nti):
                k.dma("sp", [(out[tok0 + i * 128:tok0 + i * 128 + 128, 0:32], G[:, i, :])], t_G[i], reads=[t_G[i]], acc=[t_out])
            continue
        rd_h = t_hT[:nti]
        for j in range(NEXP):
            for cc in range(4):
                s = nwb % 2
                nwb += 1
                src = wgu[j, :, cc * 256:(cc + 1) * 256].rearrange("(kc p) n -> p kc n", p=128)
                k.dma("pool", [(wb[s][:, 0:16, :], src[:, 0:16, :]), (wb[s][:, 16:32, :], src[:, 16:32, :])], t_wb[s], writes=[t_wb[s]])
                for h in range(2):
                    for kc in range(KC):
                        k.op("pe", lambda p, kc=kc, h=h: p.matmul(ups[h][:, :bn], wb[s][:, kc, h * 128:(h + 1) * 128], hT[:, kc, :bn],
                                                                   start=(kc == 0), stop=(kc == KC - 1)),
                             reads=[t_wb[s]] + rd_h if kc == 0 else (), writes=[t_up[h]] if kc == 0 else (), acc=[t_up[h]] if kc else (),
                             inc=(kc == KC - 1))
                k.op("dve", lambda v: v.tensor_scalar(g1[:, :bn], ups[0][:, :bn], bgu[:, j, 2 * cc:2 * cc + 1], 7.0, ALU.add, ALU.min),
                     reads=[t_up[0], t_c], writes=[t_g1])
                k.op("act", lambda a: a.activation(sg[:, :bn], g1[:, :bn], AF.Sigmoid, scale=1.702), reads=[t_g1], writes=[t_sg])
                k.op("dve", lambda v: v.tensor_scalar(l1[:, :bn], ups[1][:, :bn], bgu[:, j, 2 * cc + 1:2 * cc + 2], 7.0, ALU.add, ALU.min),
                     reads=[t_up[1], t_c], writes=[t_l1])
                k.op("dve", lambda v: v.tensor_scalar(l1[:, :bn], l1[:, :bn], -7.0, 1.0, ALU.max, ALU.add), reads=[t_l1], writes=[t_l1])
                k.op("dve", lambda v: v.tensor_tensor(g1[:, :bn], g1[:, :bn], sg[:, :bn], ALU.mult), reads=[t_sg], writes=[t_g1])
                k.op("dve", lambda v: v.tensor_tensor(actT[:, j, cc, :bn], g1[:, :bn], l1[:, :bn], ALU.mult), reads=[t_g1, t_l1], writes=[t_act[j][cc]])
        for cg in range(8):
            c0 = cg * 512
            for eh in range(NEXP // 4):
                s = nwd % 2
                nwd += 1
                k.dma("pool", [(wdb[s][:, jl, :, :], wd[4 * eh + jl, :, c0:c0 + 512].rearrange("(fc p) n -> p fc n", p=128)) for jl in range(4)],
                      t_wd[s], writes=[t_wd[s]])
                k.dma("sp", [(bdt[s][:, jl, :], bd[4 * eh + jl:4 * eh + jl + 1, c0:c0 + 512]) for jl in range(4)], t_bd[s], writes=[t_bd[s]])
                for i in range(nti):
                    for jl in range(4):
                        j = 4 * eh + jl
                        d = ndp % 2
                        ndp += 1
                        for fc in range(4):
                            k.op("pe", lambda p, fc=fc: p.matmul(dps[d][:, :], actT[:, j, fc, i * 128:(i + 1) * 128], wdb[s][:, jl, fc, :],
                                                                  start=(fc == 0), stop=False),
                                 reads=[t_wd[s]] + t_act[j] if fc == 0 else (), writes=[t_dp[d]] if fc == 0 else (), acc=[t_dp[d]] if fc else (),
                                 inc=False)
                        k.op("pe", lambda p: p.matmul(dps[d][:, :], ones[:, :], bdt[s][:, jl, :], start=False, stop=True),
                             reads=[t_one, t_bd[s]], acc=[t_dp[d]])
                        if j == 0:
                            k.op("dve", lambda v: v.tensor_scalar(acc[:, i, :], dps[d][:], G[:, i, 0:1], None, ALU.mult),
                                 reads=[t_dp[d], t_G[i]], writes=[t_acc[i]])
                        else:
                            k.op("dve", lambda v: v.scalar_tensor_tensor(acc[:, i, :], dps[d][:], G[:, i, j:j + 1], acc[:, i, :], ALU.mult, ALU.add),
                                 reads=[t_dp[d], t_G[i]], writes=[t_acc[i]])
            for i in range(nti):
                r0 = tok0 + i * 128
                k.dma("sp", [(out[r0:r0 + 128, c0:c0 + 512], acc[:, i, :])], t_acc[i], reads=[t_acc[i]], acc=[t_out])
    k.finish([t_out])
    return k


T = TSEQ
SEGS = [(0, NCTX), (NCTX, TSEQ)]
TBLK = [(i * 512, min(512, TSEQ - i * 512)) for i in range(9)]


def rev(ap2d, lo, hi):
    return ap2d[:, hi - 1::-1] if lo == 0 else ap2d[:, hi - 1:lo - 1:-1]


class Pool9:
    def __init__(self, k, n=9):
        self.t = [k.sb([128, T], F32) for _ in range(n)]
        self.k = k.toks(n)


def shifted_mac(k, out, t_out, src, t_src, wcol, off, extra_reads=()):
    for (lo, hi) in SEGS:
        o_lo, o_hi = max(lo, lo - off), min(hi, hi - off)
        k.op("dve", lambda v: v.scalar_tensor_tensor(out[:, o_lo:o_hi], src[:, o_lo + off:o_hi + off], wcol, out[:, o_lo:o_hi],
                                                     ALU.mult, ALU.add),
             reads=[t_src] + list(extra_reads), writes=[t_out])


def emit_lru(k, P, PS, tPS, prm, t_prm, gw, t_gw, xaT, gateT, yT_out, t_yout):
    S, tS = P.t, P.k
    spn = k.sb([128, 4, 4], F32)
    t_sp = k.tok()
    k.op("act", lambda a: a.activation(spn[:, :, 0:2], prm[:, :, 9:11], AF.Exp, scale=-1.0), reads=[t_prm], writes=[t_sp])
    k.op("act", lambda a: a.activation(spn[:, :, 0:2], spn[:, :, 0:2], AF.Ln, bias=1.0), writes=[t_sp])
    k.op("dve", lambda v: v.tensor_scalar(spn[:, :, 2:4], spn[:, :, 0:2], -16.0, None, ALU.mult), writes=[t_sp])
    k.op("dve", lambda v: v.tensor_scalar(spn[:, :, 0:2], spn[:, :, 0:2], -8.0, None, ALU.mult), writes=[t_sp])
    ps = PS[0:2]
    t_ps = tPS[0:2]
    npi = 0
    for n in range(4):
        pr = prm[:, n, :]
        k.dma("sp", [(S[0][:, :], xaT[n * 128:(n + 1) * 128, :])], tS[0], writes=[tS[0]])
        k.dma("sp", [(S[7][:, :], gateT[n * 128:(n + 1) * 128, :])], tS[7], writes=[tS[7]])
        k.op("dve", lambda v: v.tensor_scalar(S[1][:, :], S[0][:, :], pr[:, 2:3], pr[:, 4:5], ALU.mult, ALU.add),
             reads=[tS[0], t_prm], writes=[tS[1]])
        shifted_mac(k, S[1], tS[1], S[0], tS[0], pr[:, 0:1], -2)
        shifted_mac(k, S[1], tS[1], S[0], tS[0], pr[:, 1:2], -1)
        shifted_mac(k, S[1], tS[1], S[0], tS[0], pr[:, 3:4], +1)
        for d in range(2):
            hdst = S[5 + d]
            t_h = tS[5 + d]
            for gi, dst in ((0, 2), (1, 3)):
                wi = d * 2 + gi
                for (t0, nn) in TBLK:
                    pb = npi % 2
                    npi += 1
                    k.op("pe", lambda p: p.matmul(ps[pb][:, :nn], gw[:, n, wi, :], S[1][:, t0:t0 + nn], start=True, stop=True),
                         reads=[tS[1], t_gw], writes=[t_ps[pb]])
                    k.op("act", lambda a: a.activation(S[dst][:, t0:t0 + nn], ps[pb][:, :nn], AF.Sigmoid, bias=pr[:, 5 + wi:6 + wi]),
                         reads=[t_ps[pb], t_prm], writes=[tS[dst]])
            k.op("act", lambda a: a.activation(S[4][:, :], S[2][:, :], AF.Exp, scale=spn[:, n, d:d + 1]), reads=[tS[2], t_sp], writes=[tS[4]])
            k.op("act", lambda a: a.activation(S[2][:, :], S[2][:, :], AF.Exp, scale=spn[:, n, 2 + d:3 + d]), reads=[t_sp], writes=[tS[2]])
            k.op("act", lambda a: a.activation(S[2][:, :], S[2][:, :], AF.Sqrt, bias=1.0, scale=-1.0), writes=[tS[2]])
            k.op("dve", lambda v: v.tensor_tensor(S[3][:, :], S[3][:, :], S[1][:, :], ALU.mult), reads=[tS[1]], writes=[tS[3]])
            k.op("dve", lambda v: v.tensor_tensor(S[3][:, :], S[3][:, :], S[2][:, :], ALU.mult), reads=[tS[2]], writes=[tS[3]])
            if d == 0:
                k.op("dve", lambda v: v.tensor_tensor_scan(hdst[:, :], S[4][:, :], S[3][:, :], 0.0, ALU.mult, ALU.add),
                     reads=[tS[4], tS[3]], writes=[t_h])
            else:
                k.op("dve", lambda v: v.tensor_tensor_scan(rev(hdst, 0, NCTX), rev(S[4], 0, NCTX), rev(S[3], 0, NCTX), 0.0, ALU.mult, ALU.add),
                     reads=[tS[4], tS[3]], writes=[t_h])
                k.op("dve", lambda v: v.tensor_tensor_scan(rev(hdst, NCTX, T), rev(S[4], NCTX, T), rev(S[3], NCTX, T), hdst[:, 0:1],
                                                           ALU.mult, ALU.add),
                     reads=[tS[4], tS[3]], writes=[t_h])
        k.op("dve", lambda v: v.tensor_tensor(S[5][:, :], S[5][:, :], S[6][:, :], ALU.add), reads=[tS[6]], writes=[tS[5]])
        emit_gelu(k, S[7], tS[7], S[8], tS[8])
        k.op("dve", lambda v: v.tensor_tensor(S[5][:, :], S[5][:, :], S[8][:, :], ALU.mult), reads=[tS[8]], writes=[tS[5]])
        k.dma("sp", [(yT_out[n * 128:(n + 1) * 128, :], S[5][:, :])], tS[5], reads=[tS[5]], acc=[t_yout])


def emit_gelu(k, x, t_x, o, t_o):
    k.op("act", lambda a: a.activation(o[:, :], x[:, :], AF.Square), reads=[t_x], writes=[t_o])
    k.op("dve", lambda v: v.tensor_scalar(o[:, :], o[:, :], 0.044715, 1.0, ALU.mult, ALU.add), writes=[t_o])
    k.op("dve", lambda v: v.tensor_tensor(o[:, :], o[:, :], x[:, :], ALU.mult), reads=[t_x], writes=[t_o])
    k.op("act", lambda a: a.activation(o[:, :], o[:, :], AF.Tanh, scale=0.7978845608028654), writes=[t_o])
    k.op("dve", lambda v: v.tensor_scalar(o[:, :], o[:, :], 1.0, 0.5, ALU.add, ALU.mult), writes=[t_o])
    k.op("dve", lambda v: v.tensor_tensor(o[:, :], o[:, :], x[:, :], ALU.mult), reads=[t_x], writes=[t_o])


G8 = 8
CH = 128
YB = 32


def token_shift(k, src, t_src, dst, t_dst, onem_mu, half_mu, t_prm, rows=128):
    k.op("dve", lambda v: v.tensor_scalar(dst[:rows, :], src[:rows, :], onem_mu, None, ALU.mult), reads=[t_src, t_prm], writes=[t_dst])
    for off in (-1, 1):
        for (lo, hi) in SEGS:
            o_lo, o_hi = max(lo, lo - off), min(hi, hi - off)
            k.op("dve", lambda v: v.scalar_tensor_tensor(dst[:rows, o_lo:o_hi], src[:rows, o_lo + off:o_hi + off], half_mu,
                                                         dst[:rows, o_lo:o_hi], ALU.mult, ALU.add),
                 reads=[t_src], writes=[t_dst])


def rev_copy(k, src, t_src, dst, t_dst):
    for (lo, hi) in SEGS:
        k.op("act", lambda a: a.activation(dst[:, lo:hi], rev(src, lo, hi), AF.Copy), reads=[t_src], writes=[t_dst])


def emit_rwkv(k, P, PS, tPS, cst, t_cst, rkvT, loT, prm, t_prm, mulo, t_mulo, w2, a2, g2, t_wts, yT_out, t_yout, nsteps=T):
    S, tS = P.t, P.k
    BO = cst[:, 0:128]
    SI = cst[:, 128:192]
    HM = cst[:, 192:194]
    SC = {q: k.dram("sc_" + q, [G8, 128, T], F32).ap() for q in "RWKVAB"}
    tSC = {q: k.tok() for q in "RWKVAB"}
    LOA = k.dram("loa", [6, 128, T], F32).ap(); t_loa = k.tok()
    BON = k.dram("bon", [4, 128, T], F32).ap(); t_bon = k.tok()
    GS = k.dram("gs", [4, 128, T], F32).ap(); t_gs = k.tok()
    YSC = k.dram("ysc", [G8, 2, 64, T], F32).ap(); t_ysc = k.tok()
    dp = k.sb([128, 4, 8], F32); t_dp = k.tok()
    k.op("dve", lambda v: v.tensor_scalar(dp[:, :, 0:3], prm[:, :, 0:3], -1.0, 1.0, ALU.mult, ALU.add), reads=[t_prm], writes=[t_dp])
    k.op("dve", lambda v: v.tensor_scalar(dp[:, :, 3:6], prm[:, :, 0:3], 0.5, None, ALU.mult), reads=[t_prm], writes=[t_dp])
    k.op("dve", lambda v: v.tensor_scalar(dp[:, :, 6:7], prm[:, :, 4:5], -1.0, 1.0, ALU.mult, ALU.add), reads=[t_prm], writes=[t_dp])
    dl = k.sb([128, 2, 6], F32); t_dl = k.tok()
    k.op("dve", lambda v: v.tensor_scalar(dl[:, 0, :], mulo[:, :], -1.0, 1.0, ALU.mult, ALU.add), reads=[t_mulo], writes=[t_dl])
    k.op("dve", lambda v: v.tensor_scalar(dl[:, 1, :], mulo[:, :], 0.5, None, ALU.mult), reads=[t_mulo], writes=[t_dl])

    for c in range(6):
        rows = 96 if c == 5 else 128
        k.dma("sp", [(S[0][:rows, :], loT[c * 128:c * 128 + rows, :])], tS[0], writes=[tS[0]])
        token_shift(k, S[0], tS[0], S[1], tS[1], dl[:rows, 0, c:c + 1], dl[:rows, 1, c:c + 1], t_dl, rows=rows)
        if c == 0:
            k.op("act", lambda a: a.activation(S[1][:rows, :], S[1][:rows, :], AF.Tanh), writes=[tS[1]])
        elif c >= 2:
            k.op("act", lambda a: a.activation(S[1][:rows, :], S[1][:rows, :], AF.Sigmoid), writes=[tS[1]])
        k.dma("sp", [(LOA[c, :rows, :], S[1][:rows, :])], tS[1], reads=[tS[1]], acc=[t_loa])

    lb = [k.sb([128, 6, 512], F32) for _ in range(2)]; t_lb = k.toks(2)
    nlb = 0
    npsA = 0

    def bo_apply(src, t_src, fn):
        nonlocal npsA
        for (t0, nn) in TBLK:
            pb = npsA % 2
            npsA += 1
            k.op("pe", lambda p: p.matmul(PS[pb][:, :nn], BO, src[:, t0:t0 + nn], start=True, stop=True),
                 reads=[t_src, t_cst], writes=[tPS[pb]])
            fn(t0, nn, PS[pb], tPS[pb])

    def store_scan(q, g, src, t_src):
        d = g // 4
        if d == 0:
            k.dma("sp", [(SC[q][g, :, :], src[:, :])], t_src, reads=[t_src], acc=[tSC[q]])
        else:
            rev_copy(k, src, t_src, S[2], tS[2])
            k.dma("sp", [(SC[q][g, :, :], S[2][:, :])], tS[2], reads=[tS[2]], acc=[tSC[q]])

    for p in range(4):
        pr = prm[:, p, :]
        for qi, dst in ((0, 3), (1, 4), (2, 5)):
            k.dma("sp", [(S[0][:, :], rkvT[qi, p * 128:(p + 1) * 128, :])], tS[0], writes=[tS[0]])
            token_shift(k, S[0], tS[0], S[dst], tS[dst], dp[:, p, qi:qi + 1], dp[:, p, 3 + qi:4 + qi], t_dp)
        k.op("dve", lambda v: v.tensor_scalar(S[6][:, :], S[4][:, :], pr[:, 3:4], None, ALU.mult), reads=[tS[4], t_prm], writes=[tS[6]])
        k.op("act", lambda a: a.activation(S[7][:, :], S[6][:, :], AF.Square), reads=[tS[6]], writes=[tS[7]])

        def kk_fn(t0, nn, ps, t_ps):
            k.op("act", lambda a: a.activation(S[8][:, t0:t0 + nn], ps[:, :nn], AF.Sqrt), reads=[t_ps], writes=[tS[8]])
        bo_apply(S[7], tS[7], kk_fn)
        k.op("dve", lambda v: v.tensor_scalar(S[8][:, :], S[8][:, :], 1e-12, None, ALU.max), writes=[tS[8]])
        k.op("dve", lambda v: v.reciprocal(S[8][:, :], S[8][:, :]), writes=[tS[8]])
        k.op("dve", lambda v: v.tensor_tensor(S[6][:, :], S[6][:, :], S[8][:, :], ALU.mult), reads=[tS[8]], writes=[tS[6]])
        k.op("dve", lambda v: v.tensor_scalar(S[7][:, :], S[6][:, :], -1.0, None, ALU.mult), reads=[tS[6]], writes=[tS[7]])
        for d in range(2):
            store_scan("A", d * 4 + p, S[7], tS[7])
            store_scan("R", d * 4 + p, S[3], tS[3])
            store_scan("V", d * 4 + p, S[5], tS[5])
        for d in range(2):
            for (t0, nn) in TBLK:
                s_ = nlb % 2
                nlb += 1
                k.dma("sp", [(lb[s_][:, :, :nn], LOA[:, :, t0:t0 + nn].rearrange("c p t -> p c t"))], t_lb[s_], reads=[t_loa], writes=[t_lb[s_]])
                if d == 0:
                    pb = npsA % 2; npsA += 1
                    for c in range(4):
                        rows = 96 if c == 3 else 128
                        k.op("pe", lambda pe, c=c, rows=rows: pe.matmul(PS[pb][:, :nn], g2[:rows, c, p * 128:(p + 1) * 128], lb[s_][:rows, 2 + c, :nn],
                                                                        start=(c == 0), stop=(c == 3)),
                             reads=[t_lb[s_], t_wts] if c == 0 else (), writes=[tPS[pb]] if c == 0 else (), acc=[tPS[pb]] if c else (), inc=(c == 3))
                    k.op("act", lambda a: a.activation(S[1][:, t0:t0 + nn], PS[pb][:, :nn], AF.Copy), reads=[tPS[pb]], writes=[tS[1]])
                pb = npsA % 2; npsA += 1
                k.op("pe", lambda pe: pe.matmul(PS[pb][:, :nn], w2[:, d, p * 128:(p + 1) * 128], lb[s_][:, 0, :nn], start=True, stop=True),
                     reads=[t_lb[s_], t_wts], writes=[tPS[pb]])
                k.op("act", lambda a: a.activation(S[7][:, t0:t0 + nn], PS[pb][:, :nn], AF.Sigmoid, bias=pr[:, 8 + d:9 + d]),
                     reads=[tPS[pb], t_prm], writes=[tS[7]])
                pb = npsA % 2; npsA += 1
                k.op("pe", lambda pe: pe.matmul(PS[pb][:, :nn], a2[:, d, p * 128:(p + 1) * 128], lb[s_][:, 1, :nn], start=True, stop=True),
                     reads=[t_lb[s_], t_wts], writes=[tPS[pb]])
                k.op("act", lambda a: a.activation(S[8][:, t0:t0 + nn], PS[pb][:, :nn], AF.Sigmoid, bias=pr[:, 10 + d:11 + d]),
                     reads=[tPS[pb], t_prm], writes=[tS[8]])
            if d == 0:
                k.dma("sp", [(GS[p, :, :], S[1][:, :])], tS[1], reads=[tS[1]], acc=[t_gs])
            k.op("act", lambda a: a.activation(S[7][:, :], S[7][:, :], AF.Exp, scale=-0.6065306597126334), writes=[tS[7]])
            store_scan("W", d * 4 + p, S[7], tS[7])
            k.op("dve", lambda v: v.tensor_tensor(S[7][:, :], S[6][:, :], S[8][:, :], ALU.mult), reads=[tS[6], tS[8]], writes=[tS[7]])
            store_scan("B", d * 4 + p, S[7], tS[7])
            k.op("dve", lambda v: v.tensor_scalar(S[8][:, :], S[8][:, :], pr[:, 4:5], dp[:, p, 6:7], ALU.mult, ALU.add), reads=[t_prm, t_dp], writes=[tS[8]])
            k.op("dve", lambda v: v.tensor_tensor(S[8][:, :], S[8][:, :], S[4][:, :], ALU.mult), reads=[tS[4]], writes=[tS[8]])
            store_scan("K", d * 4 + p, S[8], tS[8])
            if d == 0:
                k.op("dve", lambda v: v.tensor_copy(S[0][:, :], S[8][:, :]), reads=[tS[8]], writes=[tS[0]])
            else:
                k.op("dve", lambda v: v.tensor_tensor(S[0][:, :], S[0][:, :], S[8][:, :], ALU.add), reads=[tS[8]], writes=[tS[0]])
        k.op("dve", lambda v: v.tensor_tensor(S[0][:, :], S[0][:, :], S[3][:, :], ALU.mult), reads=[tS[3]], writes=[tS[0]])
        k.op("dve", lambda v: v.tensor_scalar(S[0][:, :], S[0][:, :], pr[:, 5:6], None, ALU.mult), reads=[t_prm], writes=[tS[0]])

        def bon_fn(t0, nn, ps, t_ps):
            k.op("dve", lambda v: v.tensor_tensor(S[7][:, t0:t0 + nn], ps[:, :nn], S[5][:, t0:t0 + nn], ALU.mult), reads=[t_ps, tS[5]], writes=[tS[7]])
        bo_apply(S[0], tS[0], bon_fn)
        k.dma("sp", [(BON[p, :, :], S[7][:, :])], tS[7], reads=[tS[7]], acc=[t_bon])

    def carve(i, off, a, b, rows=128):
        return S[i][:rows, off:off + a * b].rearrange("p (a b) -> p a b", a=a)

    def inherit(i):
        t = k.tok()
        t.w = dict(tS[i].w); t.r = dict(tS[i].r)
        return t
    carved = []

    def ctok(i):
        t = inherit(i)
        carved.append((i, t))
        return t
    chunk = {}
    t_chunk = {}
    for qi, q in enumerate("RWKVAB"):
        base = qi // 2
        o = (qi % 2) * 2048
        chunk[q] = [carve(base, o, G8, CH), carve(base, o + 1024, G8, CH)]
        t_chunk[q] = [ctok(base), ctok(base)]
    NR = 4
    Abd = [carve(3, r * 1024, G8, 128) for r in range(NR)]; t_Abd = [ctok(3) for _ in range(NR)]
    Vsi = [carve(4, r * 512, G8, 64) for r in range(NR)]; t_Vsi = [ctok(4) for _ in range(NR)]
    tmp1 = carve(4, 2048, G8, 64); tmp2 = carve(4, 2560, G8, 64); t_t1 = ctok(4); t_t2 = ctok(4)
    H = carve(4, 3072, G8, 64); t_H = ctok(4)
    Rm = [carve(4, 3584 + r * 16, G8, 2) for r in range(NR)]; t_Rm = [ctok(4) for _ in range(NR)]
    ysb = [carve(5, r * 2048, G8 * 2, CH, rows=64) for r in range(2)]; t_ysb = [ctok(5), ctok(5)]
    k.op("dve", lambda v: v.memset(H, 0.0), writes=[t_H])
    sa_ps, t_sa = PS[2], tPS[2]
    vb_ps, t_vb = [PS[3], PS[4]], [tPS[3], tPS[4]]
    y_ps, t_yp = [PS[5], PS[6]], [tPS[5], tPS[6]]
    nchunks = (nsteps + CH - 1) // CH
    for ci in range(nchunks):
        s0 = ci * CH
        cb = ci % 2
        for q in "RWKVAB":
            k.dma("sp", [(chunk[q][cb][:, :, :], SC[q][:, :, s0:s0 + CH].rearrange("g p s -> p g s"))], t_chunk[q][cb],
                  reads=[tSC[q]], writes=[t_chunk[q][cb]])
        Rc, Wc, Kc, Vc, Ac, Bc = (chunk[q][cb] for q in "RWKVAB")
        tR, tW, tK, tV, tA, tB = (t_chunk[q][cb] for q in "RWKVAB")
        for c in range(CH):
            s = s0 + c
            if s >= nsteps:
                break
            r_ = s % NR
            k.op("pool", lambda g: g.tensor_tensor(Abd[r_][:, :, :], BO.unsqueeze(1).to_broadcast([128, G8, 128]),
                                                   Ac[:, :, c:c + 1].to_broadcast([128, G8, 128]), ALU.mult),
                 reads=[tA, t_cst], writes=[t_Abd[r_]])
            k.op("pool", lambda g: g.tensor_tensor(Vsi[r_][:, :, :], SI.unsqueeze(1).to_broadcast([128, G8, 64]),
                                                   Vc[:, :, c:c + 1].to_broadcast([128, G8, 64]), ALU.mult),
                 reads=[tV, t_cst], writes=[t_Vsi[r_]])
            k.op("pool", lambda g: g.tensor_tensor(Rm[r_][:, :, :], HM.unsqueeze(1).to_broadcast([128, G8, 2]),
                                                   Rc[:, :, c:c + 1].to_broadcast([128, G8, 2]), ALU.mult),
                 reads=[tR, t_cst], writes=[t_Rm[r_]])
            vb = s % 2
            for g in range(G8):
                k.op("pe", lambda pe, g=g: pe.matmul(vb_ps[vb][:, g * 64:(g + 1) * 64], BO, Vsi[r_][:, g, :], start=True, stop=True),
                     reads=[t_Vsi[r_], t_cst] if g == 0 else (), writes=[t_vb[vb]] if g == 0 else (), acc=[t_vb[vb]] if g else (), inc=(g == G8 - 1))
            for g in range(G8):
                k.op("pe", lambda pe, g=g: pe.matmul(sa_ps[:, g * 64:(g + 1) * 64], Abd[r_][:, g, :], H[:, g, :], start=True, stop=True),
                     reads=[t_Abd[r_], t_H] if g == 0 else (), writes=[t_sa] if g == 0 else (), acc=[t_sa] if g else (), inc=(g == G8 - 1))
            H3 = H
            k.op("dve", lambda v: v.tensor_tensor(tmp1[:, :, :], sa_ps[:, :].rearrange("p (g i) -> p g i", g=G8),
                                                  Bc[:, :, c:c + 1].to_broadcast([128, G8, 64]), ALU.mult),
                 reads=[t_sa, tB], writes=[t_t1])
            k.op("dve", lambda v: v.tensor_tensor(H3, H3, Wc[:, :, c:c + 1].to_broadcast([128, G8, 64]), ALU.mult), reads=[tW], writes=[t_H])
            k.op("dve", lambda v: v.tensor_tensor(H3, H3, tmp1[:, :, :], ALU.add), reads=[t_t1], writes=[t_H])
            k.op("dve", lambda v: v.tensor_tensor(tmp2[:, :, :], vb_ps[vb][:, :].rearrange("p (g i) -> p g i", g=G8),
                                                  Kc[:, :, c:c + 1].to_broadcast([128, G8, 64]), ALU.mult),
                 reads=[t_vb[vb], tK], writes=[t_t2])
            k.op("dve", lambda v: v.tensor_tensor(H3, H3, tmp2[:, :, :], ALU.add), reads=[t_t2], writes=[t_H])
            yb = (s // YB) % 2
            col = s % YB
            for g in range(G8):
                first = (col == 0 and g == 0)
                k.op("pe", lambda pe, g=g: pe.matmul(y_ps[yb][0:64, (col * G8 + g) * 2:(col * G8 + g) * 2 + 2], H[:, g, :], Rm[r_][:, g, :],
                                                     start=True, stop=True),
                     reads=[t_H, t_Rm[r_]] if g == 0 else (), writes=[t_yp[yb]] if first else (), acc=[] if first else [t_yp[yb]], inc=(g == G8 - 1))
            if col == YB - 1 or s == nsteps - 1:
                c0 = c - col
                nst = col + 1
                k.op("act", lambda a: a.activation(ysb[cb][:, :, c0:c0 + nst].rearrange("p gh s -> p s gh"),
                                                   y_ps[yb][0:64, 0:nst * 16].rearrange("p (s gh) -> p s gh", gh=16), AF.Copy),
                     reads=[t_yp[yb]], acc=[t_ysb[cb]])
        nst_c = min(CH, nsteps - s0)
        k.dma("sp", [(YSC[:, :, :, s0:s0 + nst_c].rearrange("g h i s -> i (g h) s"), ysb[cb][:, :, :nst_c])], t_ysb[cb],
              reads=[t_ysb[cb]], acc=[t_ysc])

    for (i, t) in carved:
        K._merge(tS[i].r, t.w)
        K._merge(tS[i].r, t.r)
    for p in range(4):
        pr = prm[:, p, :]
        k.dma("sp", [(S[0][:, :], YSC[p].rearrange("h i s -> (h i) s"))], tS[0], reads=[t_ysc], writes=[tS[0]])
        k.dma("sp", [(S[1][:, :], YSC[4 + p].rearrange("h i s -> (h i) s"))], tS[1], reads=[t_ysc], writes=[tS[1]])
        for (lo, hi) in SEGS:
            k.op("dve", lambda v: v.tensor_tensor(S[0][:, lo:hi], S[0][:, lo:hi], rev(S[1], lo, hi), ALU.add), reads=[tS[1]], writes=[tS[0]])

        def mean_fn(t0, nn, ps, t_ps):
            k.op("dve", lambda v: v.scalar_tensor_tensor(S[2][:, t0:t0 + nn], ps[:, :nn], -1.0 / 64, S[0][:, t0:t0 + nn], ALU.mult, ALU.add),
                 reads=[t_ps, tS[0]], writes=[tS[2]])
        bo_apply(S[0], tS[0], mean_fn)
        k.op("act", lambda a: a.activation(S[3][:, :], S[2][:, :], AF.Square), reads=[tS[2]], writes=[tS[3]])

        def var_fn(t0, nn, ps, t_ps):
            k.op("act", lambda a: a.activation(S[4][:, t0:t0 + nn], ps[:, :nn], AF.Sqrt, bias=64e-5, scale=1.0 / 64), reads=[t_ps], writes=[tS[4]])
        bo_apply(S[3], tS[3], var_fn)
        k.op("dve", lambda v: v.reciprocal(S[4][:, :], S[4][:, :]), writes=[tS[4]])
        k.op("dve", lambda v: v.tensor_tensor(S[2][:, :], S[2][:, :], S[4][:, :], ALU.mult), reads=[tS[4]], writes=[tS[2]])
        k.op("dve", lambda v: v.tensor_scalar(S[2][:, :], S[2][:, :], pr[:, 6:7], pr[:, 7:8], ALU.mult, ALU.add), reads=[t_prm], writes=[tS[2]])
        k.dma("sp", [(S[5][:, :], BON[p])], tS[5], reads=[t_bon], writes=[tS[5]])
        k.dma("sp", [(S[6][:, :], GS[p])], tS[6], reads=[t_gs], writes=[tS[6]])
        k.op("dve", lambda v: v.tensor_tensor(S[2][:, :], S[2][:, :], S[5][:, :], ALU.add), reads=[tS[5]], writes=[tS[2]])
        k.op("dve", lambda v: v.tensor_tensor(S[2][:, :], S[2][:, :], S[6][:, :], ALU.mult), reads=[tS[6]], writes=[tS[2]])
        k.dma("sp", [(yT_out[512 + p * 128:512 + (p + 1) * 128, :], S[2][:, :])], tS[2], reads=[tS[2]], acc=[t_yout])


def build_even_mixer(nsteps=T, do_lru=True):
    k = K()
    xaT = k.inp("xaT", [512, T])
    gateT = k.inp("gateT", [512, T])
    rkvT = k.inp("rkvT", [3, 512, T])
    loT = k.inp("loT", [736, T])
    lprm_d = k.inp("lprm", [128, 4, 16])
    gw_d = k.inp("gw", [128, 4, 4, 128])
    rprm_d = k.inp("rprm", [128, 4, 16])
    mulo_d = k.inp("mulo", [128, 6])
    w2_d = k.inp("w2", [128, 2, 512])
    a2_d = k.inp("a2", [128, 2, 512])
    g2_d = k.inp("g2", [128, 4, 512])
    cst_d = k.inp("cst", [128, 194])
    yT = k.outp("yT", [1024, T])
    lprm = k.sb([128, 4, 16], F32); rprm = k.sb([128, 4, 16], F32); mulo = k.sb([128, 6], F32); t_prm = k.tok()
    k.dma("sp", [(lprm[:], lprm_d), (rprm[:], rprm_d), (mulo[:], mulo_d)], t_prm, writes=[t_prm])
    gw = k.sb([128, 4, 4, 128], F32); w2 = k.sb([128, 2, 512], F32); a2 = k.sb([128, 2, 512], F32); g2 = k.sb([128, 4, 512], F32)
    cst = k.sb([128, 194], F32); t_w = k.tok()
    k.dma("sp", [(gw[:], gw_d), (w2[:], w2_d), (a2[:], a2_d), (g2[:], g2_d), (cst[:], cst_d)], t_w, writes=[t_w])
    P = Pool9(k)
    PS = [k.ps([128, 512], F32) for _ in range(8)]; tPS = k.toks(8)
    t_y = k.tok()
    if do_lru:
        emit_lru(k, P, PS, tPS, lprm, t_prm, gw, t_w, xaT, gateT, yT, t_y)
    emit_rwkv(k, P, PS, tPS, cst, t_w, rkvT, loT, rprm, t_prm, mulo, t_prm, w2, a2, g2, t_w, yT, t_y, nsteps=nsteps)
    k.finish([t_y])
    return k


def even_consts():
    c = np.zeros((128, 194), np.float32)
    pi = np.arange(128)
    c[:, 0:128] = (pi[:, None] // 64 == pi[None, :] // 64)
    c[:, 128:192] = (pi[:, None] % 64 == np.arange(64)[None, :])
    c[:, 192:194] = (pi[:, None] // 64 == np.arange(2)[None, :])
    return c


def rwkv_core_params(q, mu, w0, w2, a0, a2, g2, k_k, k_a, r_k, ln_w, ln_b):
    prm = np.zeros((128, 4, 16), np.float32)
    rk = r_k.reshape(-1)
    for p in range(4):
        ch = slice(512 * q + 128 * p, 512 * q + 128 * (p + 1))
        for qi in range(3):
            prm[:, p, qi] = mu[2048 * qi:2048 * (qi + 1)][ch]
        prm[:, p, 3] = k_k[ch]; prm[:, p, 4] = k_a[ch]; prm[:, p, 5] = rk[ch]
        prm[:, p, 6] = ln_w[ch]; prm[:, p, 7] = ln_b[ch]
        for d in range(2):
            prm[:, p, 8 + d] = w0[d, ch]; prm[:, p, 10 + d] = a0[d, ch]
    mulo = np.zeros((128, 6), np.float32)
    ml = mu[6144:]
    for c in range(6):
        rows = 96 if c == 5 else 128
        mulo[:rows, c] = ml[c * 128:c * 128 + rows]
    cs = slice(512 * q, 512 * q + 512)
    w2c = np.ascontiguousarray(w2[:, :, cs].transpose(1, 0, 2))
    a2c = np.ascontiguousarray(a2[:, :, cs].transpose(1, 0, 2))
    g2p = np.zeros((512, 512), np.float32); g2p[:480] = g2[:, cs]
    g2c = np.ascontiguousarray(g2p.reshape(4, 128, 512).transpose(1, 0, 2))
    return prm, mulo, w2c, a2c, g2c


NCHUNK = 68
GQ = 4


def build_gla(nchunks=NCHUNK):
    k = K()
    qT = k.inp("qT", [4, 256, T])
    kT = k.inp("kT", [4, 256, T])
    ktok = k.inp("ktok", [4, T, 256])
    vtok = k.inp("vtok", [4, T, 512])
    gklo = k.inp("gklo", [4, 16, T])
    gkw_d = k.inp("gkw", [16, 4, 256])
    gkb_d = k.inp("gkb", [1, 4, 256])
    gate = k.inp("gate", [2, T, 512])
    nw_d = k.inp("nw", [128, 512])
    cst_d = k.inp("cst", [128, 65 + 64 + 128])
    y = k.outp("y", [2, T, 512])
    O = k.dram("osc", [4, T, 512], F32).ap(); t_O = k.tok()

    gkw = k.sb([16, 4, 256], F32); gkb = k.sb([1, 4, 256], F32); nw = k.sb([128, 512], F32); cst = k.sb([128, 257], F32)
    t_c = k.tok()
    k.dma("sp", [(gkw[:], gkw_d), (gkb[:], gkb_d), (nw[:], nw_d), (cst[:], cst_d)], t_c, writes=[t_c])
    LT = cst[0:64, 0:65]
    ONES = cst[0:64, 65:129]
    J = cst[:, 129:257]
    ones1 = cst[0:1, 65:129]

    PSB = [k.ps([128, 512], F32) for _ in range(7)]
    tP = k.toks(7)
    ser = k.toks(7)
    qg = [k.sb([128, 2, GQ * 64], F32) for _ in range(2)]
    kg = [k.sb([128, 2, GQ * 64], F32) for _ in range(2)]
    ktg = [k.sb([64, GQ, 256], F32) for _ in range(2)]
    vtg = [k.sb([64, GQ, 512], F32) for _ in range(2)]
    glg = [k.sb([16, GQ * 64], F32) for _ in range(2)]
    t_in = k.toks(2)
    la = k.sb([64, 256], F32); t_la = k.tok()
    cumt = k.sb([64, 256], F32); t_cumt = k.tok()
    e1 = k.sb([64, 256], F32); t_e1 = k.tok()
    kend = k.sb([64, 256], F32); t_kend = k.tok()
    eT = k.sb([128, 2, 64], F32); einvT = k.sb([128, 2, 64], F32); t_eT = k.tok(); t_einv = k.tok()
    qdec = k.sb([128, 2, 64], F32); kinv = k.sb([128, 2, 64], F32); t_qdec = k.tok(); t_kinv = k.tok()
    dec = k.sb([128, 2], F32); t_dec = k.tok()
    scT = k.sb([64, 64], F32); t_sc = k.tok()
    osb = [k.sb([64, 512], F32) for _ in range(2)]; t_osb = k.toks(2)
    Sst = k.sb([128, 2, 512], F32); t_S = k.tok()
    ng = 0
    for dh in range(4):
        k.op("dve", lambda v: v.memset(Sst[:], 0.0), writes=[t_S])
        for gi in range((nchunks + GQ - 1) // GQ):
            b = ng % 2
            ng += 1
            g0 = gi * GQ * 64
            gl = min(GQ, nchunks - gi * GQ) * 64
            k.dma("sp", [(qg[b][:, :, :gl], qT[dh, :, g0:g0 + gl].rearrange("(c p) t -> p c t", p=128)),
                         (kg[b][:, :, :gl], kT[dh, :, g0:g0 + gl].rearrange("(c p) t -> p c t", p=128)),
                         (ktg[b][:, :gl // 64, :], ktok[dh, g0:g0 + gl, :].rearrange("(c j) k -> j c k", j=64)),
                         (vtg[b][:, :gl // 64, :], vtok[dh, g0:g0 + gl, :].rearrange("(c j) k -> j c k", j=64)),
                         (glg[b][:, :gl], gklo[dh, :, g0:g0 + gl])], t_in[b], writes=[t_in[b]])
            for ci in range(gl // 64):
                n = gi * GQ + ci
                c0 = n * 64
                lo = ci * 64
                k.op("pe", lambda p: p.matmul(PSB[0][0:64, 0:256], glg[b][:, lo:lo + 64], gkw[:, dh, :], start=True, stop=False),
                     reads=[t_in[b], t_c], writes=[tP[0]], inc=False)
                k.op("pe", lambda p: p.matmul(PSB[0][0:64, 0:256], ones1, gkb[:, dh, :], start=False, stop=True), reads=[t_c], acc=[tP[0]])
                k.op("act", lambda a: a.activation(la[:, :], PSB[0][0:64, 0:256], AF.Sigmoid), reads=[tP[0]], writes=[t_la, ser[0]])
                k.op("act", lambda a: a.activation(la[:, :], la[:, :], AF.Ln), writes=[t_la])
                k.op("dve", lambda v: v.tensor_scalar(la[:, :], la[:, :], 1.0 / 16, -1.0, ALU.mult, ALU.max), writes=[t_la])
                k.op("pe", lambda p: p.matmul(PSB[1][0:64, 0:256], LT[:, 0:64], la[:, :], start=True, stop=True),
                     reads=[t_la, t_c], writes=[tP[1]], inc=False)
                k.op("pe", lambda p: p.matmul(PSB[1][0:64, 256:512], ONES, la[:, :], start=True, stop=True), reads=[t_la], acc=[tP[1]])
                for c in range(2):
                    k.op("pe", lambda p, c=c: p.matmul(PSB[2][:, c * 128:c * 128 + 65], la[:, c * 128:(c + 1) * 128], LT, start=True, stop=True),
                         reads=[t_la, t_c], writes=[tP[2]] if c == 0 else (), acc=[tP[2]] if c else (), inc=(c == 1))
                k.op("act", lambda a: a.activation(cumt[:, :], PSB[1][0:64, 0:256], AF.Copy), reads=[tP[1]], writes=[t_cumt, ser[1]])
                k.op("dve", lambda v: v.tensor_tensor(e1[:, :], PSB[1][0:64, 256:512], cumt[:, :], ALU.subtract),
                     reads=[tP[1], t_cumt], writes=[t_e1, ser[1]])
                k.op("act", lambda a: a.activation(e1[:, :], e1[:, :], AF.Exp), writes=[t_e1])
                k.op("dve", lambda v: v.tensor_tensor(kend[:, :], ktg[b][:, ci, :], e1[:, :], ALU.mult), reads=[t_in[b], t_e1], writes=[t_kend])
                cview = PSB[2][:, 0:256].rearrange("p (c x) -> p c x", c=2)
                k.op("act", lambda a: a.activation(eT[:, :, :], cview[:, :, 0:64], AF.Exp), reads=[tP[2]], writes=[t_eT, ser[2]])
                k.op("act", lambda a: a.activation(einvT[:, :, :], cview[:, :, 0:64], AF.Exp, scale=-1.0), reads=[tP[2]], writes=[t_einv, ser[2]])
                k.op("act", lambda a: a.activation(dec[:, :], cview[:, :, 64], AF.Exp), reads=[tP[2]], writes=[t_dec, ser[2]])
                k.op("dve", lambda v: v.scalar_tensor_tensor(qdec[:, :, :], qg[b][:, :, lo:lo + 64], 0.0625, eT[:, :, :], ALU.mult, ALU.mult),
                     reads=[t_in[b], t_eT], writes=[t_qdec])
                k.op("dve", lambda v: v.tensor_tensor(kinv[:, :, :], kg[b][:, :, lo:lo + 64], einvT[:, :, :], ALU.mult),
                     reads=[t_in[b], t_einv], writes=[t_kinv])
                for c in range(2):
                    k.op("pe", lambda p, c=c: p.matmul(PSB[3][0:64, 0:64], kinv[:, c, :], qdec[:, c, :], start=(c == 0), stop=(c == 1)),
                         reads=[t_kinv, t_qdec] if c == 0 else (), writes=[tP[3]] if c == 0 else (), acc=[tP[3]] if c else (), inc=(c == 1))
                k.op("dve", lambda v: v.tensor_tensor(scT[:, :], PSB[3][0:64, 0:64], LT[:, 0:64], ALU.mult), reads=[tP[3], t_c], writes=[t_sc, ser[3]])
                k.op("pe", lambda p: p.matmul(PSB[4][0:64, :], scT[:, :], vtg[b][:, ci, :], start=True, stop=False),
                     reads=[t_sc, t_in[b]], writes=[tP[4]], inc=False)
                for c in range(2):
                    k.op("pe", lambda p, c=c: p.matmul(PSB[4][0:64, :], qdec[:, c, :], Sst[:, c, :], start=False, stop=(c == 1)),
                         reads=[t_qdec, t_S] if c == 0 else (), acc=[tP[4]], inc=(c == 1))
                ob = n % 2
                k.op("act", lambda a: a.activation(osb[ob][:, :], PSB[4][0:64, :], AF.Copy), reads=[tP[4]], writes=[t_osb[ob], ser[4]])
                k.dma("sp", [(O[dh, c0:c0 + 64, :], osb[ob][:, :])], t_osb[ob], reads=[t_osb[ob]], acc=[t_O])
                for c in range(2):
                    k.op("pe", lambda p, c=c: p.matmul(PSB[5 + c][:, :], kend[:, c * 128:(c + 1) * 128], vtg[b][:, ci, :], start=True, stop=True),
                         reads=[t_kend, t_in[b]], writes=[tP[5 + c]])
                    k.op("dve", lambda v, c=c: v.scalar_tensor_tensor(Sst[:, c, :], Sst[:, c, :], dec[:, c:c + 1], PSB[5 + c][:, :], ALU.mult, ALU.add),
                         reads=[tP[5 + c], t_dec], writes=[t_S, ser[5 + c]])
    o0 = [k.sb([128, 512], F32) for _ in range(2)]; o1 = [k.sb([128, 512], F32) for _ in range(2)]; gt = [k.sb([128, 512], F32) for _ in range(2)]
    t_o0 = k.toks(2); t_o1 = k.toks(2); t_gt = k.toks(2)
    junk = k.sb([128, 512], F32); t_junk = k.tok()
    st = k.sb([128, 4], F32); t_st = k.tok()
    t_y = k.tok()
    nn_ = 0
    ntile = (nchunks * 64) // 128
    for hl in range(2):
        for m in range(ntile):
            b = nn_ % 2
            nn_ += 1
            r0 = m * 128
            mir = (1 - m) if m < 2 else (2 + (31 - (m - 2)))
            k.dma("sp", [(o0[b][:, :], O[hl, r0:r0 + 128, :])], t_o0[b], reads=[t_O], writes=[t_o0[b]])
            k.dma("sp", [(o1[b][:, :], O[2 + hl, mir * 128:mir * 128 + 128, :])], t_o1[b], reads=[t_O], writes=[t_o1[b]])
            k.dma("sp", [(gt[b][:, :], gate[hl, r0:r0 + 128, :])], t_gt[b], writes=[t_gt[b]])
            k.op("pe", lambda p: p.matmul(PSB[0][:, :], J, o1[b][:, :], start=True, stop=True), reads=[t_o1[b], t_c], writes=[tP[0]])
            k.op("dve", lambda v: v.tensor_tensor(o0[b][:, :], o0[b][:, :], PSB[0][:, :], ALU.add), reads=[tP[0]], writes=[t_o0[b], ser[0]])
            k.op("act", lambda a: a.activation(junk[:, :], o0[b][:, :], AF.Square, accum_out=st[:, 0:1]), reads=[t_o0[b]], writes=[t_junk, t_st])
            k.op("act", lambda a: a.activation(st[:, 1:2], st[:, 0:1], AF.Sqrt, bias=1e-5, scale=1.0 / 512), writes=[t_st])
            k.op("dve", lambda v: v.reciprocal(st[:, 2:3], st[:, 1:2]), writes=[t_st])
            k.op("dve", lambda v: v.scalar_tensor_tensor(o0[b][:, :], o0[b][:, :], st[:, 2:3], nw[:, :], ALU.mult, ALU.mult),
                 reads=[t_st, t_c], writes=[t_o0[b]])
            k.op("act", lambda a: a.activation(gt[b][:, :], gt[b][:, :], AF.Silu), writes=[t_gt[b]])
            k.op("dve", lambda v: v.tensor_tensor(o0[b][:, :], o0[b][:, :], gt[b][:, :], ALU.mult), reads=[t_gt[b]], writes=[t_o0[b]])
            k.dma("sp", [(y[hl, r0:r0 + 128, :], o0[b][:, :])], t_o0[b], reads=[t_o0[b]], acc=[t_y])
    k.finish([t_y])
    return k


def gla_consts():
    c = np.zeros((128, 257), np.float32)
    j = np.arange(64)
    c[0:64, 0:64] = (j[:, None] <= j[None, :])
    c[0:64, 64] = 1.0
    c[0:64, 65:129] = 1.0
    c[:, 129:257] = np.eye(128, dtype=np.float32)[::-1]
    return c


def seg_rev_np(z, axis=0):
    idx = np.concatenate([np.arange(NCTX - 1, -1, -1), np.arange(TSEQ - 1, NCTX - 1, -1)])
    return np.take(z, idx, axis=axis)


def build_lru_only():
    k = K()
    xaT = k.inp("xaT", [512, T])
    gateT = k.inp("gateT", [512, T])
    prm_d = k.inp("prm", [128, 4, 16])
    gw_d = k.inp("gw", [128, 4, 4, 128])
    yT = k.outp("yT", [512, T])
    prm = k.sb([128, 4, 16], F32); t_prm = k.tok()
    gw = k.sb([128, 4, 4, 128], F32); t_gw = k.tok()
    k.dma("sp", [(prm[:], prm_d)], t_prm, writes=[t_prm])
    k.dma("sp", [(gw[:], gw_d)], t_gw, writes=[t_gw])
    P = Pool9(k)
    PS = [k.ps([128, 512], F32) for _ in range(8)]; tPS = k.toks(8)
    t_y = k.tok()
    emit_lru(k, P, PS, tPS, prm, t_prm, gw, t_gw, xaT, gateT, yT, t_y)
    k.finish([t_y])
    return k


def lru_core_params(q, conv_w, conv_b, ga_w, ga_b, gx_w, gx_b, lam):
    prm = np.zeros((128, 4, 16), np.float32)
    gw = np.zeros((128, 4, 4, 128), np.float32)
    for n in range(4):
        ch = slice((4 * q + n) * 128, (4 * q + n + 1) * 128)
        prm[:, n, 0:4] = conv_w[:, ch].T
        prm[:, n, 4] = conv_b[ch]
        for d in range(2):
            prm[:, n, 5 + 2 * d] = ga_b[d, ch]
            prm[:, n, 6 + 2 * d] = gx_b[d, ch]
            prm[:, n, 9 + d] = lam[d, ch]
            gw[:, n, 2 * d, :] = ga_w[d, 4 * q + n]
            gw[:, n, 2 * d + 1, :] = gx_w[d, 4 * q + n]
    return prm, gw


def fm_vec(v):
    return np.ascontiguousarray(np.asarray(v, np.float32).reshape(KC, 128).T)


def token_shards(x, ctx):
    out = []
    for b in range(2):
        seq = np.concatenate([ctx[b], x[b]], axis=0)
        for q in range(4):
            out.append(np.ascontiguousarray(seq[q * NTOK:(q + 1) * NTOK]))
    return out


def run_mods(c, c_ctx, ada_w, ada_b):
    cond = np.stack([c[0], c[1], c_ctx], 0).astype(np.float32)
    condT = np.ascontiguousarray(cond.T.reshape(KC, 128, 3).transpose(1, 0, 2))
    ins = []
    for core in range(8):
        sl = slice(core * 3072, (core + 1) * 3072)
        ins.append({"condT": condT, "aw": np.ascontiguousarray(ada_w[:, :, sl]),
                    "ab": np.ascontiguousarray(np.broadcast_to(ada_b[:, None, sl], (2, 3, 3072)))})
    res = run_bass_kernel_spmd(build_mods().nc, ins, core_ids=list(range(8)))
    return np.concatenate([res.results[core]["mod"] for core in range(8)], axis=2)


def inproj_vecs(mod_l, gain, b, q):
    lat = mod_l[b]
    ctxm = mod_l[2]
    first = ctxm if q == 0 else lat
    vec = np.zeros((128, 8, KC), np.float32)
    vec[:, 0] = fm_vec(gain)
    vec[:, 1] = fm_vec(first[D:2 * D]); vec[:, 2] = fm_vec(first[0:D])
    vec[:, 3] = fm_vec(lat[D:2 * D]); vec[:, 4] = fm_vec(lat[0:D])
    return vec


GU_ORDER = np.concatenate([np.concatenate([np.arange(cc * 128, cc * 128 + 128), np.arange(512 + cc * 128, 512 + cc * 128 + 128)])
                           for cc in range(4)])


def moe_core_inputs(core, router_w_l, router_b_l, w_gu_l, b_gu_l, w_down_l, b_down_l):
    own = list(range(NEXP * core, NEXP * core + NEXP))
    perm = own + [e for e in range(32) if e not in own]
    rw = np.ascontiguousarray(router_w_l[:, perm])
    rb = np.ascontiguousarray(np.broadcast_to(router_b_l[perm][None, :], (128, 32)))
    wgu = np.ascontiguousarray(w_gu_l[own][:, :, GU_ORDER])
    bg = b_gu_l[own][:, GU_ORDER].reshape(NEXP, 8, 128)
    bgu = np.ascontiguousarray(bg.transpose(2, 0, 1))
    return {"rw": rw, "rb": rb, "wgu": wgu, "bgu": bgu, "wd": np.ascontiguousarray(w_down_l[own]),
            "bd": np.ascontiguousarray(b_down_l[own])}


def moe_vecs(mod_l, gain, b):
    vec = np.zeros((128, 8, KC), np.float32)
    vec[:, 0] = fm_vec(gain)
    for ci, row in enumerate([2, b]):
        vec[:, 1 + 2 * ci] = fm_vec(mod_l[row][4 * D:5 * D])
        vec[:, 2 + 2 * ci] = fm_vec(mod_l[row][3 * D:4 * D])
    return vec


def build_combine_inproj(ncols, nparts=4):
    k = K()
    parts = k.inp("parts", [nparts, NTOK, D])
    x1 = k.inp("x1", [NTOK, D])
    g_d = k.inp("g5", [128, 2, D])
    vec_d = k.inp("vec", [128, 8, KC])
    W = k.inp("w", [D, ncols])
    x2 = k.outp("x2", [NTOK, D])
    outT = k.outp("projT", [ncols, NTOK])
    g5 = k.sb([128, 2, D], F32); t_g = k.tok()
    k.dma("sp", [(g5[:, c, :], g_d[:, c, :]) for c in range(2)], t_g, writes=[t_g])
    t_x2 = k.tok()
    vec = k.sb([128, 8, KC], F32); t_vec = k.tok()
    AB = k.sb([128, 4, KC], F32); t_AB = k.tok()
    k.dma("sp", [(vec[:], vec_d)], t_vec, writes=[t_vec])
    make_AB(k, vec, t_vec, AB, t_AB, 0, 1, 2, 0)
    make_AB(k, vec, t_vec, AB, t_AB, 0, 3, 4, 2)
    fb = FrontBufs(k)
    emit_combine(k, parts, x1, g5, t_g, x2, t_x2, nparts=nparts, bufs=(fb.xt, fb.t_xt))
    hT = k.sb([128, KC, NTOK], BF16)
    t_hT = k.toks(NT)
    front_end(k, fb, x2, lambda i: AB[:, 0 if i < 2 else 2, :], lambda i: AB[:, 1 if i < 2 else 3, :], t_AB, hT, t_hT, t_src=t_x2)
    gb = GemmBufs(k, nwb=2)
    t_out = k.tok()
    gemm_f(k, gb, W, ncols, hT, t_hT, outT, t_out)
    k.finish([t_out, t_x2])
    return k


def build_combine_final(nparts=4):
    k = K()
    parts = k.inp("parts", [nparts, NTOK, D])
    x1 = k.inp("x1", [NTOK, D])
    g_d = k.inp("g5", [128, 2, D])
    gn_d = k.inp("g", [128, D])
    out = k.outp("o", [NTOK, D])
    x4 = k.dram("x4", [NTOK, D], F32).ap(); t_x4 = k.tok()
    g5 = k.sb([128, 2, D], F32); t_g = k.tok()
    k.dma("sp", [(g5[:, c, :], g_d[:, c, :]) for c in range(2)], t_g, writes=[t_g])
    gn = k.sb([128, D], F32); t_gn = k.tok()
    k.dma("sp", [(gn[:], gn_d)], t_gn, writes=[t_gn])
    xt = [k.sb([128, D], F32) for _ in range(2)]; t_x = k.toks(2)
    emit_combine(k, parts, x1, g5, t_g, x4, t_x4, nparts=nparts, bufs=(xt, t_x))
    emit_final_norm(k, x4, t_x4, gn, t_gn, out, xt, t_x)
    return k


def emit_final_norm(k, x, t_src, g, t_g, out, xt, t_x):
    junk = k.sb([128, D], BF16); t_j = k.tok()
    st = k.sb([128, NT, 3], F32); t_s = k.toks(NT)
    t_out = k.tok()
    for i in range(NT):
        rows = tile_rows(i)
        s = i % 2
        r0 = i * 128
        k.dma("sp", [(xt[s][:rows, 0:2048], x[r0:r0 + rows, 0:2048]), (xt[s][:rows, 2048:4096], x[r0:r0 + rows, 2048:4096])],
              t_x[s], reads=[t_src] if t_src is not None else (), writes=[t_x[s]])
        k.op("act", lambda a: a.activation(junk[:rows, :], xt[s][:rows, :], AF.Square, accum_out=st[:rows, i, 0:1]),
             reads=[t_x[s]], writes=[t_j, t_s[i]])
        k.op("act", lambda a: a.activation(st[:rows, i, 1:2], st[:rows, i, 0:1], AF.Sqrt, bias=EPS, scale=1.0 / D),
             reads=[t_s[i]], writes=[t_s[i]])
        k.op("dve", lambda v: v.reciprocal(st[:rows, i, 2:3], st[:rows, i, 1:2]), reads=[t_s[i]], writes=[t_s[i]])
        k.op("act", lambda a: a.activation(xt[s][:rows, :], xt[s][:rows, :], AF.Copy, scale=st[:rows, i, 2:3]),
             reads=[t_s[i]], writes=[t_x[s]])
        k.op("dve", lambda v: v.tensor_tensor(xt[s][:rows, :], xt[s][:rows, :], g[:rows, :], ALU.mult),
             reads=[t_g], writes=[t_x[s]])
        k.dma("sp", [(out[r0:r0 + rows, :], xt[s][:rows, :])], t_x[s], reads=[t_x[s]], acc=[t_out])
    k.finish([t_out])


CORES = list(range(8))
_PROG = {}


def prog(name, builder, *a):
    return builder(*a).nc


def launch(nc, ins):
    return run_bass_kernel_spmd(nc, ins, core_ids=CORES).results


def rep128(v):
    return np.ascontiguousarray(np.broadcast_to(np.asarray(v, np.float32)[None], (128,) + tuple(np.shape(v))))


def gate_rows(mod_l, b, q, chunk):
    lat = mod_l[b][chunk * D:(chunk + 1) * D]
    first = mod_l[2][chunk * D:(chunk + 1) * D] if q == 0 else lat
    return rep128(np.stack([first, lat], 0))


def col_major_index():
    r = np.arange(64)
    perm = (r[None, :] * 64 + np.arange(64)[:, None]).reshape(-1)
    return np.concatenate([np.arange(NCTX), NCTX + perm])


def assemble_seq(per_core, axis):
    return [np.concatenate([per_core[b * 4 + q] for q in range(4)], axis=axis) for b in range(2)]


def run_moe_layer(xs, mod_l, gain, rw, rb, wgu, bgu, wdn, bdn):
    seq = assemble_seq(xs, 0)
    eg_in = [moe_core_inputs(eg, rw, rb, wgu, bgu, wdn, bdn) for eg in range(4)]
    ins = []
    for b in range(2):
        vec = moe_vecs(mod_l, gain, b)
        for eg in range(4):
            d = dict(eg_in[eg])
            d["x1"] = seq[b]
            d["vec"] = vec
            ins.append(d)
    res = launch(prog("moe", build_moe), ins)
    parts = []
    for b in range(2):
        for q in range(4):
            parts.append(np.ascontiguousarray(np.stack([res[b * 4 + eg]["ypart"][q * NTOK:(q + 1) * NTOK] for eg in range(4)], 0)))
    return parts


def kernel(x, c, ctx, c_ctx, ada_w, ada_b, norm_mix, norm_ffn, norm_final,
           ev_w_in, ev_w_out, lru_conv_w, lru_conv_b, lru_gate_a_w, lru_gate_a_b,
           lru_gate_x_w, lru_gate_x_b, lru_lambda,
           rwkv_mu, rwkv_w0, rwkv_w2, rwkv_a0, rwkv_a2, rwkv_g2, rwkv_k_k, rwkv_k_a,
           rwkv_r_k, rwkv_ln_w, rwkv_ln_b,
           od_w_in, od_w_out, gla_gk_w2, gla_gk_b, gla_norm_w,
           router_w, router_b, exp_w_gu, exp_b_gu, exp_w_down, exp_b_down):
    A = lambda z: np.asarray(z, np.float32)
    x, ctx = A(x), A(ctx)
    norm_mix, norm_ffn = A(norm_mix), A(norm_ffn)
    mod = run_mods(A(c), A(c_ctx), A(ada_w), A(ada_b))
    xs = token_shards(x, ctx)

    w_in = np.ascontiguousarray(A(ev_w_in)[0])
    res = launch(prog("inproj", build_inproj, EVEN_IN),
                 [{"x": xs[cc], "vec": inproj_vecs(mod[0], norm_mix[0], cc // 4, cc % 4), "w": w_in} for cc in CORES])
    PT = assemble_seq([r["projT"] for r in res], 1)
    del res
    cst = even_consts()
    ins = []
    for b in range(2):
        for q in range(4):
            cs = slice(512 * q, 512 * q + 512)
            lprm, gw = lru_core_params(q, A(lru_conv_w)[0], A(lru_conv_b)[0], A(lru_gate_a_w)[0], A(lru_gate_a_b)[0],
                                       A(lru_gate_x_w)[0], A(lru_gate_x_b)[0], A(lru_lambda)[0])
            rprm, mulo, w2c, a2c, g2c = rwkv_core_params(q, A(rwkv_mu)[0], A(rwkv_w0)[0], A(rwkv_w2)[0], A(rwkv_a0)[0], A(rwkv_a2)[0],
                                                         A(rwkv_g2)[0], A(rwkv_k_k)[0], A(rwkv_k_a)[0], A(rwkv_r_k)[0],
                                                         A(rwkv_ln_w)[0], A(rwkv_ln_b)[0])
            P_ = PT[b]
            rkvT = np.ascontiguousarray(np.stack([P_[4096 + 2048 * i:4096 + 2048 * (i + 1)][cs] for i in range(3)], 0))
            ins.append({"gateT": np.ascontiguousarray(P_[0:2048][cs]), "xaT": np.ascontiguousarray(P_[2048:4096][cs]), "rkvT": rkvT,
                        "loT": np.ascontiguousarray(P_[10240:10976]), "lprm": lprm, "gw": gw, "rprm": rprm, "mulo": mulo,
                        "w2": w2c, "a2": a2c, "g2": g2c, "cst": cst})
    del PT
    res = launch(prog("even", build_even_mixer), ins)
    del ins
    YT = []
    for b in range(2):
        yt = np.empty((D, TSEQ), np.float32)
        for q in range(4):
            r = res[b * 4 + q]["yT"]
            yt[512 * q:512 * q + 512] = r[0:512]
            yt[2048 + 512 * q:2048 + 512 * q + 512] = r[512:1024]
        YT.append(yt)
    del res
    w_out = np.ascontiguousarray(A(ev_w_out)[0])
    res = launch(prog("outproj", build_outproj),
                 [{"yT": np.ascontiguousarray(YT[cc // 4][:, (cc % 4) * NTOK:(cc % 4 + 1) * NTOK]), "x": xs[cc],
                   "g2": gate_rows(mod[0], cc // 4, cc % 4, 2), "w": w_out} for cc in CORES])
    x1 = [r["x1"] for r in res]
    del res, YT
    parts = run_moe_layer(x1, mod[0], norm_ffn[0], A(router_w)[0], A(router_b)[0], A(exp_w_gu)[0], A(exp_b_gu)[0],
                          A(exp_w_down)[0], A(exp_b_down)[0])

    w_in1 = np.ascontiguousarray(A(od_w_in)[0])
    res = launch(prog("cinproj", build_combine_inproj, ODD_IN),
                 [{"parts": parts[cc], "x1": x1[cc], "g5": gate_rows(mod[0], cc // 4, cc % 4, 5),
                   "vec": inproj_vecs(mod[1], norm_mix[1], cc // 4, cc % 4), "w": w_in1} for cc in CORES])
    del parts
    x2 = [r["x2"] for r in res]
    PT = assemble_seq([r["projT"] for r in res], 1)
    del res
    cm = col_major_index()
    gcst = gla_consts()
    nw = rep128(A(gla_norm_w)[0])
    gk_w2, gk_b = A(gla_gk_w2)[0], A(gla_gk_b)[0]
    ins = []
    for b in range(2):
        Pp = PT[b][:, cm]
        for hp in range(4):
            d = {k_: [] for k_ in ("qT", "kT", "ktok", "vtok", "gklo")}
            gkw = np.zeros((16, 4, 256), np.float32)
            gkb = np.zeros((1, 4, 256), np.float32)
            for dd in range(2):
                for hl in range(2):
                    h = 2 * hp + hl
                    qs = Pp[h * 256:(h + 1) * 256]
                    ks = Pp[2048 + h * 256:2048 + (h + 1) * 256]
                    vs = Pp[4096 + h * 512:4096 + (h + 1) * 512]
                    gl = Pp[12288 + dd * 16:12288 + (dd + 1) * 16]
                    if dd == 1:
                        qs, ks, vs, gl = (seg_rev_np(z, axis=1) for z in (qs, ks, vs, gl))
                    d["qT"].append(qs); d["kT"].append(ks); d["ktok"].append(ks.T); d["vtok"].append(vs.T); d["gklo"].append(gl)
                    gkw[:, dd * 2 + hl, :] = gk_w2[dd][:, h * 256:(h + 1) * 256]
                    gkb[0, dd * 2 + hl, :] = gk_b[dd][h * 256:(h + 1) * 256]
            o = {k_: np.ascontiguousarray(np.stack(v_, 0)) for k_, v_ in d.items()}
            o["gkw"] = gkw; o["gkb"] = gkb
            o["gate"] = np.ascontiguousarray(np.stack([Pp[8192 + (2 * hp + hl) * 512:8192 + (2 * hp + hl + 1) * 512].T for hl in range(2)], 0))
            o["nw"] = nw; o["cst"] = gcst
            ins.append(o)
    del PT
    res = launch(prog("gla", build_gla), ins)
    del ins
    YT = []
    for b in range(2):
        yp = np.concatenate([res[b * 4 + hp]["y"][hl] for hp in range(4) for hl in range(2)], axis=1)
        yn = np.empty_like(yp)
        yn[cm] = yp
        YT.append(np.ascontiguousarray(yn.T))
    del res
    w_out1 = np.ascontiguousarray(A(od_w_out)[0])
    res = launch(prog("outproj", build_outproj),
                 [{"yT": np.ascontiguousarray(YT[cc // 4][:, (cc % 4) * NTOK:(cc % 4 + 1) * NTOK]), "x": x2[cc],
                   "g2": gate_rows(mod[1], cc // 4, cc % 4, 2), "w": w_out1} for cc in CORES])
    x3 = [r["x1"] for r in res]
    del res, YT
    parts = run_moe_layer(x3, mod[1], norm_ffn[1], A(router_w)[1], A(router_b)[1], A(exp_w_gu)[1], A(exp_b_gu)[1],
                          A(exp_w_down)[1], A(exp_b_down)[1])
    gfin = rep128(A(norm_final))
    res = launch(prog("cfinal", build_combine_final),
                 [{"parts": parts[cc], "x1": x3[cc], "g5": gate_rows(mod[1], cc // 4, cc % 4, 5), "g": gfin} for cc in CORES])
    out = np.zeros((2, 4096, D), np.float32)
    for b in range(2):
        seq = np.concatenate([res[b * 4 + q]["o"] for q in range(4)], axis=0)
        out[b] = seq[NCTX:]
    return out
```

```python
import numpy as np
import concourse.bass as bass
import concourse.mybir as mybir
from concourse.bass_utils import run_bass_kernel_spmd

F32 = mybir.dt.float32
BF16 = mybir.dt.bfloat16
I32 = mybir.dt.int32
U32 = mybir.dt.uint32
AF = mybir.ActivationFunctionType
ALU = mybir.AluOpType
AX = mybir.AxisListType

D = 4096
KC = 32
NTOK = 1088
NT = 9
TSEQ = 4352
NCTX = 256
EVEN_IN = 10976
ODD_IN = 12320
EPS = 1e-6


class Tok:
    __slots__ = ("name", "w", "r", "chan", "ccount")

    def __init__(self, name=""):
        self.name = name
        self.w = {}
        self.r = {}
        self.chan = None
        self.ccount = 0


class K:
    def __init__(self):
        self.nc = bass.Bass("TRN2", target_bir_lowering=False)
        nc = self.nc
        self.eng = {"pe": nc.tensor, "dve": nc.vector, "act": nc.scalar, "pool": nc.gpsimd, "sp": nc.sync}
        self.sem = {e: nc.alloc_semaphore("c_" + e) for e in self.eng}
        self.cnt = {e: 0 for e in self.eng}
        self.waited = {}
        self.pending = {}
        self.ninst = 0
        self._uid = 0

    def uid(self, p):
        self._uid += 1
        return f"{p}{self._uid}"

    def sb(self, shape, dt, name=None):
        return self.nc.alloc_sbuf_tensor(name or self.uid("sb"), list(shape), dt)

    def ps(self, shape, dt=F32, name=None):
        return self.nc.alloc_psum_tensor(name or self.uid("ps"), list(shape), dt)

    def dram(self, name, shape, dt, kind="Internal"):
        return self.nc.dram_tensor(name, list(shape), dt, kind=kind)

    def inp(self, name, shape, dt=F32):
        return self.dram(name, shape, dt, kind="ExternalInput").ap()

    def outp(self, name, shape, dt=F32):
        return self.dram(name, shape, dt, kind="ExternalOutput").ap()

    def tok(self, name=""):
        return Tok(name)

    def toks(self, n):
        return [Tok() for _ in range(n)]

    def _wait(self, e, ev):
        for sid, (sem, val) in ev.items():
            if e == "pe" and sem is self.sem["pe"]:
                continue
            key = (e, sid)
            if self.waited.get(key, 0) >= val:
                continue
            self.waited[key] = val
            self.eng[e].wait_ge(sem, val)
            self.ninst += 1

    @staticmethod
    def _merge(d, ev):
        for sid, (sem, val) in ev.items():
            if sid not in d or d[sid][1] < val:
                d[sid] = (sem, val)

    def _deps(self, e, reads, writes, acc):
        for t in reads:
            self._wait(e, t.w)
        for t in writes:
            self._wait(e, t.w)
            self._wait(e, t.r)
        for t in acc:
            self._wait(e, t.r)

    def _commit(self, ev, reads, writes, acc):
        for t in reads:
            self._merge(t.r, ev)
        for t in writes:
            t.w = dict(ev)
            t.r = {}
        for t in acc:
            self._merge(t.w, ev)
            t.r = {}

    def op(self, e, fn, reads=(), writes=(), acc=(), inc=True):
        self._deps(e, reads, writes, acc)
        ins = fn(self.eng[e])
        self.ninst += 1
        pend = self.pending.setdefault(e, [])
        pend.append((tuple(reads), tuple(writes), tuple(acc)))
        if not inc:
            return None
        self.cnt[e] += 1
        ins.then_inc(self.sem[e], 1)
        ev = {id(self.sem[e]): (self.sem[e], self.cnt[e])}
        for (r, w, a) in pend:
            self._commit(ev, r, w, a)
        self.pending[e] = []
        return ev

    def dma(self, e, pairs, owner, reads=(), writes=(), acc=(), **kw):
        if owner.chan is None:
            owner.chan = self.nc.alloc_semaphore(self.uid("d"))
        self._deps(e, reads, writes, acc)
        for (o, i) in pairs:
            self.eng[e].dma_start(out=o, in_=i, **kw).then_inc(owner.chan, 16)
            owner.ccount += 16
            self.ninst += 1
        ev = {id(owner.chan): (owner.chan, owner.ccount)}
        self._commit(ev, reads, writes, acc)
        return ev

    def finish(self, toks, e="sp"):
        for t in toks:
            self._wait(e, t.w)
            self._wait(e, t.r)


def make_ident(k, dt=F32):
    idf = k.sb([128, 128], F32)
    t = k.tok()
    k.op("pool", lambda g: g.memset(idf[:], 0.0), writes=[t])
    k.op("pool", lambda g: g.affine_select(out=idf[:], in_=idf[:], compare_op=ALU.not_equal, fill=1.0,
                                            base=0, pattern=[[-1, 128]], channel_multiplier=1),
         reads=[t], writes=[t])
    if dt == F32:
        return idf, t
    idb = k.sb([128, 128], dt)
    t2 = k.tok()
    k.op("dve", lambda v: v.tensor_copy(idb[:], idf[:]), reads=[t], writes=[t2])
    return idb, t2


def tile_rows(i):
    return 128 if i < 8 else 64


def build_mods():
    k = K()
    condT = k.inp("condT", [128, KC, 3])
    aw = k.inp("aw", [2, D, 3072])
    ab = k.inp("ab", [2, 3, 3072])
    out = k.outp("mod", [2, 3, 3072])
    ct = k.sb([128, KC, 3], F32); t_c = k.tok()
    sc = k.sb([128, KC, 3], F32); t_s = k.tok()
    abt = k.sb([3, 2, 3072], F32); t_ab = k.tok()
    ot = k.sb([3, 2, 3072], F32); t_o = k.tok()
    k.dma("sp", [(ct[:], condT)], t_c, writes=[t_c])
    k.dma("sp", [(abt[:, l, :], ab[l]) for l in range(2)], t_ab, writes=[t_ab])
    k.op("act", lambda a: a.activation(sc[:], ct[:], AF.Silu), reads=[t_c], writes=[t_s])
    wb = [k.sb([128, KC, 512], F32) for _ in range(2)]
    t_w = k.toks(2)
    pss = [k.ps([3, 512], F32) for _ in range(2)]
    t_p = k.toks(2)
    it = 0
    for l in range(2):
        for nb in range(6):
            s = it % 2
            src = aw[l, :, nb * 512:(nb + 1) * 512].rearrange("(kc p) n -> p kc n", p=128)
            k.dma("sp", [(wb[s][:, 0:16, :], src[:, 0:16, :]), (wb[s][:, 16:32, :], src[:, 16:32, :])],
                  t_w[s], writes=[t_w[s]])
            for kc in range(KC):
                k.op("pe", lambda p, kc=kc, s=s: p.matmul(pss[s][:], sc[:, kc, :], wb[s][:, kc, :],
                                                           start=(kc == 0), stop=(kc == KC - 1)),
                     reads=[t_s, t_w[s]], writes=[t_p[s]] if kc == 0 else (), acc=[t_p[s]] if kc else ())
            k.op("dve", lambda v, s=s, l=l, nb=nb: v.tensor_tensor(ot[:, l, nb * 512:(nb + 1) * 512], pss[s][:],
                                                                   abt[:, l, nb * 512:(nb + 1) * 512], ALU.add),
                 reads=[t_p[s], t_ab], acc=[t_o])
            it += 1
    t_out = k.tok()
    k.dma("sp", [(out[l], ot[:, l, :]) for l in range(2)], t_o, reads=[t_o], writes=[t_out])
    k.finish([t_out])
    return k


class FrontBufs:
    def __init__(self, k, nbuf=2):
        self.nbuf = nbuf
        self.xt = [k.sb([128, D], F32) for _ in range(nbuf)]
        self.t_xt = k.toks(nbuf)
        self.junk = k.sb([128, D], BF16)
        self.t_junk = k.tok()
        self.ss = k.sb([128, NT], F32)
        self.den = k.sb([128, NT], F32)
        self.rstd = k.sb([128, NT], F32)
        self.t_ss = k.toks(NT)
        self.tp = [k.ps([128, 4, 128], F32) for _ in range(2)]
        self.t_tp = k.toks(2)
        self.t_ser = k.toks(2)
        self.ident, self.t_id = make_ident(k, F32)
        self.n = 0


def front_end(k, fb, x_src, A_of, B_of, t_vec, hT, t_hT, f32T_cb=None, tiles=None, rows_of=tile_rows, tile_done=None, t_src=None):
    for i in (tiles if tiles is not None else range(NT)):
        rows = rows_of(i)
        s = fb.n % fb.nbuf
        fb.n += 1
        xt = fb.xt[s]
        t_x = fb.t_xt[s]
        k.dma("sp", [(xt[:rows, 0:2048], x_src[i * 128:i * 128 + rows, 0:2048]),
                     (xt[:rows, 2048:4096], x_src[i * 128:i * 128 + rows, 2048:4096])], t_x,
              reads=[t_src] if t_src is not None else (), writes=[t_x])
        k.op("act", lambda a: a.activation(fb.junk[:rows, :], xt[:rows, :], AF.Square, accum_out=fb.ss[:rows, i:i + 1]),
             reads=[t_x], writes=[fb.t_junk, fb.t_ss[i]])
        k.op("act", lambda a: a.activation(fb.den[:rows, i:i + 1], fb.ss[:rows, i:i + 1], AF.Sqrt, bias=EPS, scale=1.0 / D),
             reads=[fb.t_ss[i]], writes=[fb.t_ss[i]])
        k.op("dve", lambda v: v.reciprocal(fb.rstd[:rows, i:i + 1], fb.den[:rows, i:i + 1]),
             reads=[fb.t_ss[i]], writes=[fb.t_ss[i]])
        k.op("act", lambda a: a.activation(xt[:rows, :], xt[:rows, :], AF.Copy, scale=fb.rstd[:rows, i:i + 1]),
             reads=[fb.t_ss[i]], writes=[t_x])
        A = A_of(i)
        B = B_of(i)
        for g in range(8):
            ps = fb.tp[g % 2]
            t_ps = fb.t_tp[g % 2]
            for j in range(4):
                kc = g * 4 + j
                k.op("pe", lambda p, kc=kc, j=j: p.transpose(ps[:, j, :rows], xt[:rows, kc * 128:(kc + 1) * 128],
                                                              fb.ident[:rows, :rows]),
                     reads=[t_x, fb.t_id], writes=[t_ps] if j == 0 else (), acc=[t_ps] if j else (), inc=(j == 3))
            for j in range(4):
                kc = g * 4 + j
                dst = hT[:, kc, i * 128:i * 128 + rows]
                if j % 2 == 0:
                    k.op("dve", lambda v, kc=kc, j=j, dst=dst: v.tensor_scalar(dst, ps[:, j, :rows], A[:, kc:kc + 1], B[:, kc:kc + 1],
                                                                               ALU.mult, ALU.add),
                         reads=[t_ps, t_vec], writes=[t_hT[i], fb.t_ser[g % 2]])
                else:
                    k.op("act", lambda a, kc=kc, j=j, dst=dst: a.activation(dst, ps[:, j, :rows], AF.Identity,
                                                                            bias=B[:, kc:kc + 1], scale=A[:, kc:kc + 1]),
                         reads=[t_ps, t_vec], writes=[t_hT[i], fb.t_ser[g % 2]])
                if f32T_cb is not None:
                    f32T_cb(i, kc, ps[:, j, :rows], t_ps, A, B, fb.t_ser[g % 2])
        if tile_done is not None:
            tile_done(i)


def make_AB(k, vec, t_vec, AB, t_AB, gi, sci, shi, oi):
    k.op("dve", lambda v: v.tensor_scalar(AB[:, oi, :], vec[:, sci, :], 1.0, None, ALU.add), reads=[t_vec], writes=[t_AB])
    k.op("dve", lambda v: v.tensor_tensor(AB[:, oi, :], AB[:, oi, :], vec[:, gi, :], ALU.mult), reads=[t_vec, t_AB], writes=[t_AB])
    k.op("dve", lambda v: v.tensor_copy(AB[:, oi + 1, :], vec[:, shi, :]), reads=[t_vec, t_AB], writes=[t_AB])


NBLK = [(0, 512), (512, 512), (1024, 64)]


class GemmBufs:
    def __init__(self, k, cb=256, nwb=3):
        self.cb = cb
        self.nwb = nwb
        self.wb = [k.sb([128, KC, cb], BF16) for _ in range(nwb)]
        self.t_wb = k.toks(nwb)
        self.ps = [[k.ps([128, 512], F32) for _ in range(3)] for _ in range(2)]
        self.t_ps = [k.toks(3) for _ in range(2)]
        self.osb = [k.sb([128, NTOK], F32) for _ in range(2)]
        self.t_osb = k.toks(2)
        self.nw = 0
        self.nc_ = 0


def gemm_f(k, gb, W, ncols, hT, t_hT, outT, t_out, epilogue=None):
    cb = gb.cb
    nblocks = (ncols + cb - 1) // cb
    for b in range(nblocks):
        c0 = b * cb
        cw = min(cb, ncols - c0)
        s = gb.nw % gb.nwb
        gb.nw += 1
        wb = gb.wb[s]
        t_w = gb.t_wb[s]
        src = W[:, c0:c0 + cw].rearrange("(kc p) n -> p kc n", p=128)
        k.dma("pool", [(wb[:, 0:16, :cw], src[:, 0:16, :]), (wb[:, 16:32, :cw], src[:, 16:32, :])], t_w, writes=[t_w])
        for sub in range((cw + 127) // 128):
            m0 = sub * 128
            m = min(128, cw - m0)
            pi = gb.nc_ % 2
            gb.nc_ += 1
            for nt, (t0, n) in enumerate(NBLK):
                ps = gb.ps[pi][nt]
                t_p = gb.t_ps[pi][nt]
                rd = [t_w] + [t_hT[i] for i in range(t0 // 128, (t0 + n + 127) // 128)]
                for kc in range(KC):
                    k.op("pe", lambda p, kc=kc, ps=ps, t0=t0, n=n, m0=m0, m=m: p.matmul(
                        ps[:m, :n], wb[:, kc, m0:m0 + m], hT[:, kc, t0:t0 + n], start=(kc == 0), stop=(kc == KC - 1)),
                        reads=rd if kc == 0 else [t_w], writes=[t_p] if kc == 0 else (), acc=[t_p] if kc else (),
                        inc=(kc == KC - 1))
            if epilogue is not None:
                epilogue(c0 + m0, m, gb.ps[pi], gb.t_ps[pi])
                continue
            osb = gb.osb[pi]
            t_o = gb.t_osb[pi]
            for nt, (t0, n) in enumerate(NBLK):
                ps = gb.ps[pi][nt]
                t_p = gb.t_ps[pi][nt]
                if nt == 1:
                    k.op("dve", lambda v, ps=ps, t0=t0, n=n: v.tensor_copy(osb[:m, t0:t0 + n], ps[:m, :n]),
                         reads=[t_p], writes=[t_o])
                else:
                    k.op("act", lambda a, ps=ps, t0=t0, n=n: a.activation(osb[:m, t0:t0 + n], ps[:m, :n], AF.Copy),
                         reads=[t_p], writes=[t_o])
            k.dma("sp", [(outT[c0 + m0:c0 + m0 + m, :], osb[:m, :])], t_o, reads=[t_o], acc=[t_out])


def build_inproj(ncols):
    k = K()
    x = k.inp("x", [NTOK, D])
    vec_d = k.inp("vec", [128, 8, KC])
    W = k.inp("w", [D, ncols])
    outT = k.outp("projT", [ncols, NTOK])
    vec = k.sb([128, 8, KC], F32); t_vec = k.tok()
    AB = k.sb([128, 4, KC], F32); t_AB = k.tok()
    k.dma("sp", [(vec[:], vec_d)], t_vec, writes=[t_vec])
    make_AB(k, vec, t_vec, AB, t_AB, 0, 1, 2, 0)
    make_AB(k, vec, t_vec, AB, t_AB, 0, 3, 4, 2)
    hT = k.sb([128, KC, NTOK], BF16)
    t_hT = k.toks(NT)
    fb = FrontBufs(k)
    front_end(k, fb, x, lambda i: AB[:, 0 if i < 2 else 2, :], lambda i: AB[:, 1 if i < 2 else 3, :], t_AB, hT, t_hT)
    gb = GemmBufs(k)
    t_out = k.tok()
    gemm_f(k, gb, W, ncols, hT, t_hT, outT, t_out)
    k.finish([t_out])
    return k


def build_final_norm():
    k = K()
    x = k.inp("x", [NTOK, D])
    g_d = k.inp("g", [128, D])
    out = k.outp("o", [NTOK, D])
    g = k.sb([128, D], F32); t_g = k.tok()
    k.dma("sp", [(g[:], g_d)], t_g, writes=[t_g])
    xt = [k.sb([128, D], F32) for _ in range(2)]; t_x = k.toks(2)
    junk = k.sb([128, D], BF16); t_j = k.tok()
    st = k.sb([128, NT, 3], F32); t_s = k.toks(NT)
    t_out = k.tok()
    for i in range(NT):
        rows = tile_rows(i)
        s = i % 2
        r0 = i * 128
        k.dma("sp", [(xt[s][:rows, 0:2048], x[r0:r0 + rows, 0:2048]), (xt[s][:rows, 2048:4096], x[r0:r0 + rows, 2048:4096])],
              t_x[s], writes=[t_x[s]])
        k.op("act", lambda a: a.activation(junk[:rows, :], xt[s][:rows, :], AF.Square, accum_out=st[:rows, i, 0:1]),
             reads=[t_x[s]], writes=[t_j, t_s[i]])
        k.op("act", lambda a: a.activation(st[:rows, i, 1:2], st[:rows, i, 0:1], AF.Sqrt, bias=EPS, scale=1.0 / D),
             reads=[t_s[i]], writes=[t_s[i]])
        k.op("dve", lambda v: v.reciprocal(st[:rows, i, 2:3], st[:rows, i, 1:2]), reads=[t_s[i]], writes=[t_s[i]])
        k.op("act", lambda a: a.activation(xt[s][:rows, :], xt[s][:rows, :], AF.Copy, scale=st[:rows, i, 2:3]),
             reads=[t_s[i]], writes=[t_x[s]])
        k.op("dve", lambda v: v.tensor_tensor(xt[s][:rows, :], xt[s][:rows, :], g[:rows, :], ALU.mult),
             reads=[t_g], writes=[t_x[s]])
        k.dma("sp", [(out[r0:r0 + rows, :], xt[s][:rows, :])], t_x[s], reads=[t_x[s]], acc=[t_out])
    k.finish([t_out])
    return k


def build_outproj():
    k = K()
    yT_d = k.inp("yT", [D, NTOK])
    x = k.inp("x", [NTOK, D])
    g_d = k.inp("g2", [128, 2, D])
    W = k.inp("w", [D, D])
    out = k.outp("x1", [NTOK, D])
    yT = k.sb([128, KC, NTOK], BF16); t_y = k.tok()
    src = yT_d.rearrange("(kc p) t -> p kc t", p=128)
    k.dma("pool", [(yT[:, 8 * j:8 * j + 8, :], src[:, 8 * j:8 * j + 8, :]) for j in range(4)], t_y, writes=[t_y])
    g2 = k.sb([128, 2, D], F32); t_g = k.tok()
    k.dma("sp", [(g2[:, c, :], g_d[:, c, :]) for c in range(2)], t_g, writes=[t_g])
    wb = [k.sb([128, KC, 512], BF16) for _ in range(2)]; t_w = k.toks(2)
    ps = [k.ps([128, 512], F32) for _ in range(2)]; t_p = k.toks(2)
    xt = [k.sb([128, 512], F32) for _ in range(2)]; t_x = k.toks(2)
    tm = [k.sb([128, 512], F32) for _ in range(2)]; t_t = k.toks(2)
    t_out = k.tok()
    it = 0
    for cg in range(8):
        c0 = cg * 512
        s = cg % 2
        wsrc = W[:, c0:c0 + 512].rearrange("(kc p) n -> p kc n", p=128)
        k.dma("pool", [(wb[s][:, 0:16, :], wsrc[:, 0:16, :]), (wb[s][:, 16:32, :], wsrc[:, 16:32, :])], t_w[s], writes=[t_w[s]])
        for i in range(NT):
            rows = tile_rows(i)
            r0 = i * 128
            b = it % 2
            it += 1
            cls = 0 if i < 2 else 1
            k.dma("sp", [(xt[b][:rows, :], x[r0:r0 + rows, c0:c0 + 512])], t_x[b], writes=[t_x[b]])
            for kc in range(KC):
                k.op("pe", lambda p, kc=kc: p.matmul(ps[b][:rows, :], yT[:, kc, r0:r0 + rows], wb[s][:, kc, :],
                                                      start=(kc == 0), stop=(kc == KC - 1)),
                     reads=[t_y, t_w[s]] if kc == 0 else (), writes=[t_p[b]] if kc == 0 else (), acc=[t_p[b]] if kc else (),
                     inc=(kc == KC - 1))
            k.op("dve", lambda v: v.tensor_tensor(tm[b][:rows, :], ps[b][:rows, :], g2[:rows, cls, c0:c0 + 512], ALU.mult),
                 reads=[t_p[b], t_g], writes=[t_t[b]])
            k.op("dve", lambda v: v.tensor_tensor(tm[b][:rows, :], tm[b][:rows, :], xt[b][:rows, :], ALU.add),
                 reads=[t_x[b]], writes=[t_t[b]])
            k.dma("sp", [(out[r0:r0 + rows, c0:c0 + 512], tm[b][:rows, :])], t_t[b], reads=[t_t[b]], acc=[t_out])
    k.finish([t_out])
    return k


def emit_combine(k, parts, x1, g5, t_g5, out, t_out, nparts=8, bufs=None, nld=1):
    if bufs is not None:
        acc, t_a = bufs
    else:
        acc = [k.sb([128, D], F32) for _ in range(2)]; t_a = k.toks(2)
    ld = [k.sb([128, D], F32) for _ in range(nld)]; t_l = k.toks(nld)
    n = 0
    for i in range(NT):
        rows = tile_rows(i)
        r0 = i * 128
        a = i % 2
        cls = 0 if i < 2 else 1
        k.dma("sp", [(acc[a][:rows, :], parts[0, r0:r0 + rows, :])], t_a[a], writes=[t_a[a]])
        for c in range(1, nparts + 1):
            b = n % nld
            n += 1
            srcap = parts[c, r0:r0 + rows, :] if c < nparts else x1[r0:r0 + rows, :]
            k.dma("sp", [(ld[b][:rows, :], srcap)], t_l[b], writes=[t_l[b]])
            if c == nparts:
                k.op("dve", lambda v: v.tensor_tensor(acc[a][:rows, :], acc[a][:rows, :], g5[:rows, cls, :], ALU.mult),
                     reads=[t_g5], writes=[t_a[a]])
            k.op("dve", lambda v: v.tensor_tensor(acc[a][:rows, :], acc[a][:rows, :], ld[b][:rows, :], ALU.add),
                 reads=[t_l[b]], writes=[t_a[a]])
        k.dma("sp", [(out[r0:r0 + rows, :], acc[a][:rows, :])], t_a[a], reads=[t_a[a]], acc=[t_out])


NEXP = 8
MOE_BLOCKS = [(i * 512, 512) for i in range(8)] + [(4096, 256)]


def build_moe(nblocks=len(MOE_BLOCKS), stage=3):
    k = K()
    x1 = k.inp("x1", [TSEQ, D])
    vec_d = k.inp("vec", [128, 8, KC])
    rw_d = k.inp("rw", [D, 32])
    rb_d = k.inp("rb", [128, 32])
    wgu = k.inp("wgu", [NEXP, D, 1024])
    bgu_d = k.inp("bgu", [128, NEXP, 8])
    wd = k.inp("wd", [NEXP, 512, D])
    bd = k.inp("bd", [NEXP, D])
    out = k.outp("ypart", [TSEQ, D])

    vec = k.sb([128, 8, KC], F32); t_vec = k.tok()
    AB = k.sb([128, 4, KC], F32); t_AB = k.tok()
    k.dma("sp", [(vec[:], vec_d)], t_vec, writes=[t_vec])
    for c in range(2):
        make_AB(k, vec, t_vec, AB, t_AB, 0, 1 + 2 * c, 2 + 2 * c, 2 * c)
    rw = k.sb([128, KC, 32], F32); t_rw = k.tok()
    k.dma("sp", [(rw[:], rw_d.rearrange("(kc p) e -> p kc e", p=128))], t_rw, writes=[t_rw])
    rb = k.sb([128, 32], F32)
    bgu = k.sb([128, NEXP, 8], F32)
    ones = k.sb([1, 128], F32)
    t_c = k.tok()
    k.dma("sp", [(rb[:], rb_d), (bgu[:], bgu_d)], t_c, writes=[t_c])
    t_one = k.tok()
    k.op("dve", lambda v: v.memset(ones[:], 1.0), writes=[t_one])

    wgu_bf = k.dram("wgu_bf", [NEXP, 4, 128, KC, 256], BF16).ap()
    wd_bf = k.dram("wd_bf", [NEXP, 8, 128, 4, 512], BF16).ap()
    t_cgu = k.toks(NEXP)
    t_cwd = k.toks(NEXP)
    for j in range(NEXP):
        for cc in range(4):
            src = wgu[j, :, cc * 256:(cc + 1) * 256].rearrange("(kc p) n -> p kc n", p=128)
            k.dma("pool", [(wgu_bf[j, cc, :, 0:16, :], src[:, 0:16, :]), (wgu_bf[j, cc, :, 16:32, :], src[:, 16:32, :])], t_cgu[j], acc=[t_cgu[j]])
    for j in range(NEXP):
        k.dma("pool", [(wd_bf[j, cg], wd[j, :, cg * 512:(cg + 1) * 512].rearrange("(fc p) n -> p fc n", p=128)) for cg in range(8)],
              t_cwd[j], acc=[t_cwd[j]])

    hT = k.sb([128, KC, 512], BF16); t_hT = k.toks(4)
    fb = FrontBufs(k, nbuf=1)
    f32s = k.sb([128, KC, 128], F32); t_f32 = k.toks(KC)
    lg_ps = k.ps([128, 32], F32); t_lg = k.tok()
    lg = k.sb([128, 4, 32], F32)
    m8 = k.sb([128, 4, 8], F32)
    sm = k.sb([128, 4, 4], F32)
    ex = k.sb([128, 4, 32], F32)
    G = k.sb([128, 4, 32], F32)
    t_G = k.toks(4)
    wb = [k.sb([128, KC, 256], BF16) for _ in range(2)]; t_wb = k.toks(2)
    ups = [k.ps([128, 512], F32) for _ in range(2)]; t_up = k.toks(2)
    g1 = k.sb([128, 512], F32); sg = k.sb([128, 512], F32); l1 = k.sb([128, 512], F32)
    t_g1 = k.tok(); t_sg = k.tok(); t_l1 = k.tok()
    actT = k.sb([128, NEXP, 4, 512], BF16); t_act = [k.toks(4) for _ in range(NEXP)]
    wdb = [k.sb([128, 4, 4, 512], BF16) for _ in range(2)]; t_wd = k.toks(2)
    bdt = [k.sb([1, 4, 512], F32) for _ in range(2)]; t_bd = k.toks(2)
    dps = [k.ps([128, 512], F32) for _ in range(2)]; t_dp = k.toks(2)
    acc = k.sb([128, 4, 512], F32); t_acc = k.toks(4)
    t_out = k.tok()
    nwb = 0
    nwd = 0
    ndp = 0

    for blk in range(nblocks):
        tok0, bn = MOE_BLOCKS[blk]
        nti = bn // 128

        def f32_cb(i, kc, ps_ap, t_ps, A, B, t_ser):
            k.op("dve", lambda v: v.tensor_scalar(f32s[:, kc, :], ps_ap, A[:, kc:kc + 1], B[:, kc:kc + 1], ALU.mult, ALU.add),
                 reads=[t_ps, t_AB], writes=[t_f32[kc], t_ser])
            k.op("pe", lambda p: p.matmul(lg_ps[:, :], f32s[:, kc, :], rw[:, kc, :], start=(kc == 0), stop=(kc == KC - 1)),
                 reads=[t_f32[kc], t_rw], writes=[t_lg] if kc == 0 else (), acc=[t_lg] if kc else (), inc=(kc == KC - 1))

        def tile_done(i):
            tg = t_G[i]
            k.op("dve", lambda v: v.tensor_tensor(lg[:, i, :], lg_ps[:, :], rb[:, :], ALU.add), reads=[t_lg, t_c], writes=[tg])
            k.op("dve", lambda v: v.max(m8[:, i, :], lg[:, i, :]), reads=[tg], writes=[tg])
            k.op("dve", lambda v: v.tensor_scalar(sm[:, i, 0:1], m8[:, i, 0:1], -1.0, None, ALU.mult), reads=[tg], writes=[tg])
            k.op("act", lambda a: a.activation(ex[:, i, :], lg[:, i, :], AF.Exp, bias=sm[:, i, 0:1], scale=1.0), reads=[tg], writes=[tg])
            k.op("dve", lambda v: v.tensor_scalar(G[:, i, :], lg[:, i, :], m8[:, i, 3:4], None, ALU.is_ge), reads=[tg], writes=[tg])
            k.op("dve", lambda v: v.tensor_tensor(ex[:, i, :], ex[:, i, :], G[:, i, :], ALU.mult), reads=[tg], writes=[tg])
            k.op("dve", lambda v: v.reduce_sum(sm[:, i, 1:2], ex[:, i, :], axis=AX.X), reads=[tg], writes=[tg])
            k.op("dve", lambda v: v.reciprocal(sm[:, i, 2:3], sm[:, i, 1:2]), reads=[tg], writes=[tg])
            k.op("dve", lambda v: v.tensor_scalar(G[:, i, :], ex[:, i, :], sm[:, i, 2:3], None, ALU.mult), reads=[tg], writes=[tg])

        def cls(i):
            return 0 if (tok0 // 128 + i) < 2 else 1
        front_end(k, fb, x1[tok0:tok0 + bn, :], lambda i: AB[:, 2 * cls(i), :], lambda i: AB[:, 2 * cls(i) + 1, :],
                  t_AB, hT, t_hT, f32T_cb=f32_cb, tiles=range(nti), rows_of=lambda i: 128, tile_done=tile_done)
        if stage == 1:
            for i in range(nti):
                k.dma("sp", [(out[tok0 + i * 128:tok0 + i * 128 + 128, 0:32], G[:, i, :])], t_G[i], reads=[t_G[i]], acc=[t_out])
            continue
        rd_h = t_hT[:nti]
        for j in range(NEXP):
            for cc in range(4):
                s = nwb % 2
                nwb += 1
                k.dma("sp", [(wb[s][:, :, :], wgu_bf[j, cc])], t_wb[s], reads=[t_cgu[j]], writes=[t_wb[s]])
                for h in range(2):
                    for kc in range(KC):
                        k.op("pe", lambda p, kc=kc, h=h: p.matmul(ups[h][:, :bn], wb[s][:, kc, h * 128:(h + 1) * 128], hT[:, kc, :bn],
                                                                   start=(kc == 0), stop=(kc == KC - 1)),
                             reads=[t_wb[s]] + rd_h if kc == 0 else (), writes=[t_up[h]] if kc == 0 else (), acc=[t_up[h]] if kc else (),
                             inc=(kc == KC - 1))
                k.op("dve", lambda v: v.tensor_scalar(g1[:, :bn], ups[0][:, :bn], bgu[:, j, 2 * cc:2 * cc + 1], 7.0, ALU.add, ALU.min),
                     reads=[t_up[0], t_c], writes=[t_g1])
                k.op("act", lambda a: a.activation(sg[:, :bn], g1[:, :bn], AF.Sigmoid, scale=1.702), reads=[t_g1], writes=[t_sg])
                k.op("dve", lambda v: v.tensor_scalar(l1[:, :bn], ups[1][:, :bn], bgu[:, j, 2 * cc + 1:2 * cc + 2], 7.0, ALU.add, ALU.min),
                     reads=[t_up[1], t_c], writes=[t_l1])
                k.op("dve", lambda v: v.tensor_scalar(l1[:, :bn], l1[:, :bn], -7.0, 1.0, ALU.max, ALU.add), reads=[t_l1], writes=[t_l1])
                k.op("dve", lambda v: v.tensor_tensor(g1[:, :bn], g1[:, :bn], sg[:, :bn], ALU.mult), reads=[t_sg], writes=[t_g1])
                k.op("dve", lambda v: v.tensor_tensor(actT[:, j, cc, :bn], g1[:, :bn], l1[:, :bn], ALU.mult), reads=[t_g1, t_l1], writes=[t_act[j][cc]])
        for cg in range(8):
            c0 = cg * 512
            for eh in range(NEXP // 4):
                s = nwd % 2
                nwd += 1
                k.dma("sp", [(wdb[s][:, jl, :, :], wd_bf[4 * eh + jl, cg]) for jl in range(4)], t_wd[s],
                      reads=[t_cwd[4 * eh + jl] for jl in range(4)], writes=[t_wd[s]])
                k.dma("sp", [(bdt[s][:, jl, :], bd[4 * eh + jl:4 * eh + jl + 1, c0:c0 + 512]) for jl in range(4)], t_bd[s], writes=[t_bd[s]])
                for i in range(nti):
                    for jl in range(4):
                        j = 4 * eh + jl
                        d = ndp % 2
                        ndp += 1
                        for fc in range(4):
                            k.op("pe", lambda p, fc=fc: p.matmul(dps[d][:, :], actT[:, j, fc, i * 128:(i + 1) * 128], wdb[s][:, jl, fc, :],
                                                                  start=(fc == 0), stop=False),
                                 reads=[t_wd[s]] + t_act[j] if fc == 0 else (), writes=[t_dp[d]] if fc == 0 else (), acc=[t_dp[d]] if fc else (),
                                 inc=False)
                        k.op("pe", lambda p: p.matmul(dps[d][:, :], ones[:, :], bdt[s][:, jl, :], start=False, stop=True),
                             reads=[t_one, t_bd[s]], acc=[t_dp[d]])
                        if j == 0:
                            k.op("dve", lambda v: v.tensor_scalar(acc[:, i, :], dps[d][:], G[:, i, 0:1], None, ALU.mult),
                                 reads=[t_dp[d], t_G[i]], writes=[t_acc[i]])
                        else:
                            k.op("dve", lambda v: v.scalar_tensor_tensor(acc[:, i, :], dps[d][:], G[:, i, j:j + 1], acc[:, i, :], ALU.mult, ALU.add),
                                 reads=[t_dp[d], t_G[i]], writes=[t_acc[i]])
            for i in range(nti):
                r0 = tok0 + i * 128
                k.dma("sp", [(out[r0:r0 + 128, c0:c0 + 512], acc[:, i, :])], t_acc[i], reads=[t_acc[i]], acc=[t_out])
    k.finish([t_out])
    return k


T = TSEQ
SEGS = [(0, NCTX), (NCTX, TSEQ)]
TBLK = [(i * 512, min(512, TSEQ - i * 512)) for i in range(9)]


def rev(ap2d, lo, hi):
    return ap2d[:, hi - 1::-1] if lo == 0 else ap2d[:, hi - 1:lo - 1:-1]


class Pool9:
    def __init__(self, k, n=9):
        self.t = [k.sb([128, T], F32) for _ in range(n)]
        self.k = k.toks(n)


def shifted_mac(k, out, t_out, src, t_src, wcol, off, extra_reads=()):
    for (lo, hi) in SEGS:
        o_lo, o_hi = max(lo, lo - off), min(hi, hi - off)
        k.op("dve", lambda v: v.scalar_tensor_tensor(out[:, o_lo:o_hi], src[:, o_lo + off:o_hi + off], wcol, out[:, o_lo:o_hi],
                                                     ALU.mult, ALU.add),
             reads=[t_src] + list(extra_reads), writes=[t_out])


def emit_lru(k, P, PS, tPS, prm, t_prm, gw, t_gw, xaT, gateT, yT_out, t_yout):
    S, tS = P.t, P.k
    spn = k.sb([128, 4, 4], F32)
    t_sp = k.tok()
    k.op("act", lambda a: a.activation(spn[:, :, 0:2], prm[:, :, 9:11], AF.Exp, scale=-1.0), reads=[t_prm], writes=[t_sp])
    k.op("act", lambda a: a.activation(spn[:, :, 0:2], spn[:, :, 0:2], AF.Ln, bias=1.0), writes=[t_sp])
    k.op("dve", lambda v: v.tensor_scalar(spn[:, :, 2:4], spn[:, :, 0:2], -16.0, None, ALU.mult), writes=[t_sp])
    k.op("dve", lambda v: v.tensor_scalar(spn[:, :, 0:2], spn[:, :, 0:2], -8.0, None, ALU.mult), writes=[t_sp])
    ps = PS[0:2]
    t_ps = tPS[0:2]
    npi = 0
    for n in range(4):
        pr = prm[:, n, :]
        k.dma("sp", [(S[0][:, :], xaT[n * 128:(n + 1) * 128, :])], tS[0], writes=[tS[0]])
        k.dma("sp", [(S[7][:, :], gateT[n * 128:(n + 1) * 128, :])], tS[7], writes=[tS[7]])
        k.op("dve", lambda v: v.tensor_scalar(S[1][:, :], S[0][:, :], pr[:, 2:3], pr[:, 4:5], ALU.mult, ALU.add),
             reads=[tS[0], t_prm], writes=[tS[1]])
        shifted_mac(k, S[1], tS[1], S[0], tS[0], pr[:, 0:1], -2)
        shifted_mac(k, S[1], tS[1], S[0], tS[0], pr[:, 1:2], -1)
        shifted_mac(k, S[1], tS[1], S[0], tS[0], pr[:, 3:4], +1)
        for d in range(2):
            hdst = S[5 + d]
            t_h = tS[5 + d]
            for gi, dst in ((0, 2), (1, 3)):
                wi = d * 2 + gi
                for (t0, nn) in TBLK:
                    pb = npi % 2
                    npi += 1
                    k.op("pe", lambda p: p.matmul(ps[pb][:, :nn], gw[:, n, wi, :], S[1][:, t0:t0 + nn], start=True, stop=True),
                         reads=[tS[1], t_gw], writes=[t_ps[pb]])
                    k.op("act", lambda a: a.activation(S[dst][:, t0:t0 + nn], ps[pb][:, :nn], AF.Sigmoid, bias=pr[:, 5 + wi:6 + wi]),
                         reads=[t_ps[pb], t_prm], writes=[tS[dst]])
            k.op("act", lambda a: a.activation(S[4][:, :], S[2][:, :], AF.Exp, scale=spn[:, n, d:d + 1]), reads=[tS[2], t_sp], writes=[tS[4]])
            k.op("act", lambda a: a.activation(S[2][:, :], S[2][:, :], AF.Exp, scale=spn[:, n, 2 + d:3 + d]), reads=[t_sp], writes=[tS[2]])
            k.op("act", lambda a: a.activation(S[2][:, :], S[2][:, :], AF.Sqrt, bias=1.0, scale=-1.0), writes=[tS[2]])
            k.op("dve", lambda v: v.tensor_tensor(S[3][:, :], S[3][:, :], S[1][:, :], ALU.mult), reads=[tS[1]], writes=[tS[3]])
            k.op("dve", lambda v: v.tensor_tensor(S[3][:, :], S[3][:, :], S[2][:, :], ALU.mult), reads=[tS[2]], writes=[tS[3]])
            if d == 0:
                k.op("dve", lambda v: v.tensor_tensor_scan(hdst[:, :], S[4][:, :], S[3][:, :], 0.0, ALU.mult, ALU.add),
                     reads=[tS[4], tS[3]], writes=[t_h])
            else:
                k.op("dve", lambda v: v.tensor_tensor_scan(rev(hdst, 0, NCTX), rev(S[4], 0, NCTX), rev(S[3], 0, NCTX), 0.0, ALU.mult, ALU.add),
                     reads=[tS[4], tS[3]], writes=[t_h])
                k.op("dve", lambda v: v.tensor_tensor_scan(rev(hdst, NCTX, T), rev(S[4], NCTX, T), rev(S[3], NCTX, T), hdst[:, 0:1],
                                                           ALU.mult, ALU.add),
                     reads=[tS[4], tS[3]], writes=[t_h])
        k.op("dve", lambda v: v.tensor_tensor(S[5][:, :], S[5][:, :], S[6][:, :], ALU.add), reads=[tS[6]], writes=[tS[5]])
        emit_gelu(k, S[7], tS[7], S[8], tS[8])
        k.op("dve", lambda v: v.tensor_tensor(S[5][:, :], S[5][:, :], S[8][:, :], ALU.mult), reads=[tS[8]], writes=[tS[5]])
        k.dma("sp", [(yT_out[n * 128:(n + 1) * 128, :], S[5][:, :])], tS[5], reads=[tS[5]], acc=[t_yout])


def emit_gelu(k, x, t_x, o, t_o):
    k.op("act", lambda a: a.activation(o[:, :], x[:, :], AF.Square), reads=[t_x], writes=[t_o])
    k.op("dve", lambda v: v.tensor_scalar(o[:, :], o[:, :], 0.044715, 1.0, ALU.mult, ALU.add), writes=[t_o])
    k.op("dve", lambda v: v.tensor_tensor(o[:, :], o[:, :], x[:, :], ALU.mult), reads=[t_x], writes=[t_o])
    k.op("act", lambda a: a.activation(o[:, :], o[:, :], AF.Tanh, scale=0.7978845608028654), writes=[t_o])
    k.op("dve", lambda v: v.tensor_scalar(o[:, :], o[:, :], 1.0, 0.5, ALU.add, ALU.mult), writes=[t_o])
    k.op("dve", lambda v: v.tensor_tensor(o[:, :], o[:, :], x[:, :], ALU.mult), reads=[t_x], writes=[t_o])


G8 = 8
CH = 128
YB = 32


def token_shift(k, src, t_src, dst, t_dst, onem_mu, half_mu, t_prm, rows=128):
    k.op("dve", lambda v: v.tensor_scalar(dst[:rows, :], src[:rows, :], onem_mu, None, ALU.mult), reads=[t_src, t_prm], writes=[t_dst])
    for off in (-1, 1):
        for (lo, hi) in SEGS:
            o_lo, o_hi = max(lo, lo - off), min(hi, hi - off)
            k.op("dve", lambda v: v.scalar_tensor_tensor(dst[:rows, o_lo:o_hi], src[:rows, o_lo + off:o_hi + off], half_mu,
                                                         dst[:rows, o_lo:o_hi], ALU.mult, ALU.add),
                 reads=[t_src], writes=[t_dst])


def rev_copy(k, src, t_src, dst, t_dst):
    for (lo, hi) in SEGS:
        k.op("act", lambda a: a.activation(dst[:, lo:hi], rev(src, lo, hi), AF.Copy), reads=[t_src], writes=[t_dst])


def emit_rwkv(k, P, PS, tPS, cst, t_cst, rkvT, loT, prm, t_prm, mulo, t_mulo, w2, a2, g2, t_wts, yT_out, t_yout, nsteps=T):
    S, tS = P.t, P.k
    BO = cst[:, 0:128]
    SI = cst[:, 128:192]
    HM = cst[:, 192:194]
    SC = {q: k.dram("sc_" + q, [G8, 128, T], F32).ap() for q in "RWKVAB"}
    tSC = {q: k.tok() for q in "RWKVAB"}
    LOA = k.dram("loa", [6, 128, T], F32).ap(); t_loa = k.tok()
    BON = k.dram("bon", [4, 128, T], F32).ap(); t_bon = k.tok()
    GS = k.dram("gs", [4, 128, T], F32).ap(); t_gs = k.tok()
    YSC = k.dram("ysc", [G8, 2, 64, T], F32).ap(); t_ysc = k.tok()
    dp = k.sb([128, 4, 8], F32); t_dp = k.tok()
    k.op("dve", lambda v: v.tensor_scalar(dp[:, :, 0:3], prm[:, :, 0:3], -1.0, 1.0, ALU.mult, ALU.add), reads=[t_prm], writes=[t_dp])
    k.op("dve", lambda v: v.tensor_scalar(dp[:, :, 3:6], prm[:, :, 0:3], 0.5, None, ALU.mult), reads=[t_prm], writes=[t_dp])
    k.op("dve", lambda v: v.tensor_scalar(dp[:, :, 6:7], prm[:, :, 4:5], -1.0, 1.0, ALU.mult, ALU.add), reads=[t_prm], writes=[t_dp])
    dl = k.sb([128, 2, 6], F32); t_dl = k.tok()
    k.op("dve", lambda v: v.tensor_scalar(dl[:, 0, :], mulo[:, :], -1.0, 1.0, ALU.mult, ALU.add), reads=[t_mulo], writes=[t_dl])
    k.op("dve", lambda v: v.tensor_scalar(dl[:, 1, :], mulo[:, :], 0.5, None, ALU.mult), reads=[t_mulo], writes=[t_dl])

    for c in range(6):
        rows = 96 if c == 5 else 128
        k.dma("sp", [(S[0][:rows, :], loT[c * 128:c * 128 + rows, :])], tS[0], writes=[tS[0]])
        token_shift(k, S[0], tS[0], S[1], tS[1], dl[:rows, 0, c:c + 1], dl[:rows, 1, c:c + 1], t_dl, rows=rows)
        if c == 0:
            k.op("act", lambda a: a.activation(S[1][:rows, :], S[1][:rows, :], AF.Tanh), writes=[tS[1]])
        elif c >= 2:
            k.op("act", lambda a: a.activation(S[1][:rows, :], S[1][:rows, :], AF.Sigmoid), writes=[tS[1]])
        k.dma("sp", [(LOA[c, :rows, :], S[1][:rows, :])], tS[1], reads=[tS[1]], acc=[t_loa])

    lb = [k.sb([128, 6, 512], F32) for _ in range(2)]; t_lb = k.toks(2)
    nlb = 0
    npsA = 0

    def bo_apply(src, t_src, fn):
        nonlocal npsA
        for (t0, nn) in TBLK:
            pb = npsA % 2
            npsA += 1
            k.op("pe", lambda p: p.matmul(PS[pb][:, :nn], BO, src[:, t0:t0 + nn], start=True, stop=True),
                 reads=[t_src, t_cst], writes=[tPS[pb]])
            fn(t0, nn, PS[pb], tPS[pb])

    def store_scan(q, g, src, t_src):
        d = g // 4
        if d == 0:
            k.dma("sp", [(SC[q][g, :, :], src[:, :])], t_src, reads=[t_src], acc=[tSC[q]])
        else:
            rev_copy(k, src, t_src, S[2], tS[2])
            k.dma("sp", [(SC[q][g, :, :], S[2][:, :])], tS[2], reads=[tS[2]], acc=[tSC[q]])

    for p in range(4):
        pr = prm[:, p, :]
        for qi, dst in ((0, 3), (1, 4), (2, 5)):
            k.dma("sp", [(S[0][:, :], rkvT[qi, p * 128:(p + 1) * 128, :])], tS[0], writes=[tS[0]])
            token_shift(k, S[0], tS[0], S[dst], tS[dst], dp[:, p, qi:qi + 1], dp[:, p, 3 + qi:4 + qi], t_dp)
        k.op("dve", lambda v: v.tensor_scalar(S[6][:, :], S[4][:, :], pr[:, 3:4], None, ALU.mult), reads=[tS[4], t_prm], writes=[tS[6]])
        k.op("act", lambda a: a.activation(S[7][:, :], S[6][:, :], AF.Square), reads=[tS[6]], writes=[tS[7]])

        def kk_fn(t0, nn, ps, t_ps):
            k.op("act", lambda a: a.activation(S[8][:, t0:t0 + nn], ps[:, :nn], AF.Sqrt), reads=[t_ps], writes=[tS[8]])
        bo_apply(S[7], tS[7], kk_fn)
        k.op("dve", lambda v: v.tensor_scalar(S[8][:, :], S[8][:, :], 1e-12, None, ALU.max), writes=[tS[8]])
        k.op("dve", lambda v: v.reciprocal(S[8][:, :], S[8][:, :]), writes=[tS[8]])
        k.op("dve", lambda v: v.tensor_tensor(S[6][:, :], S[6][:, :], S[8][:, :], ALU.mult), reads=[tS[8]], writes=[tS[6]])
        k.op("dve", lambda v: v.tensor_scalar(S[7][:, :], S[6][:, :], -1.0, None, ALU.mult), reads=[tS[6]], writes=[tS[7]])
        for d in range(2):
            store_scan("A", d * 4 + p, S[7], tS[7])
            store_scan("R", d * 4 + p, S[3], tS[3])
            store_scan("V", d * 4 + p, S[5], tS[5])
        for d in range(2):
            for (t0, nn) in TBLK:
                s_ = nlb % 2
                nlb += 1
                k.dma("sp", [(lb[s_][:, :, :nn], LOA[:, :, t0:t0 + nn].rearrange("c p t -> p c t"))], t_lb[s_], reads=[t_loa], writes=[t_lb[s_]])
                if d == 0:
                    pb = npsA % 2; npsA += 1
                    for c in range(4):
                        rows = 96 if c == 3 else 128
                        k.op("pe", lambda pe, c=c, rows=rows: pe.matmul(PS[pb][:, :nn], g2[:rows, c, p * 128:(p + 1) * 128], lb[s_][:rows, 2 + c, :nn],
                                                                        start=(c == 0), stop=(c == 3)),
                             reads=[t_lb[s_], t_wts] if c == 0 else (), writes=[tPS[pb]] if c == 0 else (), acc=[tPS[pb]] if c else (), inc=(c == 3))
                    k.op("act", lambda a: a.activation(S[1][:, t0:t0 + nn], PS[pb][:, :nn], AF.Copy), reads=[tPS[pb]], writes=[tS[1]])
                pb = npsA % 2; npsA += 1
                k.op("pe", lambda pe: pe.matmul(PS[pb][:, :nn], w2[:, d, p * 128:(p + 1) * 128], lb[s_][:, 0, :nn], start=True, stop=True),
                     reads=[t_lb[s_], t_wts], writes=[tPS[pb]])
                k.op("act", lambda a: a.activation(S[7][:, t0:t0 + nn], PS[pb][:, :nn], AF.Sigmoid, bias=pr[:, 8 + d:9 + d]),
                     reads=[tPS[pb], t_prm], writes=[tS[7]])
                pb = npsA % 2; npsA += 1
                k.op("pe", lambda pe: pe.matmul(PS[pb][:, :nn], a2[:, d, p * 128:(p + 1) * 128], lb[s_][:, 1, :nn], start=True, stop=True),
                     reads=[t_lb[s_], t_wts], writes=[tPS[pb]])
                k.op("act", lambda a: a.activation(S[8][:, t0:t0 + nn], PS[pb][:, :nn], AF.Sigmoid, bias=pr[:, 10 + d:11 + d]),
                     reads=[tPS[pb], t_prm], writes=[tS[8]])
            if d == 0:
                k.dma("sp", [(GS[p, :, :], S[1][:, :])], tS[1], reads=[tS[1]], acc=[t_gs])
            k.op("act", lambda a: a.activation(S[7][:, :], S[7][:, :], AF.Exp, scale=-0.6065306597126334), writes=[tS[7]])
            store_scan("W", d * 4 + p, S[7], tS[7])
            k.op("dve", lambda v: v.tensor_tensor(S[7][:, :], S[6][:, :], S[8][:, :], ALU.mult), reads=[tS[6], tS[8]], writes=[tS[7]])
            store_scan("B", d * 4 + p, S[7], tS[7])
            k.op("dve", lambda v: v.tensor_scalar(S[8][:, :], S[8][:, :], pr[:, 4:5], dp[:, p, 6:7], ALU.mult, ALU.add), reads=[t_prm, t_dp], writes=[tS[8]])
            k.op("dve", lambda v: v.tensor_tensor(S[8][:, :], S[8][:, :], S[4][:, :], ALU.mult), reads=[tS[4]], writes=[tS[8]])
            store_scan("K", d * 4 + p, S[8], tS[8])
            if d == 0:
                k.op("dve", lambda v: v.tensor_copy(S[0][:, :], S[8][:, :]), reads=[tS[8]], writes=[tS[0]])
            else:
                k.op("dve", lambda v: v.tensor_tensor(S[0][:, :], S[0][:, :], S[8][:, :], ALU.add), reads=[tS[8]], writes=[tS[0]])
        k.op("dve", lambda v: v.tensor_tensor(S[0][:, :], S[0][:, :], S[3][:, :], ALU.mult), reads=[tS[3]], writes=[tS[0]])
        k.op("dve", lambda v: v.tensor_scalar(S[0][:, :], S[0][:, :], pr[:, 5:6], None, ALU.mult), reads=[t_prm], writes=[tS[0]])

        def bon_fn(t0, nn, ps, t_ps):
            k.op("dve", lambda v: v.tensor_tensor(S[7][:, t0:t0 + nn], ps[:, :nn], S[5][:, t0:t0 + nn], ALU.mult), reads=[t_ps, tS[5]], writes=[tS[7]])
        bo_apply(S[0], tS[0], bon_fn)
        k.dma("sp", [(BON[p, :, :], S[7][:, :])], tS[7], reads=[tS[7]], acc=[t_bon])

    def carve(i, off, a, b, rows=128):
        return S[i][:rows, off:off + a * b].rearrange("p (a b) -> p a b", a=a)

    def inherit(i):
        t = k.tok()
        t.w = dict(tS[i].w); t.r = dict(tS[i].r)
        return t
    carved = []

    def ctok(i):
        t = inherit(i)
        carved.append((i, t))
        return t
    chunk = {}
    t_chunk = {}
    for qi, q in enumerate("RWKVAB"):
        base = qi // 2
        o = (qi % 2) * 2048
        chunk[q] = [carve(base, o, G8, CH), carve(base, o + 1024, G8, CH)]
        t_chunk[q] = [ctok(base), ctok(base)]
    NR = 4

    def carve_bf(i, off_f32, a, b_):
        return S[i][:, off_f32:off_f32 + (a * b_) // 2].bitcast(BF16).rearrange("p (a b) -> p a b", a=a)
    Vsi = [carve_bf(4, r * 256, G8, 64) for r in range(NR)]; t_Vsi = [ctok(4) for _ in range(NR)]
    Rm = [carve_bf(4, 1024 + r * 8, G8, 2) for r in range(NR)]; t_Rm = [ctok(4) for _ in range(NR)]
    Hbf = [carve_bf(4, 1280 + r * 256, G8, 64) for r in range(2)]; t_Hbf = [ctok(4), ctok(4)]
    BObf = S[4][:, 2048:2112].bitcast(BF16)
    t_BObf = ctok(4)
    k.op("dve", lambda v: v.tensor_copy(BObf, BO), reads=[t_cst], writes=[t_BObf])
    Hb = [carve(6, r * 512, G8, 64) for r in range(2)]; t_Hb = [ctok(6), ctok(6)]
    HW = carve(6, 1024, G8, 64); KV = carve(6, 1536, G8, 64); tmp1 = carve(6, 2048, G8, 64); AH = carve(6, 2560, G8, 64)
    t_HW = ctok(6); t_KV = ctok(6); t_t1 = ctok(6); t_AH = ctok(6)
    ysb = [carve(5, r * 2048, G8 * 2, CH, rows=64) for r in range(2)]; t_ysb = [ctok(5), ctok(5)]
    k.op("dve", lambda v: v.memset(Hb[0], 0.0), writes=[t_Hb[0]])
    sa_ps, t_sa = PS[2], tPS[2]
    vb_ps, t_vb = [PS[3], PS[4]], [tPS[3], tPS[4]]
    y_ps, t_yp = [PS[5], PS[6]], [tPS[5], tPS[6]]
    nchunks = (nsteps + CH - 1) // CH
    chunk_of = {}

    def emit_y(s):
        cbs, c = chunk_of[s]
        r_ = s % NR
        hb = (s + 1) % 2
        yb = (s // YB) % 2
        col = s % YB
        for g in range(G8):
            first = (col == 0 and g == 0)
            k.op("pe", lambda pe, g=g: pe.matmul(y_ps[yb][0:64, (col * G8 + g) * 2:(col * G8 + g) * 2 + 2], Hbf[hb][:, g, :], Rm[r_][:, g, :],
                                                 start=True, stop=True),
                 reads=[t_Hbf[hb], t_Rm[r_]] if g == 0 else (), writes=[t_yp[yb]] if first else (), acc=[] if first else [t_yp[yb]], inc=(g == G8 - 1))
        if col == YB - 1 or s == nsteps - 1:
            c0 = c - col
            nst = col + 1
            k.op("act", lambda a: a.activation(ysb[cbs][:, :, c0:c0 + nst].rearrange("p gh s -> p s gh"),
                                               y_ps[yb][0:64, 0:nst * 16].rearrange("p (s gh) -> p s gh", gh=16), AF.Copy),
                 reads=[t_yp[yb]], acc=[t_ysb[cbs]])
        if c == CH - 1 or s == nsteps - 1:
            s0_ = s - c
            nst_c = c + 1
            k.dma("sp", [(YSC[:, :, :, s0_:s0_ + nst_c].rearrange("g h i s -> i (g h) s"), ysb[cbs][:, :, :nst_c])], t_ysb[cbs],
                  reads=[t_ysb[cbs]], acc=[t_ysc])

    for ci in range(nchunks):
        s0 = ci * CH
        cb = ci % 2
        for q in "RWKVAB":
            k.dma("sp", [(chunk[q][cb][:, :, :], SC[q][:, :, s0:s0 + CH].rearrange("g p s -> p g s"))], t_chunk[q][cb],
                  reads=[tSC[q]], writes=[t_chunk[q][cb]])
        Rc, Wc, Kc, Vc, Ac, Bc = (chunk[q][cb] for q in "RWKVAB")
        tR, tW, tK, tV, tA, tB = (t_chunk[q][cb] for q in "RWKVAB")
        for c in range(CH):
            s = s0 + c
            if s >= nsteps:
                break
            chunk_of[s] = (cb, c)
            r_ = s % NR
            Hc, t_Hc = Hb[s % 2], t_Hb[s % 2]
            Hn, t_Hn = Hb[(s + 1) % 2], t_Hb[(s + 1) % 2]
            hbn = (s + 1) % 2
            k.op("pool", lambda g: g.tensor_tensor(Vsi[r_], SI.unsqueeze(1).to_broadcast([128, G8, 64]),
                                                   Vc[:, :, c:c + 1].to_broadcast([128, G8, 64]), ALU.mult),
                 reads=[tV, t_cst], writes=[t_Vsi[r_]])
            k.op("pool", lambda g: g.tensor_tensor(Rm[r_], HM.unsqueeze(1).to_broadcast([128, G8, 2]),
                                                   Rc[:, :, c:c + 1].to_broadcast([128, G8, 2]), ALU.mult),
                 reads=[tR, t_cst], writes=[t_Rm[r_]])
            k.op("pool", lambda g: g.tensor_tensor(HW, Hc, Wc[:, :, c:c + 1].to_broadcast([128, G8, 64]), ALU.mult), reads=[t_Hc, tW], writes=[t_HW])
            vb = s % 2
            k.op("pe", lambda pe: pe.matmul(vb_ps[vb][:, :], BObf, Vsi[r_].rearrange("p g i -> p (g i)"), start=True, stop=True),
                 reads=[t_Vsi[r_], t_BObf], writes=[t_vb[vb]])
            k.op("dve", lambda v: v.tensor_tensor(AH, Hc, Ac[:, :, c:c + 1].to_broadcast([128, G8, 64]), ALU.mult), reads=[t_Hc, tA], writes=[t_AH])
            k.op("pe", lambda pe: pe.matmul(sa_ps[:, :], BO, AH.rearrange("p g i -> p (g i)"), start=True, stop=True),
                 reads=[t_AH, t_cst], writes=[t_sa])
            if s > 0:
                emit_y(s - 1)
            k.op("dve", lambda v: v.tensor_tensor(KV, vb_ps[vb][:, :].rearrange("p (g i) -> p g i", g=G8),
                                                  Kc[:, :, c:c + 1].to_broadcast([128, G8, 64]), ALU.mult),
                 reads=[t_vb[vb], tK], writes=[t_KV])
            k.op("dve", lambda v: v.tensor_tensor(KV, KV, HW, ALU.add), reads=[t_HW], writes=[t_KV])
            k.op("dve", lambda v: v.tensor_tensor(tmp1, sa_ps[:, :].rearrange("p (g i) -> p g i", g=G8),
                                                  Bc[:, :, c:c + 1].to_broadcast([128, G8, 64]), ALU.mult),
                 reads=[t_sa, tB], writes=[t_t1])
            k.op("dve", lambda v: v.tensor_tensor(Hn, KV, tmp1, ALU.add), reads=[t_KV, t_t1], writes=[t_Hn])
            k.op("act", lambda a: a.activation(Hbf[hbn], Hn, AF.Copy), reads=[t_Hn], writes=[t_Hbf[hbn]])
    emit_y(nsteps - 1)

    for (i, t) in carved:
        K._merge(tS[i].r, t.w)
        K._merge(tS[i].r, t.r)
    for p in range(4):
        pr = prm[:, p, :]
        k.dma("sp", [(S[0][:, :], YSC[p].rearrange("h i s -> (h i) s"))], tS[0], reads=[t_ysc], writes=[tS[0]])
        k.dma("sp", [(S[1][:, :], YSC[4 + p].rearrange("h i s -> (h i) s"))], tS[1], reads=[t_ysc], writes=[tS[1]])
        for (lo, hi) in SEGS:
            k.op("dve", lambda v: v.tensor_tensor(S[0][:, lo:hi], S[0][:, lo:hi], rev(S[1], lo, hi), ALU.add), reads=[tS[1]], writes=[tS[0]])

        def mean_fn(t0, nn, ps, t_ps):
            k.op("dve", lambda v: v.scalar_tensor_tensor(S[2][:, t0:t0 + nn], ps[:, :nn], -1.0 / 64, S[0][:, t0:t0 + nn], ALU.mult, ALU.add),
                 reads=[t_ps, tS[0]], writes=[tS[2]])
        bo_apply(S[0], tS[0], mean_fn)
        k.op("act", lambda a: a.activation(S[3][:, :], S[2][:, :], AF.Square), reads=[tS[2]], writes=[tS[3]])

        def var_fn(t0, nn, ps, t_ps):
            k.op("act", lambda a: a.activation(S[4][:, t0:t0 + nn], ps[:, :nn], AF.Sqrt, bias=64e-5, scale=1.0 / 64), reads=[t_ps], writes=[tS[4]])
        bo_apply(S[3], tS[3], var_fn)
        k.op("dve", lambda v: v.reciprocal(S[4][:, :], S[4][:, :]), writes=[tS[4]])
        k.op("dve", lambda v: v.tensor_tensor(S[2][:, :], S[2][:, :], S[4][:, :], ALU.mult), reads=[tS[4]], writes=[tS[2]])
        k.op("dve", lambda v: v.tensor_scalar(S[2][:, :], S[2][:, :], pr[:, 6:7], pr[:, 7:8], ALU.mult, ALU.add), reads=[t_prm], writes=[tS[2]])
        k.dma("sp", [(S[5][:, :], BON[p])], tS[5], reads=[t_bon], writes=[tS[5]])
        k.dma("sp", [(S[6][:, :], GS[p])], tS[6], reads=[t_gs], writes=[tS[6]])
        k.op("dve", lambda v: v.tensor_tensor(S[2][:, :], S[2][:, :], S[5][:, :], ALU.add), reads=[tS[5]], writes=[tS[2]])
        k.op("dve", lambda v: v.tensor_tensor(S[2][:, :], S[2][:, :], S[6][:, :], ALU.mult), reads=[tS[6]], writes=[tS[2]])
        k.dma("sp", [(yT_out[512 + p * 128:512 + (p + 1) * 128, :], S[2][:, :])], tS[2], reads=[tS[2]], acc=[t_yout])


def build_even_mixer(nsteps=T, do_lru=True):
    k = K()
    xaT = k.inp("xaT", [512, T])
    gateT = k.inp("gateT", [512, T])
    rkvT = k.inp("rkvT", [3, 512, T])
    loT = k.inp("loT", [736, T])
    lprm_d = k.inp("lprm", [128, 4, 16])
    gw_d = k.inp("gw", [128, 4, 4, 128])
    rprm_d = k.inp("rprm", [128, 4, 16])
    mulo_d = k.inp("mulo", [128, 6])
    w2_d = k.inp("w2", [128, 2, 512])
    a2_d = k.inp("a2", [128, 2, 512])
    g2_d = k.inp("g2", [128, 4, 512])
    cst_d = k.inp("cst", [128, 194])
    yT = k.outp("yT", [1024, T])
    lprm = k.sb([128, 4, 16], F32); rprm = k.sb([128, 4, 16], F32); mulo = k.sb([128, 6], F32); t_prm = k.tok()
    k.dma("sp", [(lprm[:], lprm_d), (rprm[:], rprm_d), (mulo[:], mulo_d)], t_prm, writes=[t_prm])
    gw = k.sb([128, 4, 4, 128], F32); w2 = k.sb([128, 2, 512], F32); a2 = k.sb([128, 2, 512], F32); g2 = k.sb([128, 4, 512], F32)
    cst = k.sb([128, 194], F32); t_w = k.tok()
    k.dma("sp", [(gw[:], gw_d), (w2[:], w2_d), (a2[:], a2_d), (g2[:], g2_d), (cst[:], cst_d)], t_w, writes=[t_w])
    P = Pool9(k)
    PS = [k.ps([128, 512], F32) for _ in range(8)]; tPS = k.toks(8)
    t_y = k.tok()
    if do_lru:
        emit_lru(k, P, PS, tPS, lprm, t_prm, gw, t_w, xaT, gateT, yT, t_y)
    emit_rwkv(k, P, PS, tPS, cst, t_w, rkvT, loT, rprm, t_prm, mulo, t_prm, w2, a2, g2, t_w, yT, t_y, nsteps=nsteps)
    k.finish([t_y])
    return k


def even_consts():
    c = np.zeros((128, 194), np.float32)
    pi = np.arange(128)
    c[:, 0:128] = (pi[:, None] // 64 == pi[None, :] // 64)
    c[:, 128:192] = (pi[:, None] % 64 == np.arange(64)[None, :])
    c[:, 192:194] = (pi[:, None] // 64 == np.arange(2)[None, :])
    return c


def rwkv_core_params(q, mu, w0, w2, a0, a2, g2, k_k, k_a, r_k, ln_w, ln_b):
    prm = np.zeros((128, 4, 16), np.float32)
    rk = r_k.reshape(-1)
    for p in range(4):
        ch = slice(512 * q + 128 * p, 512 * q + 128 * (p + 1))
        for qi in range(3):
            prm[:, p, qi] = mu[2048 * qi:2048 * (qi + 1)][ch]
        prm[:, p, 3] = k_k[ch]; prm[:, p, 4] = k_a[ch]; prm[:, p, 5] = rk[ch]
        prm[:, p, 6] = ln_w[ch]; prm[:, p, 7] = ln_b[ch]
        for d in range(2):
            prm[:, p, 8 + d] = w0[d, ch]; prm[:, p, 10 + d] = a0[d, ch]
    mulo = np.zeros((128, 6), np.float32)
    ml = mu[6144:]
    for c in range(6):
        rows = 96 if c == 5 else 128
        mulo[:rows, c] = ml[c * 128:c * 128 + rows]
    cs = slice(512 * q, 512 * q + 512)
    w2c = np.ascontiguousarray(w2[:, :, cs].transpose(1, 0, 2))
    a2c = np.ascontiguousarray(a2[:, :, cs].transpose(1, 0, 2))
    g2p = np.zeros((512, 512), np.float32); g2p[:480] = g2[:, cs]
    g2c = np.ascontiguousarray(g2p.reshape(4, 128, 512).transpose(1, 0, 2))
    return prm, mulo, w2c, a2c, g2c


NCHUNK = 68
GQ = 4


def build_gla(nchunks=NCHUNK):
    k = K()
    qT = k.inp("qT", [4, 256, T])
    kT = k.inp("kT", [4, 256, T])
    ktok = k.inp("ktok", [4, T, 256])
    vtok = k.inp("vtok", [4, T, 512])
    gklo = k.inp("gklo", [4, 16, T])
    gkw_d = k.inp("gkw", [16, 4, 256])
    gkb_d = k.inp("gkb", [1, 4, 256])
    gate = k.inp("gate", [2, T, 512])
    nw_d = k.inp("nw", [128, 512])
    cst_d = k.inp("cst", [128, 65 + 64 + 128])
    y = k.outp("y", [2, T, 512])
    O = k.dram("osc", [4, T, 512], F32).ap(); t_O = k.tok()

    gkw = k.sb([16, 4, 256], F32); gkb = k.sb([1, 4, 256], F32); nw = k.sb([128, 512], F32); cst = k.sb([128, 257], F32)
    t_c = k.tok()
    k.dma("sp", [(gkw[:], gkw_d), (gkb[:], gkb_d), (nw[:], nw_d), (cst[:], cst_d)], t_c, writes=[t_c])
    LT = cst[0:64, 0:65]
    ONES = cst[0:64, 65:129]
    J = cst[:, 129:257]
    ones1 = cst[0:1, 65:129]

    PSB = [k.ps([128, 512], F32) for _ in range(7)]
    tP = k.toks(7)
    ser = k.toks(7)
    qg = [k.sb([128, 2, GQ * 64], F32) for _ in range(2)]
    kg = [k.sb([128, 2, GQ * 64], F32) for _ in range(2)]
    ktg = [k.sb([64, GQ, 256], F32) for _ in range(2)]
    vtg = [k.sb([64, GQ, 512], F32) for _ in range(2)]
    glg = [k.sb([16, GQ * 64], F32) for _ in range(2)]
    t_in = k.toks(2)
    la = k.sb([64, 256], F32); t_la = k.tok()
    cumt = k.sb([64, 256], F32); t_cumt = k.tok()
    e1 = k.sb([64, 256], F32); t_e1 = k.tok()
    kend = k.sb([64, 256], F32); t_kend = k.tok()
    eT = k.sb([128, 2, 64], F32); einvT = k.sb([128, 2, 64], F32); t_eT = k.tok(); t_einv = k.tok()
    qdec = k.sb([128, 2, 64], F32); kinv = k.sb([128, 2, 64], F32); t_qdec = k.tok(); t_kinv = k.tok()
    dec = k.sb([128, 2], F32); t_dec = k.tok()
    scT = k.sb([64, 64], F32); t_sc = k.tok()
    osb = [k.sb([64, 512], F32) for _ in range(2)]; t_osb = k.toks(2)
    Sst = k.sb([128, 2, 512], F32); t_S = k.tok()
    ng = 0
    for dh in range(4):
        k.op("dve", lambda v: v.memset(Sst[:], 0.0), writes=[t_S])
        for gi in range((nchunks + GQ - 1) // GQ):
            b = ng % 2
            ng += 1
            g0 = gi * GQ * 64
            gl = min(GQ, nchunks - gi * GQ) * 64
            k.dma("sp", [(qg[b][:, :, :gl], qT[dh, :, g0:g0 + gl].rearrange("(c p) t -> p c t", p=128)),
                         (kg[b][:, :, :gl], kT[dh, :, g0:g0 + gl].rearrange("(c p) t -> p c t", p=128)),
                         (ktg[b][:, :gl // 64, :], ktok[dh, g0:g0 + gl, :].rearrange("(c j) k -> j c k", j=64)),
                         (vtg[b][:, :gl // 64, :], vtok[dh, g0:g0 + gl, :].rearrange("(c j) k -> j c k", j=64)),
                         (glg[b][:, :gl], gklo[dh, :, g0:g0 + gl])], t_in[b], writes=[t_in[b]])
            for ci in range(gl // 64):
                n = gi * GQ + ci
                c0 = n * 64
                lo = ci * 64
                k.op("pe", lambda p: p.matmul(PSB[0][0:64, 0:256], glg[b][:, lo:lo + 64], gkw[:, dh, :], start=True, stop=False),
                     reads=[t_in[b], t_c], writes=[tP[0]], inc=False)
                k.op("pe", lambda p: p.matmul(PSB[0][0:64, 0:256], ones1, gkb[:, dh, :], start=False, stop=True), reads=[t_c], acc=[tP[0]])
                k.op("act", lambda a: a.activation(la[:, :], PSB[0][0:64, 0:256], AF.Sigmoid), reads=[tP[0]], writes=[t_la, ser[0]])
                k.op("act", lambda a: a.activation(la[:, :], la[:, :], AF.Ln), writes=[t_la])
                k.op("dve", lambda v: v.tensor_scalar(la[:, :], la[:, :], 1.0 / 16, -1.0, ALU.mult, ALU.max), writes=[t_la])
                k.op("pe", lambda p: p.matmul(PSB[1][0:64, 0:256], LT[:, 0:64], la[:, :], start=True, stop=True),
                     reads=[t_la, t_c], writes=[tP[1]], inc=False)
                k.op("pe", lambda p: p.matmul(PSB[1][0:64, 256:512], ONES, la[:, :], start=True, stop=True), reads=[t_la], acc=[tP[1]])
                for c in range(2):
                    k.op("pe", lambda p, c=c: p.matmul(PSB[2][:, c * 128:c * 128 + 65], la[:, c * 128:(c + 1) * 128], LT, start=True, stop=True),
                         reads=[t_la, t_c], writes=[tP[2]] if c == 0 else (), acc=[tP[2]] if c else (), inc=(c == 1))
                k.op("act", lambda a: a.activation(cumt[:, :], PSB[1][0:64, 0:256], AF.Copy), reads=[tP[1]], writes=[t_cumt, ser[1]])
                k.op("dve", lambda v: v.tensor_tensor(e1[:, :], PSB[1][0:64, 256:512], cumt[:, :], ALU.subtract),
                     reads=[tP[1], t_cumt], writes=[t_e1, ser[1]])
                k.op("act", lambda a: a.activation(e1[:, :], e1[:, :], AF.Exp), writes=[t_e1])
                k.op("dve", lambda v: v.tensor_tensor(kend[:, :], ktg[b][:, ci, :], e1[:, :], ALU.mult), reads=[t_in[b], t_e1], writes=[t_kend])
                cview = PSB[2][:, 0:256].rearrange("p (c x) -> p c x", c=2)
                k.op("act", lambda a: a.activation(eT[:, :, :], cview[:, :, 0:64], AF.Exp), reads=[tP[2]], writes=[t_eT, ser[2]])
                k.op("act", lambda a: a.activation(einvT[:, :, :], cview[:, :, 0:64], AF.Exp, scale=-1.0), reads=[tP[2]], writes=[t_einv, ser[2]])
                k.op("act", lambda a: a.activation(dec[:, :], cview[:, :, 64], AF.Exp), reads=[tP[2]], writes=[t_dec, ser[2]])
                k.op("dve", lambda v: v.scalar_tensor_tensor(qdec[:, :, :], qg[b][:, :, lo:lo + 64], 0.0625, eT[:, :, :], ALU.mult, ALU.mult),
                     reads=[t_in[b], t_eT], writes=[t_qdec])
                k.op("dve", lambda v: v.tensor_tensor(kinv[:, :, :], kg[b][:, :, lo:lo + 64], einvT[:, :, :], ALU.mult),
                     reads=[t_in[b], t_einv], writes=[t_kinv])
                for c in range(2):
                    k.op("pe", lambda p, c=c: p.matmul(PSB[3][0:64, 0:64], kinv[:, c, :], qdec[:, c, :], start=(c == 0), stop=(c == 1)),
                         reads=[t_kinv, t_qdec] if c == 0 else (), writes=[tP[3]] if c == 0 else (), acc=[tP[3]] if c else (), inc=(c == 1))
                k.op("dve", lambda v: v.tensor_tensor(scT[:, :], PSB[3][0:64, 0:64], LT[:, 0:64], ALU.mult), reads=[tP[3], t_c], writes=[t_sc, ser[3]])
                k.op("pe", lambda p: p.matmul(PSB[4][0:64, :], scT[:, :], vtg[b][:, ci, :], start=True, stop=False),
                     reads=[t_sc, t_in[b]], writes=[tP[4]], inc=False)
                for c in range(2):
                    k.op("pe", lambda p, c=c: p.matmul(PSB[4][0:64, :], qdec[:, c, :], Sst[:, c, :], start=False, stop=(c == 1)),
                         reads=[t_qdec, t_S] if c == 0 else (), acc=[tP[4]], inc=(c == 1))
                ob = n % 2
                k.op("act", lambda a: a.activation(osb[ob][:, :], PSB[4][0:64, :], AF.Copy), reads=[tP[4]], writes=[t_osb[ob], ser[4]])
                k.dma("sp", [(O[dh, c0:c0 + 64, :], osb[ob][:, :])], t_osb[ob], reads=[t_osb[ob]], acc=[t_O])
                for c in range(2):
                    k.op("pe", lambda p, c=c: p.matmul(PSB[5 + c][:, :], kend[:, c * 128:(c + 1) * 128], vtg[b][:, ci, :], start=True, stop=True),
                         reads=[t_kend, t_in[b]], writes=[tP[5 + c]])
                    k.op("dve", lambda v, c=c: v.scalar_tensor_tensor(Sst[:, c, :], Sst[:, c, :], dec[:, c:c + 1], PSB[5 + c][:, :], ALU.mult, ALU.add),
                         reads=[tP[5 + c], t_dec], writes=[t_S, ser[5 + c]])
    o0 = [k.sb([128, 512], F32) for _ in range(2)]; o1 = [k.sb([128, 512], F32) for _ in range(2)]; gt = [k.sb([128, 512], F32) for _ in range(2)]
    t_o0 = k.toks(2); t_o1 = k.toks(2); t_gt = k.toks(2)
    junk = k.sb([128, 512], F32); t_junk = k.tok()
    st = k.sb([128, 4], F32); t_st = k.tok()
    t_y = k.tok()
    nn_ = 0
    ntile = (nchunks * 64) // 128
    for hl in range(2):
        for m in range(ntile):
            b = nn_ % 2
            nn_ += 1
            r0 = m * 128
            mir = (1 - m) if m < 2 else (2 + (31 - (m - 2)))
            k.dma("sp", [(o0[b][:, :], O[hl, r0:r0 + 128, :])], t_o0[b], reads=[t_O], writes=[t_o0[b]])
            k.dma("sp", [(o1[b][:, :], O[2 + hl, mir * 128:mir * 128 + 128, :])], t_o1[b], reads=[t_O], writes=[t_o1[b]])
            k.dma("sp", [(gt[b][:, :], gate[hl, r0:r0 + 128, :])], t_gt[b], writes=[t_gt[b]])
            k.op("pe", lambda p: p.matmul(PSB[0][:, :], J, o1[b][:, :], start=True, stop=True), reads=[t_o1[b], t_c], writes=[tP[0]])
            k.op("dve", lambda v: v.tensor_tensor(o0[b][:, :], o0[b][:, :], PSB[0][:, :], ALU.add), reads=[tP[0]], writes=[t_o0[b], ser[0]])
            k.op("act", lambda a: a.activation(junk[:, :], o0[b][:, :], AF.Square, accum_out=st[:, 0:1]), reads=[t_o0[b]], writes=[t_junk, t_st])
            k.op("act", lambda a: a.activation(st[:, 1:2], st[:, 0:1], AF.Sqrt, bias=1e-5, scale=1.0 / 512), writes=[t_st])
            k.op("dve", lambda v: v.reciprocal(st[:, 2:3], st[:, 1:2]), writes=[t_st])
            k.op("dve", lambda v: v.scalar_tensor_tensor(o0[b][:, :], o0[b][:, :], st[:, 2:3], nw[:, :], ALU.mult, ALU.mult),
                 reads=[t_st, t_c], writes=[t_o0[b]])
            k.op("act", lambda a: a.activation(gt[b][:, :], gt[b][:, :], AF.Silu), writes=[t_gt[b]])
            k.op("dve", lambda v: v.tensor_tensor(o0[b][:, :], o0[b][:, :], gt[b][:, :], ALU.mult), reads=[t_gt[b]], writes=[t_o0[b]])
            k.dma("sp", [(y[hl, r0:r0 + 128, :], o0[b][:, :])], t_o0[b], reads=[t_o0[b]], acc=[t_y])
    k.finish([t_y])
    return k


def gla_consts():
    c = np.zeros((128, 257), np.float32)
    j = np.arange(64)
    c[0:64, 0:64] = (j[:, None] <= j[None, :])
    c[0:64, 64] = 1.0
    c[0:64, 65:129] = 1.0
    c[:, 129:257] = np.eye(128, dtype=np.float32)[::-1]
    return c


def seg_rev_np(z, axis=0):
    idx = np.concatenate([np.arange(NCTX - 1, -1, -1), np.arange(TSEQ - 1, NCTX - 1, -1)])
    return np.take(z, idx, axis=axis)


def build_lru_only():
    k = K()
    xaT = k.inp("xaT", [512, T])
    gateT = k.inp("gateT", [512, T])
    prm_d = k.inp("prm", [128, 4, 16])
    gw_d = k.inp("gw", [128, 4, 4, 128])
    yT = k.outp("yT", [512, T])
    prm = k.sb([128, 4, 16], F32); t_prm = k.tok()
    gw = k.sb([128, 4, 4, 128], F32); t_gw = k.tok()
    k.dma("sp", [(prm[:], prm_d)], t_prm, writes=[t_prm])
    k.dma("sp", [(gw[:], gw_d)], t_gw, writes=[t_gw])
    P = Pool9(k)
    PS = [k.ps([128, 512], F32) for _ in range(8)]; tPS = k.toks(8)
    t_y = k.tok()
    emit_lru(k, P, PS, tPS, prm, t_prm, gw, t_gw, xaT, gateT, yT, t_y)
    k.finish([t_y])
    return k


def lru_core_params(q, conv_w, conv_b, ga_w, ga_b, gx_w, gx_b, lam):
    prm = np.zeros((128, 4, 16), np.float32)
    gw = np.zeros((128, 4, 4, 128), np.float32)
    for n in range(4):
        ch = slice((4 * q + n) * 128, (4 * q + n + 1) * 128)
        prm[:, n, 0:4] = conv_w[:, ch].T
        prm[:, n, 4] = conv_b[ch]
        for d in range(2):
            prm[:, n, 5 + 2 * d] = ga_b[d, ch]
            prm[:, n, 6 + 2 * d] = gx_b[d, ch]
            prm[:, n, 9 + d] = lam[d, ch]
            gw[:, n, 2 * d, :] = ga_w[d, 4 * q + n]
            gw[:, n, 2 * d + 1, :] = gx_w[d, 4 * q + n]
    return prm, gw


def fm_vec(v):
    return np.ascontiguousarray(np.asarray(v, np.float32).reshape(KC, 128).T)


def token_shards(x, ctx):
    out = []
    for b in range(2):
        seq = np.concatenate([ctx[b], x[b]], axis=0)
        for q in range(4):
            out.append(np.ascontiguousarray(seq[q * NTOK:(q + 1) * NTOK]))
    return out


def run_mods(c, c_ctx, ada_w, ada_b):
    cond = np.stack([c[0], c[1], c_ctx], 0).astype(np.float32)
    condT = np.ascontiguousarray(cond.T.reshape(KC, 128, 3).transpose(1, 0, 2))
    ins = []
    for core in range(8):
        sl = slice(core * 3072, (core + 1) * 3072)
        ins.append({"condT": condT, "aw": np.ascontiguousarray(ada_w[:, :, sl]),
                    "ab": np.ascontiguousarray(np.broadcast_to(ada_b[:, None, sl], (2, 3, 3072)))})
    res = run_bass_kernel_spmd(build_mods().nc, ins, core_ids=list(range(8)))
    return np.concatenate([res.results[core]["mod"] for core in range(8)], axis=2)


def inproj_vecs(mod_l, gain, b, q):
    lat = mod_l[b]
    ctxm = mod_l[2]
    first = ctxm if q == 0 else lat
    vec = np.zeros((128, 8, KC), np.float32)
    vec[:, 0] = fm_vec(gain)
    vec[:, 1] = fm_vec(first[D:2 * D]); vec[:, 2] = fm_vec(first[0:D])
    vec[:, 3] = fm_vec(lat[D:2 * D]); vec[:, 4] = fm_vec(lat[0:D])
    return vec


GU_ORDER = np.concatenate([np.concatenate([np.arange(cc * 128, cc * 128 + 128), np.arange(512 + cc * 128, 512 + cc * 128 + 128)])
                           for cc in range(4)])


def moe_core_inputs(core, router_w_l, router_b_l, w_gu_l, b_gu_l, w_down_l, b_down_l):
    own = list(range(NEXP * core, NEXP * core + NEXP))
    perm = own + [e for e in range(32) if e not in own]
    rw = np.ascontiguousarray(router_w_l[:, perm])
    rb = np.ascontiguousarray(np.broadcast_to(router_b_l[perm][None, :], (128, 32)))
    wgu = np.ascontiguousarray(w_gu_l[own][:, :, GU_ORDER])
    bg = b_gu_l[own][:, GU_ORDER].reshape(NEXP, 8, 128)
    bgu = np.ascontiguousarray(bg.transpose(2, 0, 1))
    return {"rw": rw, "rb": rb, "wgu": wgu, "bgu": bgu, "wd": np.ascontiguousarray(w_down_l[own]),
            "bd": np.ascontiguousarray(b_down_l[own])}


def moe_vecs(mod_l, gain, b):
    vec = np.zeros((128, 8, KC), np.float32)
    vec[:, 0] = fm_vec(gain)
    for ci, row in enumerate([2, b]):
        vec[:, 1 + 2 * ci] = fm_vec(mod_l[row][4 * D:5 * D])
        vec[:, 2 + 2 * ci] = fm_vec(mod_l[row][3 * D:4 * D])
    return vec


def build_combine_inproj(ncols, nparts=4):
    k = K()
    parts = k.inp("parts", [nparts, NTOK, D])
    x1 = k.inp("x1", [NTOK, D])
    g_d = k.inp("g5", [128, 2, D])
    vec_d = k.inp("vec", [128, 8, KC])
    W = k.inp("w", [D, ncols])
    x2 = k.outp("x2", [NTOK, D])
    outT = k.outp("projT", [ncols, NTOK])
    g5 = k.sb([128, 2, D], F32); t_g = k.tok()
    k.dma("sp", [(g5[:, c, :], g_d[:, c, :]) for c in range(2)], t_g, writes=[t_g])
    t_x2 = k.tok()
    vec = k.sb([128, 8, KC], F32); t_vec = k.tok()
    AB = k.sb([128, 4, KC], F32); t_AB = k.tok()
    k.dma("sp", [(vec[:], vec_d)], t_vec, writes=[t_vec])
    make_AB(k, vec, t_vec, AB, t_AB, 0, 1, 2, 0)
    make_AB(k, vec, t_vec, AB, t_AB, 0, 3, 4, 2)
    fb = FrontBufs(k)
    emit_combine(k, parts, x1, g5, t_g, x2, t_x2, nparts=nparts, bufs=(fb.xt, fb.t_xt))
    hT = k.sb([128, KC, NTOK], BF16)
    t_hT = k.toks(NT)
    front_end(k, fb, x2, lambda i: AB[:, 0 if i < 2 else 2, :], lambda i: AB[:, 1 if i < 2 else 3, :], t_AB, hT, t_hT, t_src=t_x2)
    gb = GemmBufs(k, nwb=2)
    t_out = k.tok()
    gemm_f(k, gb, W, ncols, hT, t_hT, outT, t_out)
    k.finish([t_out, t_x2])
    return k


def build_combine_final(nparts=4):
    k = K()
    parts = k.inp("parts", [nparts, NTOK, D])
    x1 = k.inp("x1", [NTOK, D])
    g_d = k.inp("g5", [128, 2, D])
    gn_d = k.inp("g", [128, D])
    out = k.outp("o", [NTOK, D])
    x4 = k.dram("x4", [NTOK, D], F32).ap(); t_x4 = k.tok()
    g5 = k.sb([128, 2, D], F32); t_g = k.tok()
    k.dma("sp", [(g5[:, c, :], g_d[:, c, :]) for c in range(2)], t_g, writes=[t_g])
    gn = k.sb([128, D], F32); t_gn = k.tok()
    k.dma("sp", [(gn[:], gn_d)], t_gn, writes=[t_gn])
    xt = [k.sb([128, D], F32) for _ in range(2)]; t_x = k.toks(2)
    emit_combine(k, parts, x1, g5, t_g, x4, t_x4, nparts=nparts, bufs=(xt, t_x))
    emit_final_norm(k, x4, t_x4, gn, t_gn, out, xt, t_x)
    return k


def emit_final_norm(k, x, t_src, g, t_g, out, xt, t_x):
    junk = k.sb([128, D], BF16); t_j = k.tok()
    st = k.sb([128, NT, 3], F32); t_s = k.toks(NT)
    t_out = k.tok()
    for i in range(NT):
        rows = tile_rows(i)
        s = i % 2
        r0 = i * 128
        k.dma("sp", [(xt[s][:rows, 0:2048], x[r0:r0 + rows, 0:2048]), (xt[s][:rows, 2048:4096], x[r0:r0 + rows, 2048:4096])],
              t_x[s], reads=[t_src] if t_src is not None else (), writes=[t_x[s]])
        k.op("act", lambda a: a.activation(junk[:rows, :], xt[s][:rows, :], AF.Square, accum_out=st[:rows, i, 0:1]),
             reads=[t_x[s]], writes=[t_j, t_s[i]])
        k.op("act", lambda a: a.activation(st[:rows, i, 1:2], st[:rows, i, 0:1], AF.Sqrt, bias=EPS, scale=1.0 / D),
             reads=[t_s[i]], writes=[t_s[i]])
        k.op("dve", lambda v: v.reciprocal(st[:rows, i, 2:3], st[:rows, i, 1:2]), reads=[t_s[i]], writes=[t_s[i]])
        k.op("act", lambda a: a.activation(xt[s][:rows, :], xt[s][:rows, :], AF.Copy, scale=st[:rows, i, 2:3]),
             reads=[t_s[i]], writes=[t_x[s]])
        k.op("dve", lambda v: v.tensor_tensor(xt[s][:rows, :], xt[s][:rows, :], g[:rows, :], ALU.mult),
             reads=[t_g], writes=[t_x[s]])
        k.dma("sp", [(out[r0:r0 + rows, :], xt[s][:rows, :])], t_x[s], reads=[t_x[s]], acc=[t_out])
    k.finish([t_out])


CORES = list(range(8))
_PROG = {}


def prog(name, builder, *a):
    return builder(*a).nc


def launch(nc, ins):
    return run_bass_kernel_spmd(nc, ins, core_ids=CORES).results


def rep128(v):
    return np.ascontiguousarray(np.broadcast_to(np.asarray(v, np.float32)[None], (128,) + tuple(np.shape(v))))


def gate_rows(mod_l, b, q, chunk):
    lat = mod_l[b][chunk * D:(chunk + 1) * D]
    first = mod_l[2][chunk * D:(chunk + 1) * D] if q == 0 else lat
    return rep128(np.stack([first, lat], 0))


def col_major_index():
    r = np.arange(64)
    perm = (r[None, :] * 64 + np.arange(64)[:, None]).reshape(-1)
    return np.concatenate([np.arange(NCTX), NCTX + perm])


def assemble_seq(per_core, axis):
    return [np.concatenate([per_core[b * 4 + q] for q in range(4)], axis=axis) for b in range(2)]


def run_moe_layer(xs, mod_l, gain, rw, rb, wgu, bgu, wdn, bdn):
    seq = assemble_seq(xs, 0)
    eg_in = [moe_core_inputs(eg, rw, rb, wgu, bgu, wdn, bdn) for eg in range(4)]
    ins = []
    for b in range(2):
        vec = moe_vecs(mod_l, gain, b)
        for eg in range(4):
            d = dict(eg_in[eg])
            d["x1"] = seq[b]
            d["vec"] = vec
            ins.append(d)
    res = launch(prog("moe", build_moe), ins)
    parts = []
    for b in range(2):
        for q in range(4):
            parts.append(np.ascontiguousarray(np.stack([res[b * 4 + eg]["ypart"][q * NTOK:(q + 1) * NTOK] for eg in range(4)], 0)))
    return parts


def kernel(x, c, ctx, c_ctx, ada_w, ada_b, norm_mix, norm_ffn, norm_final,
           ev_w_in, ev_w_out, lru_conv_w, lru_conv_b, lru_gate_a_w, lru_gate_a_b,
           lru_gate_x_w, lru_gate_x_b, lru_lambda,
           rwkv_mu, rwkv_w0, rwkv_w2, rwkv_a0, rwkv_a2, rwkv_g2, rwkv_k_k, rwkv_k_a,
           rwkv_r_k, rwkv_ln_w, rwkv_ln_b,
           od_w_in, od_w_out, gla_gk_w2, gla_gk_b, gla_norm_w,
           router_w, router_b, exp_w_gu, exp_b_gu, exp_w_down, exp_b_down):
    A = lambda z: np.asarray(z, np.float32)
    x, ctx = A(x), A(ctx)
    norm_mix, norm_ffn = A(norm_mix), A(norm_ffn)
    mod = run_mods(A(c), A(c_ctx), A(ada_w), A(ada_b))
    xs = token_shards(x, ctx)

    w_in = np.ascontiguousarray(A(ev_w_in)[0])
    res = launch(prog("inproj", build_inproj, EVEN_IN),
                 [{"x": xs[cc], "vec": inproj_vecs(mod[0], norm_mix[0], cc // 4, cc % 4), "w": w_in} for cc in CORES])
    PT = assemble_seq([r["projT"] for r in res], 1)
    del res
    cst = even_consts()
    ins = []
    for b in range(2):
        for q in range(4):
            cs = slice(512 * q, 512 * q + 512)
            lprm, gw = lru_core_params(q, A(lru_conv_w)[0], A(lru_conv_b)[0], A(lru_gate_a_w)[0], A(lru_gate_a_b)[0],
                                       A(lru_gate_x_w)[0], A(lru_gate_x_b)[0], A(lru_lambda)[0])
            rprm, mulo, w2c, a2c, g2c = rwkv_core_params(q, A(rwkv_mu)[0], A(rwkv_w0)[0], A(rwkv_w2)[0], A(rwkv_a0)[0], A(rwkv_a2)[0],
                                                         A(rwkv_g2)[0], A(rwkv_k_k)[0], A(rwkv_k_a)[0], A(rwkv_r_k)[0],
                                                         A(rwkv_ln_w)[0], A(rwkv_ln_b)[0])
            P_ = PT[b]
            rkvT = np.ascontiguousarray(np.stack([P_[4096 + 2048 * i:4096 + 2048 * (i + 1)][cs] for i in range(3)], 0))
            ins.append({"gateT": np.ascontiguousarray(P_[0:2048][cs]), "xaT": np.ascontiguousarray(P_[2048:4096][cs]), "rkvT": rkvT,
                        "loT": np.ascontiguousarray(P_[10240:10976]), "lprm": lprm, "gw": gw, "rprm": rprm, "mulo": mulo,
                        "w2": w2c, "a2": a2c, "g2": g2c, "cst": cst})
    del PT
    res = launch(prog("even", build_even_mixer), ins)
    del ins
    YT = []
    for b in range(2):
        yt = np.empty((D, TSEQ), np.float32)
        for q in range(4):
            r = res[b * 4 + q]["yT"]
            yt[512 * q:512 * q + 512] = r[0:512]
            yt[2048 + 512 * q:2048 + 512 * q + 512] = r[512:1024]
        YT.append(yt)
    del res
    w_out = np.ascontiguousarray(A(ev_w_out)[0])
    res = launch(prog("outproj", build_outproj),
                 [{"yT": np.ascontiguousarray(YT[cc // 4][:, (cc % 4) * NTOK:(cc % 4 + 1) * NTOK]), "x": xs[cc],
                   "g2": gate_rows(mod[0], cc // 4, cc % 4, 2), "w": w_out} for cc in CORES])
    x1 = [r["x1"] for r in res]
    del res, YT
    parts = run_moe_layer(x1, mod[0], norm_ffn[0], A(router_w)[0], A(router_b)[0], A(exp_w_gu)[0], A(exp_b_gu)[0],
                          A(exp_w_down)[0], A(exp_b_down)[0])

    w_in1 = np.ascontiguousarray(A(od_w_in)[0])
    res = launch(prog("cinproj", build_combine_inproj, ODD_IN),
                 [{"parts": parts[cc], "x1": x1[cc], "g5": gate_rows(mod[0], cc // 4, cc % 4, 5),
                   "vec": inproj_vecs(mod[1], norm_mix[1], cc // 4, cc % 4), "w": w_in1} for cc in CORES])
    del parts
    x2 = [r["x2"] for r in res]
    PT = assemble_seq([r["projT"] for r in res], 1)
    del res
    cm = col_major_index()
    gcst = gla_consts()
    nw = rep128(A(gla_norm_w)[0])
    gk_w2, gk_b = A(gla_gk_w2)[0], A(gla_gk_b)[0]
    ins = []
    for b in range(2):
        Pp = PT[b][:, cm]
        for hp in range(4):
            d = {k_: [] for k_ in ("qT", "kT", "ktok", "vtok", "gklo")}
            gkw = np.zeros((16, 4, 256), np.float32)
            gkb = np.zeros((1, 4, 256), np.float32)
            for dd in range(2):
                for hl in range(2):
                    h = 2 * hp + hl
                    qs = Pp[h * 256:(h + 1) * 256]
                    ks = Pp[2048 + h * 256:2048 + (h + 1) * 256]
                    vs = Pp[4096 + h * 512:4096 + (h + 1) * 512]
                    gl = Pp[12288 + dd * 16:12288 + (dd + 1) * 16]
                    if dd == 1:
                        qs, ks, vs, gl = (seg_rev_np(z, axis=1) for z in (qs, ks, vs, gl))
                    d["qT"].append(qs); d["kT"].append(ks); d["ktok"].append(ks.T); d["vtok"].append(vs.T); d["gklo"].append(gl)
                    gkw[:, dd * 2 + hl, :] = gk_w2[dd][:, h * 256:(h + 1) * 256]
                    gkb[0, dd * 2 + hl, :] = gk_b[dd][h * 256:(h + 1) * 256]
            o = {k_: np.ascontiguousarray(np.stack(v_, 0)) for k_, v_ in d.items()}
            o["gkw"] = gkw; o["gkb"] = gkb
            o["gate"] = np.ascontiguousarray(np.stack([Pp[8192 + (2 * hp + hl) * 512:8192 + (2 * hp + hl + 1) * 512].T for hl in range(2)], 0))
            o["nw"] = nw; o["cst"] = gcst
            ins.append(o)
    del PT
    res = launch(prog("gla", build_gla), ins)
    del ins
    YT = []
    for b in range(2):
        yp = np.concatenate([res[b * 4 + hp]["y"][hl] for hp in range(4) for hl in range(2)], axis=1)
        yn = np.empty_like(yp)
        yn[cm] = yp
        YT.append(np.ascontiguousarray(yn.T))
    del res
    w_out1 = np.ascontiguousarray(A(od_w_out)[0])
    res = launch(prog("outproj", build_outproj),
                 [{"yT": np.ascontiguousarray(YT[cc // 4][:, (cc % 4) * NTOK:(cc % 4 + 1) * NTOK]), "x": x2[cc],
                   "g2": gate_rows(mod[1], cc // 4, cc % 4, 2), "w": w_out1} for cc in CORES])
    x3 = [r["x1"] for r in res]
    del res, YT
    parts = run_moe_layer(x3, mod[1], norm_ffn[1], A(router_w)[1], A(router_b)[1], A(exp_w_gu)[1], A(exp_b_gu)[1],
                          A(exp_w_down)[1], A(exp_b_down)[1])
    gfin = rep128(A(norm_final))
    res = launch(prog("cfinal", build_combine_final),
                 [{"parts": parts[cc], "x1": x3[cc], "g5": gate_rows(mod[1], cc // 4, cc % 4, 5), "g": gfin} for cc in CORES])
    out = np.zeros((2, 4096, D), np.float32)
    for b in range(2):
        seq = np.concatenate([res[b * 4 + q]["o"] for q in range(4)], axis=0)
        out[b] = seq[NCTX:]
    return out
```

```python
import numpy as np
import concourse.bass as bass
import concourse.mybir as mybir
from concourse.bass_utils import run_bass_kernel_spmd

F32 = mybir.dt.float32
BF16 = mybir.dt.bfloat16
I32 = mybir.dt.int32
U32 = mybir.dt.uint32
AF = mybir.ActivationFunctionType
ALU = mybir.AluOpType
AX = mybir.AxisListType

D = 4096
KC = 32
NTOK = 1088
NT = 9
TSEQ = 4352
NCTX = 256
EVEN_IN = 10976
ODD_IN = 12320
EPS = 1e-6


class Tok:
    __slots__ = ("name", "w", "r", "chan", "ccount")

    def __init__(self, name=""):
        self.name = name
        self.w = {}
        self.r = {}
        self.chan = None
        self.ccount = 0


class K:
    def __init__(self):
        self.nc = bass.Bass("TRN2", target_bir_lowering=False)
        nc = self.nc
        self.eng = {"pe": nc.tensor, "dve": nc.vector, "act": nc.scalar, "pool": nc.gpsimd, "sp": nc.sync}
        self.sem = {e: nc.alloc_semaphore("c_" + e) for e in self.eng}
        self.cnt = {e: 0 for e in self.eng}
        self.waited = {}
        self.pending = {}
        self.ninst = 0
        self._uid = 0

    def uid(self, p):
        self._uid += 1
        return f"{p}{self._uid}"

    def sb(self, shape, dt, name=None):
        return self.nc.alloc_sbuf_tensor(name or self.uid("sb"), list(shape), dt)

    def ps(self, shape, dt=F32, name=None):
        return self.nc.alloc_psum_tensor(name or self.uid("ps"), list(shape), dt)

    def dram(self, name, shape, dt, kind="Internal"):
        return self.nc.dram_tensor(name, list(shape), dt, kind=kind)

    def inp(self, name, shape, dt=F32):
        return self.dram(name, shape, dt, kind="ExternalInput").ap()

    def outp(self, name, shape, dt=F32):
        return self.dram(name, shape, dt, kind="ExternalOutput").ap()

    def tok(self, name=""):
        return Tok(name)

    def toks(self, n):
        return [Tok() for _ in range(n)]

    def _wait(self, e, ev):
        for sid, (sem, val) in ev.items():
            if e == "pe" and sem is self.sem["pe"]:
                continue
            key = (e, sid)
            if self.waited.get(key, 0) >= val:
                continue
            self.waited[key] = val
            self.eng[e].wait_ge(sem, val)
            self.ninst += 1

    @staticmethod
    def _merge(d, ev):
        for sid, (sem, val) in ev.items():
            if sid not in d or d[sid][1] < val:
                d[sid] = (sem, val)

    def _deps(self, e, reads, writes, acc):
        for t in reads:
            self._wait(e, t.w)
        for t in writes:
            self._wait(e, t.w)
            self._wait(e, t.r)
        for t in acc:
            self._wait(e, t.r)

    def _commit(self, ev, reads, writes, acc):
        for t in reads:
            self._merge(t.r, ev)
        for t in writes:
            t.w = dict(ev)
            t.r = {}
        for t in acc:
            self._merge(t.w, ev)
            t.r = {}

    def op(self, e, fn, reads=(), writes=(), acc=(), inc=True):
        self._deps(e, reads, writes, acc)
        ins = fn(self.eng[e])
        self.ninst += 1
        pend = self.pending.setdefault(e, [])
        pend.append((tuple(reads), tuple(writes), tuple(acc)))
        if not inc:
            return None
        self.cnt[e] += 1
        ins.then_inc(self.sem[e], 1)
        ev = {id(self.sem[e]): (self.sem[e], self.cnt[e])}
        for (r, w, a) in pend:
            self._commit(ev, r, w, a)
        self.pending[e] = []
        return ev

    def dma(self, e, pairs, owner, reads=(), writes=(), acc=(), **kw):
        if owner.chan is None:
            owner.chan = self.nc.alloc_semaphore(self.uid("d"))
        self._deps(e, reads, writes, acc)
        for (o, i) in pairs:
            self.eng[e].dma_start(out=o, in_=i, **kw).then_inc(owner.chan, 16)
            owner.ccount += 16
            self.ninst += 1
        ev = {id(owner.chan): (owner.chan, owner.ccount)}
        self._commit(ev, reads, writes, acc)
        return ev

    def finish(self, toks, e="sp"):
        for t in toks:
            self._wait(e, t.w)
            self._wait(e, t.r)


def make_ident(k, dt=F32):
    idf = k.sb([128, 128], F32)
    t = k.tok()
    k.op("pool", lambda g: g.memset(idf[:], 0.0), writes=[t])
    k.op("pool", lambda g: g.affine_select(out=idf[:], in_=idf[:], compare_op=ALU.not_equal, fill=1.0,
                                            base=0, pattern=[[-1, 128]], channel_multiplier=1),
         reads=[t], writes=[t])
    if dt == F32:
        return idf, t
    idb = k.sb([128, 128], dt)
    t2 = k.tok()
    k.op("dve", lambda v: v.tensor_copy(idb[:], idf[:]), reads=[t], writes=[t2])
    return idb, t2


def tile_rows(i):
    return 128 if i < 8 else 64


def build_mods():
    k = K()
    condT = k.inp("condT", [128, KC, 3])
    aw = k.inp("aw", [2, D, 3072])
    ab = k.inp("ab", [2, 3, 3072])
    out = k.outp("mod", [2, 3, 3072])
    ct = k.sb([128, KC, 3], F32); t_c = k.tok()
    sc = k.sb([128, KC, 3], F32); t_s = k.tok()
    abt = k.sb([3, 2, 3072], F32); t_ab = k.tok()
    ot = k.sb([3, 2, 3072], F32); t_o = k.tok()
    k.dma("sp", [(ct[:], condT)], t_c, writes=[t_c])
    k.dma("sp", [(abt[:, l, :], ab[l]) for l in range(2)], t_ab, writes=[t_ab])
    k.op("act", lambda a: a.activation(sc[:], ct[:], AF.Silu), reads=[t_c], writes=[t_s])
    wb = [k.sb([128, KC, 512], F32) for _ in range(2)]
    t_w = k.toks(2)
    pss = [k.ps([3, 512], F32) for _ in range(2)]
    t_p = k.toks(2)
    it = 0
    for l in range(2):
        for nb in range(6):
            s = it % 2
            src = aw[l, :, nb * 512:(nb + 1) * 512].rearrange("(kc p) n -> p kc n", p=128)
            k.dma("sp", [(wb[s][:, 0:16, :], src[:, 0:16, :]), (wb[s][:, 16:32, :], src[:, 16:32, :])],
                  t_w[s], writes=[t_w[s]])
            for kc in range(KC):
                k.op("pe", lambda p, kc=kc, s=s: p.matmul(pss[s][:], sc[:, kc, :], wb[s][:, kc, :],
                                                           start=(kc == 0), stop=(kc == KC - 1)),
                     reads=[t_s, t_w[s]], writes=[t_p[s]] if kc == 0 else (), acc=[t_p[s]] if kc else ())
            k.op("dve", lambda v, s=s, l=l, nb=nb: v.tensor_tensor(ot[:, l, nb * 512:(nb + 1) * 512], pss[s][:],
                                                                   abt[:, l, nb * 512:(nb + 1) * 512], ALU.add),
                 reads=[t_p[s], t_ab], acc=[t_o])
            it += 1
    t_out = k.tok()
    k.dma("sp", [(out[l], ot[:, l, :]) for l in range(2)], t_o, reads=[t_o], writes=[t_out])
    k.finish([t_out])
    return k


class FrontBufs:
    def __init__(self, k, nbuf=2):
        self.nbuf = nbuf
        self.xt = [k.sb([128, D], F32) for _ in range(nbuf)]
        self.t_xt = k.toks(nbuf)
        self.junk = k.sb([128, D], BF16)
        self.t_junk = k.tok()
        self.ss = k.sb([128, NT], F32)
        self.den = k.sb([128, NT], F32)
        self.rstd = k.sb([128, NT], F32)
        self.t_ss = k.toks(NT)
        self.tp = [k.ps([128, 4, 128], F32) for _ in range(2)]
        self.t_tp = k.toks(2)
        self.t_ser = k.toks(2)
        self.ident, self.t_id = make_ident(k, F32)
        self.n = 0


def front_end(k, fb, x_src, A_of, B_of, t_vec, hT, t_hT, f32T_cb=None, tiles=None, rows_of=tile_rows, tile_done=None, t_src=None):
    for i in (tiles if tiles is not None else range(NT)):
        rows = rows_of(i)
        s = fb.n % fb.nbuf
        fb.n += 1
        xt = fb.xt[s]
        t_x = fb.t_xt[s]
        k.dma("sp", [(xt[:rows, 0:2048], x_src[i * 128:i * 128 + rows, 0:2048]),
                     (xt[:rows, 2048:4096], x_src[i * 128:i * 128 + rows, 2048:4096])], t_x,
              reads=[t_src] if t_src is not None else (), writes=[t_x])
        k.op("act", lambda a: a.activation(fb.junk[:rows, :], xt[:rows, :], AF.Square, accum_out=fb.ss[:rows, i:i + 1]),
             reads=[t_x], writes=[fb.t_junk, fb.t_ss[i]])
        k.op("act", lambda a: a.activation(fb.den[:rows, i:i + 1], fb.ss[:rows, i:i + 1], AF.Sqrt, bias=EPS, scale=1.0 / D),
             reads=[fb.t_ss[i]], writes=[fb.t_ss[i]])
        k.op("dve", lambda v: v.reciprocal(fb.rstd[:rows, i:i + 1], fb.den[:rows, i:i + 1]),
             reads=[fb.t_ss[i]], writes=[fb.t_ss[i]])
        k.op("act", lambda a: a.activation(xt[:rows, :], xt[:rows, :], AF.Copy, scale=fb.rstd[:rows, i:i + 1]),
             reads=[fb.t_ss[i]], writes=[t_x])
        A = A_of(i)
        B = B_of(i)
        for g in range(8):
            ps = fb.tp[g % 2]
            t_ps = fb.t_tp[g % 2]
            for j in range(4):
                kc = g * 4 + j
                k.op("pe", lambda p, kc=kc, j=j: p.transpose(ps[:, j, :rows], xt[:rows, kc * 128:(kc + 1) * 128],
                                                              fb.ident[:rows, :rows]),
                     reads=[t_x, fb.t_id], writes=[t_ps] if j == 0 else (), acc=[t_ps] if j else (), inc=(j == 3))
            for j in range(4):
                kc = g * 4 + j
                dst = hT[:, kc, i * 128:i * 128 + rows]
                if j % 2 == 0:
                    k.op("dve", lambda v, kc=kc, j=j, dst=dst: v.tensor_scalar(dst, ps[:, j, :rows], A[:, kc:kc + 1], B[:, kc:kc + 1],
                                                                               ALU.mult, ALU.add),
                         reads=[t_ps, t_vec], writes=[t_hT[i], fb.t_ser[g % 2]])
                else:
                    k.op("act", lambda a, kc=kc, j=j, dst=dst: a.activation(dst, ps[:, j, :rows], AF.Identity,
                                                                            bias=B[:, kc:kc + 1], scale=A[:, kc:kc + 1]),
                         reads=[t_ps, t_vec], writes=[t_hT[i], fb.t_ser[g % 2]])
                if f32T_cb is not None:
                    f32T_cb(i, kc, ps[:, j, :rows], t_ps, A, B, fb.t_ser[g % 2])
        if tile_done is not None:
            tile_done(i)


def make_AB(k, vec, t_vec, AB, t_AB, gi, sci, shi, oi):
    k.op("dve", lambda v: v.tensor_scalar(AB[:, oi, :], vec[:, sci, :], 1.0, None, ALU.add), reads=[t_vec], writes=[t_AB])
    k.op("dve", lambda v: v.tensor_tensor(AB[:, oi, :], AB[:, oi, :], vec[:, gi, :], ALU.mult), reads=[t_vec, t_AB], writes=[t_AB])
    k.op("dve", lambda v: v.tensor_copy(AB[:, oi + 1, :], vec[:, shi, :]), reads=[t_vec, t_AB], writes=[t_AB])


NBLK = [(0, 512), (512, 512), (1024, 64)]


class GemmBufs:
    def __init__(self, k, cb=256, nwb=3):
        self.cb = cb
        self.nwb = nwb
        self.wb = [k.sb([128, KC, cb], BF16) for _ in range(nwb)]
        self.t_wb = k.toks(nwb)
        self.ps = [[k.ps([128, 512], F32) for _ in range(3)] for _ in range(2)]
        self.t_ps = [k.toks(3) for _ in range(2)]
        self.osb = [k.sb([128, NTOK], F32) for _ in range(2)]
        self.t_osb = k.toks(2)
        self.nw = 0
        self.nc_ = 0


def gemm_f(k, gb, W, ncols, hT, t_hT, outT, t_out, epilogue=None):
    cb = gb.cb
    nblocks = (ncols + cb - 1) // cb
    for b in range(nblocks):
        c0 = b * cb
        cw = min(cb, ncols - c0)
        s = gb.nw % gb.nwb
        gb.nw += 1
        wb = gb.wb[s]
        t_w = gb.t_wb[s]
        src = W[:, c0:c0 + cw].rearrange("(kc p) n -> p kc n", p=128)
        k.dma("pool", [(wb[:, 0:16, :cw], src[:, 0:16, :]), (wb[:, 16:32, :cw], src[:, 16:32, :])], t_w, writes=[t_w])
        for sub in range((cw + 127) // 128):
            m0 = sub * 128
            m = min(128, cw - m0)
            pi = gb.nc_ % 2
            gb.nc_ += 1
            for nt, (t0, n) in enumerate(NBLK):
                ps = gb.ps[pi][nt]
                t_p = gb.t_ps[pi][nt]
                rd = [t_w] + [t_hT[i] for i in range(t0 // 128, (t0 + n + 127) // 128)]
                for kc in range(KC):
                    k.op("pe", lambda p, kc=kc, ps=ps, t0=t0, n=n, m0=m0, m=m: p.matmul(
                        ps[:m, :n], wb[:, kc, m0:m0 + m], hT[:, kc, t0:t0 + n], start=(kc == 0), stop=(kc == KC - 1)),
                        reads=rd if kc == 0 else [t_w], writes=[t_p] if kc == 0 else (), acc=[t_p] if kc else (),
                        inc=(kc == KC - 1))
            if epilogue is not None:
                epilogue(c0 + m0, m, gb.ps[pi], gb.t_ps[pi])
                continue
            osb = gb.osb[pi]
            t_o = gb.t_osb[pi]
            for nt, (t0, n) in enumerate(NBLK):
                ps = gb.ps[pi][nt]
                t_p = gb.t_ps[pi][nt]
                if nt == 1:
                    k.op("dve", lambda v, ps=ps, t0=t0, n=n: v.tensor_copy(osb[:m, t0:t0 + n], ps[:m, :n]),
                         reads=[t_p], writes=[t_o])
                else:
                    k.op("act", lambda a, ps=ps, t0=t0, n=n: a.activation(osb[:m, t0:t0 + n], ps[:m, :n], AF.Copy),
                         reads=[t_p], writes=[t_o])
            k.dma("sp", [(outT[c0 + m0:c0 + m0 + m, :], osb[:m, :])], t_o, reads=[t_o], acc=[t_out])


def build_inproj(ncols):
    k = K()
    x = k.inp("x", [NTOK, D])
    vec_d = k.inp("vec", [128, 8, KC])
    W = k.inp("w", [D, ncols])
    outT = k.outp("projT", [ncols, NTOK])
    vec = k.sb([128, 8, KC], F32); t_vec = k.tok()
    AB = k.sb([128, 4, KC], F32); t_AB = k.tok()
    k.dma("sp", [(vec[:], vec_d)], t_vec, writes=[t_vec])
    make_AB(k, vec, t_vec, AB, t_AB, 0, 1, 2, 0)
    make_AB(k, vec, t_vec, AB, t_AB, 0, 3, 4, 2)
    hT = k.sb([128, KC, NTOK], BF16)
    t_hT = k.toks(NT)
    fb = FrontBufs(k)
    front_end(k, fb, x, lambda i: AB[:, 0 if i < 2 else 2, :], lambda i: AB[:, 1 if i < 2 else 3, :], t_AB, hT, t_hT)
    gb = GemmBufs(k)
    t_out = k.tok()
    gemm_f(k, gb, W, ncols, hT, t_hT, outT, t_out)
    k.finish([t_out])
    return k


def build_final_norm():
    k = K()
    x = k.inp("x", [NTOK, D])
    g_d = k.inp("g", [128, D])
    out = k.outp("o", [NTOK, D])
    g = k.sb([128, D], F32); t_g = k.tok()
    k.dma("sp", [(g[:], g_d)], t_g, writes=[t_g])
    xt = [k.sb([128, D], F32) for _ in range(2)]; t_x = k.toks(2)
    junk = k.sb([128, D], BF16); t_j = k.tok()
    st = k.sb([128, NT, 3], F32); t_s = k.toks(NT)
    t_out = k.tok()
    for i in range(NT):
        rows = tile_rows(i)
        s = i % 2
        r0 = i * 128
        k.dma("sp", [(xt[s][:rows, 0:2048], x[r0:r0 + rows, 0:2048]), (xt[s][:rows, 2048:4096], x[r0:r0 + rows, 2048:4096])],
              t_x[s], writes=[t_x[s]])
        k.op("act", lambda a: a.activation(junk[:rows, :], xt[s][:rows, :], AF.Square, accum_out=st[:rows, i, 0:1]),
             reads=[t_x[s]], writes=[t_j, t_s[i]])
        k.op("act", lambda a: a.activation(st[:rows, i, 1:2], st[:rows, i, 0:1], AF.Sqrt, bias=EPS, scale=1.0 / D),
             reads=[t_s[i]], writes=[t_s[i]])
        k.op("dve", lambda v: v.reciprocal(st[:rows, i, 2:3], st[:rows, i, 1:2]), reads=[t_s[i]], writes=[t_s[i]])
        k.op("act", lambda a: a.activation(xt[s][:rows, :], xt[s][:rows, :], AF.Copy, scale=st[:rows, i, 2:3]),
             reads=[t_s[i]], writes=[t_x[s]])
        k.op("dve", lambda v: v.tensor_tensor(xt[s][:rows, :], xt[s][:rows, :], g[:rows, :], ALU.mult),
             reads=[t_g], writes=[t_x[s]])
        k.dma("sp", [(out[r0:r0 + rows, :], xt[s][:rows, :])], t_x[s], reads=[t_x[s]], acc=[t_out])
    k.finish([t_out])
    return k


def build_outproj():
    k = K()
    yT_d = k.inp("yT", [D, NTOK])
    x = k.inp("x", [NTOK, D])
    g_d = k.inp("g2", [128, 2, D])
    W = k.inp("w", [D, D])
    out = k.outp("x1", [NTOK, D])
    yT = k.sb([128, KC, NTOK], BF16); t_y = k.tok()
    src = yT_d.rearrange("(kc p) t -> p kc t", p=128)
    k.dma("pool", [(yT[:, 8 * j:8 * j + 8, :], src[:, 8 * j:8 * j + 8, :]) for j in range(4)], t_y, writes=[t_y])
    g2 = k.sb([128, 2, D], F32); t_g = k.tok()
    k.dma("sp", [(g2[:, c, :], g_d[:, c, :]) for c in range(2)], t_g, writes=[t_g])
    wb = [k.sb([128, KC, 512], BF16) for _ in range(2)]; t_w = k.toks(2)
    ps = [k.ps([128, 512], F32) for _ in range(2)]; t_p = k.toks(2)
    xt = [k.sb([128, 512], F32) for _ in range(2)]; t_x = k.toks(2)
    tm = [k.sb([128, 512], F32) for _ in range(2)]; t_t = k.toks(2)
    t_out = k.tok()
    it = 0
    for cg in range(8):
        c0 = cg * 512
        s = cg % 2
        wsrc = W[:, c0:c0 + 512].rearrange("(kc p) n -> p kc n", p=128)
        k.dma("pool", [(wb[s][:, 0:16, :], wsrc[:, 0:16, :]), (wb[s][:, 16:32, :], wsrc[:, 16:32, :])], t_w[s], writes=[t_w[s]])
        for i in range(NT):
            rows = tile_rows(i)
            r0 = i * 128
            b = it % 2
            it += 1
            cls = 0 if i < 2 else 1
            k.dma("sp", [(xt[b][:rows, :], x[r0:r0 + rows, c0:c0 + 512])], t_x[b], writes=[t_x[b]])
            for kc in range(KC):
                k.op("pe", lambda p, kc=kc: p.matmul(ps[b][:rows, :], yT[:, kc, r0:r0 + rows], wb[s][:, kc, :],
                                                      start=(kc == 0), stop=(kc == KC - 1)),
                     reads=[t_y, t_w[s]] if kc == 0 else (), writes=[t_p[b]] if kc == 0 else (), acc=[t_p[b]] if kc else (),
                     inc=(kc == KC - 1))
            k.op("dve", lambda v: v.tensor_tensor(tm[b][:rows, :], ps[b][:rows, :], g2[:rows, cls, c0:c0 + 512], ALU.mult),
                 reads=[t_p[b], t_g], writes=[t_t[b]])
            k.op("dve", lambda v: v.tensor_tensor(tm[b][:rows, :], tm[b][:rows, :], xt[b][:rows, :], ALU.add),
                 reads=[t_x[b]], writes=[t_t[b]])
            k.dma("sp", [(out[r0:r0 + rows, c0:c0 + 512], tm[b][:rows, :])], t_t[b], reads=[t_t[b]], acc=[t_out])
    k.finish([t_out])
    return k


def emit_combine(k, parts, x1, g5, t_g5, out, t_out, nparts=8, bufs=None, nld=1):
    if bufs is not None:
        acc, t_a = bufs
    else:
        acc = [k.sb([128, D], F32) for _ in range(2)]; t_a = k.toks(2)
    ld = [k.sb([128, D], F32) for _ in range(nld)]; t_l = k.toks(nld)
    n = 0
    for i in range(NT):
        rows = tile_rows(i)
        r0 = i * 128
        a = i % 2
        cls = 0 if i < 2 else 1
        k.dma("sp", [(acc[a][:rows, :], parts[0, r0:r0 + rows, :])], t_a[a], writes=[t_a[a]])
        for c in range(1, nparts + 1):
            b = n % nld
            n += 1
            srcap = parts[c, r0:r0 + rows, :] if c < nparts else x1[r0:r0 + rows, :]
            k.dma("sp", [(ld[b][:rows, :], srcap)], t_l[b], writes=[t_l[b]])
            if c == nparts:
                k.op("dve", lambda v: v.tensor_tensor(acc[a][:rows, :], acc[a][:rows, :], g5[:rows, cls, :], ALU.mult),
                     reads=[t_g5], writes=[t_a[a]])
            k.op("dve", lambda v: v.tensor_tensor(acc[a][:rows, :], acc[a][:rows, :], ld[b][:rows, :], ALU.add),
                 reads=[t_l[b]], writes=[t_a[a]])
        k.dma("sp", [(out[r0:r0 + rows, :], acc[a][:rows, :])], t_a[a], reads=[t_a[a]], acc=[t_out])


NEXP = 8
MOE_BLOCKS = [(i * 512, 512) for i in range(8)] + [(4096, 256)]


def build_moe(nblocks=len(MOE_BLOCKS), stage=3):
    k = K()
    x1 = k.inp("x1", [TSEQ, D])
    vec_d = k.inp("vec", [128, 8, KC])
    rw_d = k.inp("rw", [D, 32])
    rb_d = k.inp("rb", [128, 32])
    wgu = k.inp("wgu", [NEXP, D, 1024])
    bgu_d = k.inp("bgu", [128, NEXP, 8])
    wd = k.inp("wd", [NEXP, 512, D])
    bd = k.inp("bd", [NEXP, D])
    out = k.outp("ypart", [TSEQ, D])

    vec = k.sb([128, 8, KC], F32); t_vec = k.tok()
    AB = k.sb([128, 4, KC], F32); t_AB = k.tok()
    k.dma("sp", [(vec[:], vec_d)], t_vec, writes=[t_vec])
    for c in range(2):
        make_AB(k, vec, t_vec, AB, t_AB, 0, 1 + 2 * c, 2 + 2 * c, 2 * c)
    rw = k.sb([128, KC, 32], F32); t_rw = k.tok()
    k.dma("sp", [(rw[:], rw_d.rearrange("(kc p) e -> p kc e", p=128))], t_rw, writes=[t_rw])
    rb = k.sb([128, 32], F32)
    bgu = k.sb([128, NEXP, 8], F32)
    ones = k.sb([1, 128], F32)
    t_c = k.tok()
    k.dma("sp", [(rb[:], rb_d), (bgu[:], bgu_d)], t_c, writes=[t_c])
    t_one = k.tok()
    k.op("dve", lambda v: v.memset(ones[:], 1.0), writes=[t_one])

    wgu_bf = k.dram("wgu_bf", [NEXP, 4, 128, KC, 256], BF16).ap()
    wd_bf = k.dram("wd_bf", [NEXP, 8, 128, 4, 512], BF16).ap()
    t_cgu = k.toks(NEXP)
    t_cwd = k.toks(NEXP)
    for j in range(NEXP):
        for cc in range(4):
            src = wgu[j, :, cc * 256:(cc + 1) * 256].rearrange("(kc p) n -> p kc n", p=128)
            k.dma("pool", [(wgu_bf[j, cc, :, 0:16, :], src[:, 0:16, :]), (wgu_bf[j, cc, :, 16:32, :], src[:, 16:32, :])], t_cgu[j], acc=[t_cgu[j]])
    for j in range(NEXP):
        k.dma("pool", [(wd_bf[j, cg], wd[j, :, cg * 512:(cg + 1) * 512].rearrange("(fc p) n -> p fc n", p=128)) for cg in range(8)],
              t_cwd[j], acc=[t_cwd[j]])

    hT = k.sb([128, KC, 512], BF16); t_hT = k.toks(4)
    fb = FrontBufs(k, nbuf=1)
    f32s = k.sb([128, KC, 128], F32); t_f32 = k.toks(KC)
    lg_ps = k.ps([128, 32], F32); t_lg = k.tok()
    lg = k.sb([128, 4, 32], F32)
    m8 = k.sb([128, 4, 8], F32)
    sm = k.sb([128, 4, 4], F32)
    ex = k.sb([128, 4, 32], F32)
    G = k.sb([128, 4, 32], F32)
    t_G = k.toks(4)
    wb = [k.sb([128, KC, 256], BF16) for _ in range(2)]; t_wb = k.toks(2)
    ups = [k.ps([128, 512], F32) for _ in range(2)]; t_up = k.toks(2)
    g1 = k.sb([128, 512], F32); sg = k.sb([128, 512], F32); l1 = k.sb([128, 512], F32)
    t_g1 = k.tok(); t_sg = k.tok(); t_l1 = k.tok()
    actT = k.sb([128, NEXP, 4, 512], BF16); t_act = [k.toks(4) for _ in range(NEXP)]
    wdb = [k.sb([128, 4, 4, 512], BF16) for _ in range(2)]; t_wd = k.toks(2)
    bdc = [k.sb([NEXP, 512], F32) for _ in range(2)]; t_bdc = k.toks(2)
    GT = k.sb([NEXP, 4, 128], F32); t_GT = k.toks(4)
    gt_ps = k.ps([NEXP, 128], F32); t_gtp = k.tok()
    dps = [k.ps([128, 512], F32) for _ in range(2)]; t_dp = k.toks(2)
    acc = k.sb([128, 4, 512], F32); t_acc = k.toks(4)
    t_out = k.tok()
    nwb = 0
    nwd = 0
    ndp = 0

    for blk in range(nblocks):
        tok0, bn = MOE_BLOCKS[blk]
        nti = bn // 128

        def f32_cb(i, kc, ps_ap, t_ps, A, B, t_ser):
            k.op("dve", lambda v: v.tensor_scalar(f32s[:, kc, :], ps_ap, A[:, kc:kc + 1], B[:, kc:kc + 1], ALU.mult, ALU.add),
                 reads=[t_ps, t_AB], writes=[t_f32[kc], t_ser])
            k.op("pe", lambda p: p.matmul(lg_ps[:, :], f32s[:, kc, :], rw[:, kc, :], start=(kc == 0), stop=(kc == KC - 1)),
                 reads=[t_f32[kc], t_rw], writes=[t_lg] if kc == 0 else (), acc=[t_lg] if kc else (), inc=(kc == KC - 1))

        def tile_done(i):
            tg = t_G[i]
            k.op("dve", lambda v: v.tensor_tensor(lg[:, i, :], lg_ps[:, :], rb[:, :], ALU.add), reads=[t_lg, t_c], writes=[tg])
            k.op("dve", lambda v: v.max(m8[:, i, :], lg[:, i, :]), reads=[tg], writes=[tg])
            k.op("dve", lambda v: v.tensor_scalar(sm[:, i, 0:1], m8[:, i, 0:1], -1.0, None, ALU.mult), reads=[tg], writes=[tg])
            k.op("act", lambda a: a.activation(ex[:, i, :], lg[:, i, :], AF.Exp, bias=sm[:, i, 0:1], scale=1.0), reads=[tg], writes=[tg])
            k.op("dve", lambda v: v.tensor_scalar(G[:, i, :], lg[:, i, :], m8[:, i, 3:4], None, ALU.is_ge), reads=[tg], writes=[tg])
            k.op("dve", lambda v: v.tensor_tensor(ex[:, i, :], ex[:, i, :], G[:, i, :], ALU.mult), reads=[tg], writes=[tg])
            k.op("dve", lambda v: v.reduce_sum(sm[:, i, 1:2], ex[:, i, :], axis=AX.X), reads=[tg], writes=[tg])
            k.op("dve", lambda v: v.reciprocal(sm[:, i, 2:3], sm[:, i, 1:2]), reads=[tg], writes=[tg])
            k.op("dve", lambda v: v.tensor_scalar(G[:, i, :], ex[:, i, :], sm[:, i, 2:3], None, ALU.mult), reads=[tg], writes=[tg])
            k.op("pe", lambda p: p.transpose(gt_ps[:, :], G[:, i, 0:NEXP], fb.ident[:, :]), reads=[tg, fb.t_id], writes=[t_gtp])
            k.op("act", lambda a: a.activation(GT[:, i, :], gt_ps[:, :], AF.Copy), reads=[t_gtp], writes=[t_GT[i]])

        def cls(i):
            return 0 if (tok0 // 128 + i) < 2 else 1
        front_end(k, fb, x1[tok0:tok0 + bn, :], lambda i: AB[:, 2 * cls(i), :], lambda i: AB[:, 2 * cls(i) + 1, :],
                  t_AB, hT, t_hT, f32T_cb=f32_cb, tiles=range(nti), rows_of=lambda i: 128, tile_done=tile_done)
        if stage == 1:
            for i in range(nti):
                k.dma("sp", [(out[tok0 + i * 128:tok0 + i * 128 + 128, 0:32], G[:, i, :])], t_G[i], reads=[t_G[i]], acc=[t_out])
            continue
        rd_h = t_hT[:nti]
        for j in range(NEXP):
            for cc in range(4):
                s = nwb % 2
                nwb += 1
                k.dma("sp", [(wb[s][:, :, :], wgu_bf[j, cc])], t_wb[s], reads=[t_cgu[j]], writes=[t_wb[s]])
                for h in range(2):
                    for kc in range(KC):
                        k.op("pe", lambda p, kc=kc, h=h: p.matmul(ups[h][:, :bn], wb[s][:, kc, h * 128:(h + 1) * 128], hT[:, kc, :bn],
                                                                   start=(kc == 0), stop=(kc == KC - 1)),
                             reads=[t_wb[s]] + rd_h if kc == 0 else (), writes=[t_up[h]] if kc == 0 else (), acc=[t_up[h]] if kc else (),
                             inc=(kc == KC - 1))
                k.op("dve", lambda v: v.tensor_scalar(g1[:, :bn], ups[0][:, :bn], bgu[:, j, 2 * cc:2 * cc + 1], 7.0, ALU.add, ALU.min),
                     reads=[t_up[0], t_c], writes=[t_g1])
                k.op("act", lambda a: a.activation(sg[:, :bn], g1[:, :bn], AF.Sigmoid, scale=1.702), reads=[t_g1], writes=[t_sg])
                k.op("dve", lambda v: v.tensor_scalar(l1[:, :bn], ups[1][:, :bn], bgu[:, j, 2 * cc + 1:2 * cc + 2], 7.0, ALU.add, ALU.min),
                     reads=[t_up[1], t_c], writes=[t_l1])
                k.op("dve", lambda v: v.tensor_scalar(l1[:, :bn], l1[:, :bn], -7.0, 1.0, ALU.max, ALU.add), reads=[t_l1], writes=[t_l1])
                k.op("dve", lambda v: v.tensor_tensor(g1[:, :bn], g1[:, :bn], sg[:, :bn], ALU.mult), reads=[t_sg], writes=[t_g1])
                k.op("dve", lambda v: v.tensor_tensor(actT[:, j, cc, :bn], g1[:, :bn], l1[:, :bn], ALU.mult), reads=[t_g1, t_l1], writes=[t_act[j][cc]])
        for cg in range(8):
            c0 = cg * 512
            k.dma("sp", [(bdc[cg % 2][:, :], bd[:, c0:c0 + 512])], t_bdc[cg % 2], writes=[t_bdc[cg % 2]])
            for eh in range(NEXP // 4):
                s = nwd % 2
                nwd += 1
                k.dma("sp", [(wdb[s][:, jl, :, :], wd_bf[4 * eh + jl, cg]) for jl in range(4)], t_wd[s],
                      reads=[t_cwd[4 * eh + jl] for jl in range(4)], writes=[t_wd[s]])
                for i in range(nti):
                    for jl in range(4):
                        j = 4 * eh + jl
                        d = ndp % 2
                        ndp += 1
                        for fc in range(4):
                            k.op("pe", lambda p, fc=fc: p.matmul(dps[d][:, :], actT[:, j, fc, i * 128:(i + 1) * 128], wdb[s][:, jl, fc, :],
                                                                  start=(fc == 0), stop=(fc == 3)),
                                 reads=[t_wd[s]] + t_act[j] if fc == 0 else (), writes=[t_dp[d]] if fc == 0 else (), acc=[t_dp[d]] if fc else (),
                                 inc=(fc == 3))
                        if j == 0:
                            k.op("dve", lambda v: v.tensor_scalar(acc[:, i, :], dps[d][:], G[:, i, 0:1], None, ALU.mult),
                                 reads=[t_dp[d], t_G[i]], writes=[t_acc[i]])
                        else:
                            k.op("dve", lambda v: v.scalar_tensor_tensor(acc[:, i, :], dps[d][:], G[:, i, j:j + 1], acc[:, i, :], ALU.mult, ALU.add),
                                 reads=[t_dp[d], t_G[i]], writes=[t_acc[i]])
            for i in range(nti):
                d = ndp % 2
                ndp += 1
                k.op("pe", lambda p: p.matmul(dps[d][:, :], GT[:, i, :], bdc[cg % 2][:, :], start=True, stop=True),
                     reads=[t_GT[i], t_bdc[cg % 2]], writes=[t_dp[d]])
                k.op("dve", lambda v: v.tensor_tensor(acc[:, i, :], acc[:, i, :], dps[d][:], ALU.add), reads=[t_dp[d]], writes=[t_acc[i]])
                r0 = tok0 + i * 128
                k.dma("sp", [(out[r0:r0 + 128, c0:c0 + 512], acc[:, i, :])], t_acc[i], reads=[t_acc[i]], acc=[t_out])
    k.finish([t_out])
    return k


T = TSEQ
SEGS = [(0, NCTX), (NCTX, TSEQ)]
TBLK = [(i * 512, min(512, TSEQ - i * 512)) for i in range(9)]


def rev(ap2d, lo, hi):
    return ap2d[:, hi - 1::-1] if lo == 0 else ap2d[:, hi - 1:lo - 1:-1]


class Pool9:
    def __init__(self, k, n=9):
        self.t = [k.sb([128, T], F32) for _ in range(n)]
        self.k = k.toks(n)


def shifted_mac(k, out, t_out, src, t_src, wcol, off, extra_reads=()):
    for (lo, hi) in SEGS:
        o_lo, o_hi = max(lo, lo - off), min(hi, hi - off)
        k.op("dve", lambda v: v.scalar_tensor_tensor(out[:, o_lo:o_hi], src[:, o_lo + off:o_hi + off], wcol, out[:, o_lo:o_hi],
                                                     ALU.mult, ALU.add),
             reads=[t_src] + list(extra_reads), writes=[t_out])


def emit_lru(k, P, PS, tPS, prm, t_prm, gw, t_gw, xaT, gateT, yT_out, t_yout):
    S, tS = P.t, P.k
    spn = k.sb([128, 4, 4], F32)
    t_sp = k.tok()
    k.op("act", lambda a: a.activation(spn[:, :, 0:2], prm[:, :, 9:11], AF.Exp, scale=-1.0), reads=[t_prm], writes=[t_sp])
    k.op("act", lambda a: a.activation(spn[:, :, 0:2], spn[:, :, 0:2], AF.Ln, bias=1.0), writes=[t_sp])
    k.op("dve", lambda v: v.tensor_scalar(spn[:, :, 2:4], spn[:, :, 0:2], -16.0, None, ALU.mult), writes=[t_sp])
    k.op("dve", lambda v: v.tensor_scalar(spn[:, :, 0:2], spn[:, :, 0:2], -8.0, None, ALU.mult), writes=[t_sp])
    ps = PS[0:2]
    t_ps = tPS[0:2]
    npi = 0
    for n in range(4):
        pr = prm[:, n, :]
        k.dma("sp", [(S[0][:, :], xaT[n * 128:(n + 1) * 128, :])], tS[0], writes=[tS[0]])
        k.dma("sp", [(S[7][:, :], gateT[n * 128:(n + 1) * 128, :])], tS[7], writes=[tS[7]])
        k.op("dve", lambda v: v.tensor_scalar(S[1][:, :], S[0][:, :], pr[:, 2:3], pr[:, 4:5], ALU.mult, ALU.add),
             reads=[tS[0], t_prm], writes=[tS[1]])
        shifted_mac(k, S[1], tS[1], S[0], tS[0], pr[:, 0:1], -2)
        shifted_mac(k, S[1], tS[1], S[0], tS[0], pr[:, 1:2], -1)
        shifted_mac(k, S[1], tS[1], S[0], tS[0], pr[:, 3:4], +1)
        for d in range(2):
            hdst = S[5 + d]
            t_h = tS[5 + d]
            for gi, dst in ((0, 2), (1, 3)):
                wi = d * 2 + gi
                for (t0, nn) in TBLK:
                    pb = npi % 2
                    npi += 1
                    k.op("pe", lambda p: p.matmul(ps[pb][:, :nn], gw[:, n, wi, :], S[1][:, t0:t0 + nn], start=True, stop=True),
                         reads=[tS[1], t_gw], writes=[t_ps[pb]])
                    k.op("act", lambda a: a.activation(S[dst][:, t0:t0 + nn], ps[pb][:, :nn], AF.Sigmoid, bias=pr[:, 5 + wi:6 + wi]),
                         reads=[t_ps[pb], t_prm], writes=[tS[dst]])
            k.op("act", lambda a: a.activation(S[4][:, :], S[2][:, :], AF.Exp, scale=spn[:, n, d:d + 1]), reads=[tS[2], t_sp], writes=[tS[4]])
            k.op("act", lambda a: a.activation(S[2][:, :], S[2][:, :], AF.Exp, scale=spn[:, n, 2 + d:3 + d]), reads=[t_sp], writes=[tS[2]])
            k.op("act", lambda a: a.activation(S[2][:, :], S[2][:, :], AF.Sqrt, bias=1.0, scale=-1.0), writes=[tS[2]])
            k.op("dve", lambda v: v.tensor_tensor(S[3][:, :], S[3][:, :], S[1][:, :], ALU.mult), reads=[tS[1]], writes=[tS[3]])
            k.op("dve", lambda v: v.tensor_tensor(S[3][:, :], S[3][:, :], S[2][:, :], ALU.mult), reads=[tS[2]], writes=[tS[3]])
            if d == 0:
                k.op("dve", lambda v: v.tensor_tensor_scan(hdst[:, :], S[4][:, :], S[3][:, :], 0.0, ALU.mult, ALU.add),
                     reads=[tS[4], tS[3]], writes=[t_h])
            else:
                k.op("dve", lambda v: v.tensor_tensor_scan(rev(hdst, 0, NCTX), rev(S[4], 0, NCTX), rev(S[3], 0, NCTX), 0.0, ALU.mult, ALU.add),
                     reads=[tS[4], tS[3]], writes=[t_h])
                k.op("dve", lambda v: v.tensor_tensor_scan(rev(hdst, NCTX, T), rev(S[4], NCTX, T), rev(S[3], NCTX, T), hdst[:, 0:1],
                                                           ALU.mult, ALU.add),
                     reads=[tS[4], tS[3]], writes=[t_h])
        k.op("dve", lambda v: v.tensor_tensor(S[5][:, :], S[5][:, :], S[6][:, :], ALU.add), reads=[tS[6]], writes=[tS[5]])
        emit_gelu(k, S[7], tS[7], S[8], tS[8])
        k.op("dve", lambda v: v.tensor_tensor(S[5][:, :], S[5][:, :], S[8][:, :], ALU.mult), reads=[tS[8]], writes=[tS[5]])
        k.dma("sp", [(yT_out[n * 128:(n + 1) * 128, :], S[5][:, :])], tS[5], reads=[tS[5]], acc=[t_yout])


def emit_gelu(k, x, t_x, o, t_o):
    k.op("act", lambda a: a.activation(o[:, :], x[:, :], AF.Square), reads=[t_x], writes=[t_o])
    k.op("dve", lambda v: v.tensor_scalar(o[:, :], o[:, :], 0.044715, 1.0, ALU.mult, ALU.add), writes=[t_o])
    k.op("dve", lambda v: v.tensor_tensor(o[:, :], o[:, :], x[:, :], ALU.mult), reads=[t_x], writes=[t_o])
    k.op("act", lambda a: a.activation(o[:, :], o[:, :], AF.Tanh, scale=0.7978845608028654), writes=[t_o])
    k.op("dve", lambda v: v.tensor_scalar(o[:, :], o[:, :], 1.0, 0.5, ALU.add, ALU.mult), writes=[t_o])
    k.op("dve", lambda v: v.tensor_tensor(o[:, :], o[:, :], x[:, :], ALU.mult), reads=[t_x], writes=[t_o])


G8 = 8
CH = 128
YB = 32


def token_shift(k, src, t_src, dst, t_dst, onem_mu, half_mu, t_prm, rows=128):
    k.op("dve", lambda v: v.tensor_scalar(dst[:rows, :], src[:rows, :], onem_mu, None, ALU.mult), reads=[t_src, t_prm], writes=[t_dst])
    for off in (-1, 1):
        for (lo, hi) in SEGS:
            o_lo, o_hi = max(lo, lo - off), min(hi, hi - off)
            k.op("dve", lambda v: v.scalar_tensor_tensor(dst[:rows, o_lo:o_hi], src[:rows, o_lo + off:o_hi + off], half_mu,
                                                         dst[:rows, o_lo:o_hi], ALU.mult, ALU.add),
                 reads=[t_src], writes=[t_dst])


def rev_copy(k, src, t_src, dst, t_dst):
    for (lo, hi) in SEGS:
        k.op("act", lambda a: a.activation(dst[:, lo:hi], rev(src, lo, hi), AF.Copy), reads=[t_src], writes=[t_dst])


def emit_rwkv(k, P, PS, tPS, cst, t_cst, rkvT, loT, prm, t_prm, mulo, t_mulo, w2, a2, g2, t_wts, yT_out, t_yout, nsteps=T):
    S, tS = P.t, P.k
    BO = cst[:, 0:128]
    SI = cst[:, 128:192]
    HM = cst[:, 192:194]
    SC = {q: k.dram("sc_" + q, [G8, 128, T], F32).ap() for q in "RWKVAB"}
    tSC = {q: k.tok() for q in "RWKVAB"}
    LOA = k.dram("loa", [6, 128, T], F32).ap(); t_loa = k.tok()
    BON = k.dram("bon", [4, 128, T], F32).ap(); t_bon = k.tok()
    GS = k.dram("gs", [4, 128, T], F32).ap(); t_gs = k.tok()
    YSC = k.dram("ysc", [G8, 2, 64, T], F32).ap(); t_ysc = k.tok()
    dp = k.sb([128, 4, 8], F32); t_dp = k.tok()
    k.op("dve", lambda v: v.tensor_scalar(dp[:, :, 0:3], prm[:, :, 0:3], -1.0, 1.0, ALU.mult, ALU.add), reads=[t_prm], writes=[t_dp])
    k.op("dve", lambda v: v.tensor_scalar(dp[:, :, 3:6], prm[:, :, 0:3], 0.5, None, ALU.mult), reads=[t_prm], writes=[t_dp])
    k.op("dve", lambda v: v.tensor_scalar(dp[:, :, 6:7], prm[:, :, 4:5], -1.0, 1.0, ALU.mult, ALU.add), reads=[t_prm], writes=[t_dp])
    dl = k.sb([128, 2, 6], F32); t_dl = k.tok()
    k.op("dve", lambda v: v.tensor_scalar(dl[:, 0, :], mulo[:, :], -1.0, 1.0, ALU.mult, ALU.add), reads=[t_mulo], writes=[t_dl])
    k.op("dve", lambda v: v.tensor_scalar(dl[:, 1, :], mulo[:, :], 0.5, None, ALU.mult), reads=[t_mulo], writes=[t_dl])

    for c in range(6):
        rows = 96 if c == 5 else 128
        k.dma("sp", [(S[0][:rows, :], loT[c * 128:c * 128 + rows, :])], tS[0], writes=[tS[0]])
        token_shift(k, S[0], tS[0], S[1], tS[1], dl[:rows, 0, c:c + 1], dl[:rows, 1, c:c + 1], t_dl, rows=rows)
        if c == 0:
            k.op("act", lambda a: a.activation(S[1][:rows, :], S[1][:rows, :], AF.Tanh), writes=[tS[1]])
        elif c >= 2:
            k.op("act", lambda a: a.activation(S[1][:rows, :], S[1][:rows, :], AF.Sigmoid), writes=[tS[1]])
        k.dma("sp", [(LOA[c, :rows, :], S[1][:rows, :])], tS[1], reads=[tS[1]], acc=[t_loa])

    lb = [k.sb([128, 6, 512], F32) for _ in range(2)]; t_lb = k.toks(2)
    nlb = 0
    npsA = 0

    def bo_apply(src, t_src, fn):
        nonlocal npsA
        for (t0, nn) in TBLK:
            pb = npsA % 2
            npsA += 1
            k.op("pe", lambda p: p.matmul(PS[pb][:, :nn], BO, src[:, t0:t0 + nn], start=True, stop=True),
                 reads=[t_src, t_cst], writes=[tPS[pb]])
            fn(t0, nn, PS[pb], tPS[pb])

    def store_scan(q, g, src, t_src):
        d = g // 4
        if d == 0:
            k.dma("sp", [(SC[q][g, :, :], src[:, :])], t_src, reads=[t_src], acc=[tSC[q]])
        else:
            rev_copy(k, src, t_src, S[2], tS[2])
            k.dma("sp", [(SC[q][g, :, :], S[2][:, :])], tS[2], reads=[tS[2]], acc=[tSC[q]])

    for p in range(4):
        pr = prm[:, p, :]
        for qi, dst in ((0, 3), (1, 4), (2, 5)):
            k.dma("sp", [(S[0][:, :], rkvT[qi, p * 128:(p + 1) * 128, :])], tS[0], writes=[tS[0]])
            token_shift(k, S[0], tS[0], S[dst], tS[dst], dp[:, p, qi:qi + 1], dp[:, p, 3 + qi:4 + qi], t_dp)
        k.op("dve", lambda v: v.tensor_scalar(S[6][:, :], S[4][:, :], pr[:, 3:4], None, ALU.mult), reads=[tS[4], t_prm], writes=[tS[6]])
        k.op("act", lambda a: a.activation(S[7][:, :], S[6][:, :], AF.Square), reads=[tS[6]], writes=[tS[7]])

        def kk_fn(t0, nn, ps, t_ps):
            k.op("act", lambda a: a.activation(S[8][:, t0:t0 + nn], ps[:, :nn], AF.Sqrt), reads=[t_ps], writes=[tS[8]])
        bo_apply(S[7], tS[7], kk_fn)
        k.op("dve", lambda v: v.tensor_scalar(S[8][:, :], S[8][:, :], 1e-12, None, ALU.max), writes=[tS[8]])
        k.op("dve", lambda v: v.reciprocal(S[8][:, :], S[8][:, :]), writes=[tS[8]])
        k.op("dve", lambda v: v.tensor_tensor(S[6][:, :], S[6][:, :], S[8][:, :], ALU.mult), reads=[tS[8]], writes=[tS[6]])
        k.op("dve", lambda v: v.tensor_scalar(S[7][:, :], S[6][:, :], -1.0, None, ALU.mult), reads=[tS[6]], writes=[tS[7]])
        for d in range(2):
            store_scan("A", d * 4 + p, S[7], tS[7])
            store_scan("R", d * 4 + p, S[3], tS[3])
            store_scan("V", d * 4 + p, S[5], tS[5])
        for d in range(2):
            for (t0, nn) in TBLK:
                s_ = nlb % 2
                nlb += 1
                k.dma("sp", [(lb[s_][:, :, :nn], LOA[:, :, t0:t0 + nn].rearrange("c p t -> p c t"))], t_lb[s_], reads=[t_loa], writes=[t_lb[s_]])
                if d == 0:
                    pb = npsA % 2; npsA += 1
                    for c in range(4):
                        rows = 96 if c == 3 else 128
                        k.op("pe", lambda pe, c=c, rows=rows: pe.matmul(PS[pb][:, :nn], g2[:rows, c, p * 128:(p + 1) * 128], lb[s_][:rows, 2 + c, :nn],
                                                                        start=(c == 0), stop=(c == 3)),
                             reads=[t_lb[s_], t_wts] if c == 0 else (), writes=[tPS[pb]] if c == 0 else (), acc=[tPS[pb]] if c else (), inc=(c == 3))
                    k.op("act", lambda a: a.activation(S[1][:, t0:t0 + nn], PS[pb][:, :nn], AF.Copy), reads=[tPS[pb]], writes=[tS[1]])
                pb = npsA % 2; npsA += 1
                k.op("pe", lambda pe: pe.matmul(PS[pb][:, :nn], w2[:, d, p * 128:(p + 1) * 128], lb[s_][:, 0, :nn], start=True, stop=True),
                     reads=[t_lb[s_], t_wts], writes=[tPS[pb]])
                k.op("act", lambda a: a.activation(S[7][:, t0:t0 + nn], PS[pb][:, :nn], AF.Sigmoid, bias=pr[:, 8 + d:9 + d]),
                     reads=[tPS[pb], t_prm], writes=[tS[7]])
                pb = npsA % 2; npsA += 1
                k.op("pe", lambda pe: pe.matmul(PS[pb][:, :nn], a2[:, d, p * 128:(p + 1) * 128], lb[s_][:, 1, :nn], start=True, stop=True),
                     reads=[t_lb[s_], t_wts], writes=[tPS[pb]])
                k.op("act", lambda a: a.activation(S[8][:, t0:t0 + nn], PS[pb][:, :nn], AF.Sigmoid, bias=pr[:, 10 + d:11 + d]),
                     reads=[tPS[pb], t_prm], writes=[tS[8]])
            if d == 0:
                k.dma("sp", [(GS[p, :, :], S[1][:, :])], tS[1], reads=[tS[1]], acc=[t_gs])
            k.op("act", lambda a: a.activation(S[7][:, :], S[7][:, :], AF.Exp, scale=-0.6065306597126334), writes=[tS[7]])
            store_scan("W", d * 4 + p, S[7], tS[7])
            k.op("dve", lambda v: v.tensor_tensor(S[7][:, :], S[6][:, :], S[8][:, :], ALU.mult), reads=[tS[6], tS[8]], writes=[tS[7]])
            store_scan("B", d * 4 + p, S[7], tS[7])
            k.op("dve", lambda v: v.tensor_scalar(S[8][:, :], S[8][:, :], pr[:, 4:5], dp[:, p, 6:7], ALU.mult, ALU.add), reads=[t_prm, t_dp], writes=[tS[8]])
            k.op("dve", lambda v: v.tensor_tensor(S[8][:, :], S[8][:, :], S[4][:, :], ALU.mult), reads=[tS[4]], writes=[tS[8]])
            store_scan("K", d * 4 + p, S[8], tS[8])
            if d == 0:
                k.op("dve", lambda v: v.tensor_copy(S[0][:, :], S[8][:, :]), reads=[tS[8]], writes=[tS[0]])
            else:
                k.op("dve", lambda v: v.tensor_tensor(S[0][:, :], S[0][:, :], S[8][:, :], ALU.add), reads=[tS[8]], writes=[tS[0]])
        k.op("dve", lambda v: v.tensor_tensor(S[0][:, :], S[0][:, :], S[3][:, :], ALU.mult), reads=[tS[3]], writes=[tS[0]])
        k.op("dve", lambda v: v.tensor_scalar(S[0][:, :], S[0][:, :], pr[:, 5:6], None, ALU.mult), reads=[t_prm], writes=[tS[0]])

        def bon_fn(t0, nn, ps, t_ps):
            k.op("dve", lambda v: v.tensor_tensor(S[7][:, t0:t0 + nn], ps[:, :nn], S[5][:, t0:t0 + nn], ALU.mult), reads=[t_ps, tS[5]], writes=[tS[7]])
        bo_apply(S[0], tS[0], bon_fn)
        k.dma("sp", [(BON[p, :, :], S[7][:, :])], tS[7], reads=[tS[7]], acc=[t_bon])

    def carve(i, off, a, b, rows=128):
        return S[i][:rows, off:off + a * b].rearrange("p (a b) -> p a b", a=a)

    def inherit(i):
        t = k.tok()
        t.w = dict(tS[i].w); t.r = dict(tS[i].r)
        return t
    carved = []

    def ctok(i):
        t = inherit(i)
        carved.append((i, t))
        return t
    chunk = {}
    t_chunk = {}
    for qi, q in enumerate("RWKVAB"):
        base = qi // 2
        o = (qi % 2) * 2048
        chunk[q] = [carve(base, o, G8, CH), carve(base, o + 1024, G8, CH)]
        t_chunk[q] = [ctok(base), ctok(base)]
    NR = 4

    def carve_bf(i, off_f32, a, b_):
        return S[i][:, off_f32:off_f32 + (a * b_) // 2].bitcast(BF16).rearrange("p (a b) -> p a b", a=a)
    Vsi = [carve_bf(4, r * 256, G8, 64) for r in range(NR)]; t_Vsi = [ctok(4) for _ in range(NR)]
    Rm = [carve_bf(4, 1024 + r * 8, G8, 2) for r in range(NR)]; t_Rm = [ctok(4) for _ in range(NR)]
    Hbf = [carve_bf(4, 1280 + r * 256, G8, 64) for r in range(2)]; t_Hbf = [ctok(4), ctok(4)]
    BObf = S[4][:, 2048:2112].bitcast(BF16)
    t_BObf = ctok(4)
    k.op("dve", lambda v: v.tensor_copy(BObf, BO), reads=[t_cst], writes=[t_BObf])
    Hb = [carve(6, r * 512, G8, 64) for r in range(2)]; t_Hb = [ctok(6), ctok(6)]
    HW = carve(6, 1024, G8, 64); KV = carve(6, 1536, G8, 64); tmp1 = carve(6, 2048, G8, 64); AH = carve(6, 2560, G8, 64)
    t_HW = ctok(6); t_KV = ctok(6); t_t1 = ctok(6); t_AH = ctok(6)
    ysb = [carve(5, r * 2048, G8 * 2, CH, rows=64) for r in range(2)]; t_ysb = [ctok(5), ctok(5)]
    k.op("dve", lambda v: v.memset(Hb[0], 0.0), writes=[t_Hb[0]])
    sa_ps, t_sa = PS[2], tPS[2]
    vb_ps, t_vb = [PS[3], PS[4]], [tPS[3], tPS[4]]
    y_ps, t_yp = [PS[5], PS[6]], [tPS[5], tPS[6]]
    nchunks = (nsteps + CH - 1) // CH
    chunk_of = {}

    def emit_y(s):
        cbs, c = chunk_of[s]
        r_ = s % NR
        hb = (s + 1) % 2
        yb = (s // YB) % 2
        col = s % YB
        for g in range(G8):
            first = (col == 0 and g == 0)
            k.op("pe", lambda pe, g=g: pe.matmul(y_ps[yb][0:64, (col * G8 + g) * 2:(col * G8 + g) * 2 + 2], Hbf[hb][:, g, :], Rm[r_][:, g, :],
                                                 start=True, stop=True),
                 reads=[t_Hbf[hb], t_Rm[r_]] if g == 0 else (), writes=[t_yp[yb]] if first else (), acc=[] if first else [t_yp[yb]], inc=(g == G8 - 1))
        if col == YB - 1 or s == nsteps - 1:
            c0 = c - col
            nst = col + 1
            k.op("act", lambda a: a.activation(ysb[cbs][:, :, c0:c0 + nst].rearrange("p gh s -> p s gh"),
                                               y_ps[yb][0:64, 0:nst * 16].rearrange("p (s gh) -> p s gh", gh=16), AF.Copy),
                 reads=[t_yp[yb]], acc=[t_ysb[cbs]])
        if c == CH - 1 or s == nsteps - 1:
            s0_ = s - c
            nst_c = c + 1
            k.dma("sp", [(YSC[:, :, :, s0_:s0_ + nst_c].rearrange("g h i s -> i (g h) s"), ysb[cbs][:, :, :nst_c])], t_ysb[cbs],
                  reads=[t_ysb[cbs]], acc=[t_ysc])

    for ci in range(nchunks):
        s0 = ci * CH
        cb = ci % 2
        for q in "RWKVAB":
            k.dma("sp", [(chunk[q][cb][:, :, :], SC[q][:, :, s0:s0 + CH].rearrange("g p s -> p g s"))], t_chunk[q][cb],
                  reads=[tSC[q]], writes=[t_chunk[q][cb]])
        Rc, Wc, Kc, Vc, Ac, Bc = (chunk[q][cb] for q in "RWKVAB")
        tR, tW, tK, tV, tA, tB = (t_chunk[q][cb] for q in "RWKVAB")
        for c in range(CH):
            s = s0 + c
            if s >= nsteps:
                break
            chunk_of[s] = (cb, c)
            r_ = s % NR
            Hc, t_Hc = Hb[s % 2], t_Hb[s % 2]
            Hn, t_Hn = Hb[(s + 1) % 2], t_Hb[(s + 1) % 2]
            hbn = (s + 1) % 2
            k.op("pool", lambda g: g.tensor_tensor(Vsi[r_], SI.unsqueeze(1).to_broadcast([128, G8, 64]),
                                                   Vc[:, :, c:c + 1].to_broadcast([128, G8, 64]), ALU.mult),
                 reads=[tV, t_cst], writes=[t_Vsi[r_]])
            k.op("pool", lambda g: g.tensor_tensor(Rm[r_], HM.unsqueeze(1).to_broadcast([128, G8, 2]),
                                                   Rc[:, :, c:c + 1].to_broadcast([128, G8, 2]), ALU.mult),
                 reads=[tR, t_cst], writes=[t_Rm[r_]])
            k.op("pool", lambda g: g.tensor_tensor(HW, Hc, Wc[:, :, c:c + 1].to_broadcast([128, G8, 64]), ALU.mult), reads=[t_Hc, tW], writes=[t_HW])
            vb = s % 2
            k.op("pe", lambda pe: pe.matmul(vb_ps[vb][:, :], BObf, Vsi[r_].rearrange("p g i -> p (g i)"), start=True, stop=True),
                 reads=[t_Vsi[r_], t_BObf], writes=[t_vb[vb]])
            k.op("dve", lambda v: v.tensor_tensor(AH, Hc, Ac[:, :, c:c + 1].to_broadcast([128, G8, 64]), ALU.mult), reads=[t_Hc, tA], writes=[t_AH])
            k.op("pe", lambda pe: pe.matmul(sa_ps[:, :], BO, AH.rearrange("p g i -> p (g i)"), start=True, stop=True),
                 reads=[t_AH, t_cst], writes=[t_sa])
            if s > 0:
                emit_y(s - 1)
            k.op("dve", lambda v: v.tensor_tensor(KV, vb_ps[vb][:, :].rearrange("p (g i) -> p g i", g=G8),
                                                  Kc[:, :, c:c + 1].to_broadcast([128, G8, 64]), ALU.mult),
                 reads=[t_vb[vb], tK], writes=[t_KV])
            k.op("dve", lambda v: v.tensor_tensor(KV, KV, HW, ALU.add), reads=[t_HW], writes=[t_KV])
            k.op("dve", lambda v: v.tensor_tensor(tmp1, sa_ps[:, :].rearrange("p (g i) -> p g i", g=G8),
                                                  Bc[:, :, c:c + 1].to_broadcast([128, G8, 64]), ALU.mult),
                 reads=[t_sa, tB], writes=[t_t1])
            k.op("dve", lambda v: v.tensor_tensor(Hn, KV, tmp1, ALU.add), reads=[t_KV, t_t1], writes=[t_Hn])
            k.op("act", lambda a: a.activation(Hbf[hbn], Hn, AF.Copy), reads=[t_Hn], writes=[t_Hbf[hbn]])
    emit_y(nsteps - 1)

    for (i, t) in carved:
        K._merge(tS[i].r, t.w)
        K._merge(tS[i].r, t.r)
    for p in range(4):
        pr = prm[:, p, :]
        k.dma("sp", [(S[0][:, :], YSC[p].rearrange("h i s -> (h i) s"))], tS[0], reads=[t_ysc], writes=[tS[0]])
        k.dma("sp", [(S[1][:, :], YSC[4 + p].rearrange("h i s -> (h i) s"))], tS[1], reads=[t_ysc], writes=[tS[1]])
        for (lo, hi) in SEGS:
            k.op("dve", lambda v: v.tensor_tensor(S[0][:, lo:hi], S[0][:, lo:hi], rev(S[1], lo, hi), ALU.add), reads=[tS[1]], writes=[tS[0]])

        def mean_fn(t0, nn, ps, t_ps):
            k.op("dve", lambda v: v.scalar_tensor_tensor(S[2][:, t0:t0 + nn], ps[:, :nn], -1.0 / 64, S[0][:, t0:t0 + nn], ALU.mult, ALU.add),
                 reads=[t_ps, tS[0]], writes=[tS[2]])
        bo_apply(S[0], tS[0], mean_fn)
        k.op("act", lambda a: a.activation(S[3][:, :], S[2][:, :], AF.Square), reads=[tS[2]], writes=[tS[3]])

        def var_fn(t0, nn, ps, t_ps):
            k.op("act", lambda a: a.activation(S[4][:, t0:t0 + nn], ps[:, :nn], AF.Sqrt, bias=64e-5, scale=1.0 / 64), reads=[t_ps], writes=[tS[4]])
        bo_apply(S[3], tS[3], var_fn)
        k.op("dve", lambda v: v.reciprocal(S[4][:, :], S[4][:, :]), writes=[tS[4]])
        k.op("dve", lambda v: v.tensor_tensor(S[2][:, :], S[2][:, :], S[4][:, :], ALU.mult), reads=[tS[4]], writes=[tS[2]])
        k.op("dve", lambda v: v.tensor_scalar(S[2][:, :], S[2][:, :], pr[:, 6:7], pr[:, 7:8], ALU.mult, ALU.add), reads=[t_prm], writes=[tS[2]])
        k.dma("sp", [(S[5][:, :], BON[p])], tS[5], reads=[t_bon], writes=[tS[5]])
        k.dma("sp", [(S[6][:, :], GS[p])], tS[6], reads=[t_gs], writes=[tS[6]])
        k.op("dve", lambda v: v.tensor_tensor(S[2][:, :], S[2][:, :], S[5][:, :], ALU.add), reads=[tS[5]], writes=[tS[2]])
        k.op("dve", lambda v: v.tensor_tensor(S[2][:, :], S[2][:, :], S[6][:, :], ALU.mult), reads=[tS[6]], writes=[tS[2]])
        k.dma("sp", [(yT_out[512 + p * 128:512 + (p + 1) * 128, :], S[2][:, :])], tS[2], reads=[tS[2]], acc=[t_yout])


def build_even_mixer(nsteps=T, do_lru=True):
    k = K()
    xaT = k.inp("xaT", [512, T])
    gateT = k.inp("gateT", [512, T])
    rkvT = k.inp("rkvT", [3, 512, T])
    loT = k.inp("loT", [736, T])
    lprm_d = k.inp("lprm", [128, 4, 16])
    gw_d = k.inp("gw", [128, 4, 4, 128])
    rprm_d = k.inp("rprm", [128, 4, 16])
    mulo_d = k.inp("mulo", [128, 6])
    w2_d = k.inp("w2", [128, 2, 512])
    a2_d = k.inp("a2", [128, 2, 512])
    g2_d = k.inp("g2", [128, 4, 512])
    cst_d = k.inp("cst", [128, 194])
    yT = k.outp("yT", [1024, T])
    lprm = k.sb([128, 4, 16], F32); rprm = k.sb([128, 4, 16], F32); mulo = k.sb([128, 6], F32); t_prm = k.tok()
    k.dma("sp", [(lprm[:], lprm_d), (rprm[:], rprm_d), (mulo[:], mulo_d)], t_prm, writes=[t_prm])
    gw = k.sb([128, 4, 4, 128], F32); w2 = k.sb([128, 2, 512], F32); a2 = k.sb([128, 2, 512], F32); g2 = k.sb([128, 4, 512], F32)
    cst = k.sb([128, 194], F32); t_w = k.tok()
    k.dma("sp", [(gw[:], gw_d), (w2[:], w2_d), (a2[:], a2_d), (g2[:], g2_d), (cst[:], cst_d)], t_w, writes=[t_w])
    P = Pool9(k)
    PS = [k.ps([128, 512], F32) for _ in range(8)]; tPS = k.toks(8)
    t_y = k.tok()
    if do_lru:
        emit_lru(k, P, PS, tPS, lprm, t_prm, gw, t_w, xaT, gateT, yT, t_y)
    emit_rwkv(k, P, PS, tPS, cst, t_w, rkvT, loT, rprm, t_prm, mulo, t_prm, w2, a2, g2, t_w, yT, t_y, nsteps=nsteps)
    k.finish([t_y])
    return k


def even_consts():
    c = np.zeros((128, 194), np.float32)
    pi = np.arange(128)
    c[:, 0:128] = (pi[:, None] // 64 == pi[None, :] // 64)
    c[:, 128:192] = (pi[:, None] % 64 == np.arange(64)[None, :])
    c[:, 192:194] = (pi[:, None] // 64 == np.arange(2)[None, :])
    return c


def rwkv_core_params(q, mu, w0, w2, a0, a2, g2, k_k, k_a, r_k, ln_w, ln_b):
    prm = np.zeros((128, 4, 16), np.float32)
    rk = r_k.reshape(-1)
    for p in range(4):
        ch = slice(512 * q + 128 * p, 512 * q + 128 * (p + 1))
        for qi in range(3):
            prm[:, p, qi] = mu[2048 * qi:2048 * (qi + 1)][ch]
        prm[:, p, 3] = k_k[ch]; prm[:, p, 4] = k_a[ch]; prm[:, p, 5] = rk[ch]
        prm[:, p, 6] = ln_w[ch]; prm[:, p, 7] = ln_b[ch]
        for d in range(2):
            prm[:, p, 8 + d] = w0[d, ch]; prm[:, p, 10 + d] = a0[d, ch]
    mulo = np.zeros((128, 6), np.float32)
    ml = mu[6144:]
    for c in range(6):
        rows = 96 if c == 5 else 128
        mulo[:rows, c] = ml[c * 128:c * 128 + rows]
    cs = slice(512 * q, 512 * q + 512)
    w2c = np.ascontiguousarray(w2[:, :, cs].transpose(1, 0, 2))
    a2c = np.ascontiguousarray(a2[:, :, cs].transpose(1, 0, 2))
    g2p = np.zeros((512, 512), np.float32); g2p[:480] = g2[:, cs]
    g2c = np.ascontiguousarray(g2p.reshape(4, 128, 512).transpose(1, 0, 2))
    return prm, mulo, w2c, a2c, g2c


NCHUNK = 68
GQ = 4


def build_gla(nchunks=NCHUNK):
    k = K()
    qT = k.inp("qT", [4, 256, T])
    kT = k.inp("kT", [4, 256, T])
    ktok = k.inp("ktok", [4, T, 256])
    vtok = k.inp("vtok", [4, T, 512])
    gklo = k.inp("gklo", [4, 16, T])
    gkw_d = k.inp("gkw", [16, 4, 256])
    gkb_d = k.inp("gkb", [1, 4, 256])
    gate = k.inp("gate", [2, T, 512])
    nw_d = k.inp("nw", [128, 512])
    cst_d = k.inp("cst", [128, 65 + 64 + 128])
    y = k.outp("y", [2, T, 512])
    O = k.dram("osc", [4, T, 512], F32).ap(); t_O = k.tok()

    gkw = k.sb([16, 4, 256], F32); gkb = k.sb([1, 4, 256], F32); nw = k.sb([128, 512], F32); cst = k.sb([128, 257], F32)
    t_c = k.tok()
    k.dma("sp", [(gkw[:], gkw_d), (gkb[:], gkb_d), (nw[:], nw_d), (cst[:], cst_d)], t_c, writes=[t_c])
    LT = cst[0:64, 0:65]
    ONES = cst[0:64, 65:129]
    J = cst[:, 129:257]
    ones1 = cst[0:1, 65:129]

    PSB = [k.ps([128, 512], F32) for _ in range(7)]
    tP = k.toks(7)
    ser = k.toks(7)
    qg = [k.sb([128, 2, GQ * 64], F32) for _ in range(2)]
    kg = [k.sb([128, 2, GQ * 64], F32) for _ in range(2)]
    ktg = [k.sb([64, GQ, 256], F32) for _ in range(2)]
    vtg = [k.sb([64, GQ, 512], F32) for _ in range(2)]
    glg = [k.sb([16, GQ * 64], F32) for _ in range(2)]
    t_in = k.toks(2)
    la = k.sb([64, 256], F32); t_la = k.tok()
    cumt = k.sb([64, 256], F32); t_cumt = k.tok()
    e1 = k.sb([64, 256], F32); t_e1 = k.tok()
    kend = k.sb([64, 256], F32); t_kend = k.tok()
    eT = k.sb([128, 2, 64], F32); einvT = k.sb([128, 2, 64], F32); t_eT = k.tok(); t_einv = k.tok()
    qdec = k.sb([128, 2, 64], F32); kinv = k.sb([128, 2, 64], F32); t_qdec = k.tok(); t_kinv = k.tok()
    dec = k.sb([128, 2], F32); t_dec = k.tok()
    scT = k.sb([64, 64], F32); t_sc = k.tok()
    osb = [k.sb([64, 512], F32) for _ in range(2)]; t_osb = k.toks(2)
    Sst = k.sb([128, 2, 512], F32); t_S = k.tok()
    ng = 0
    for dh in range(4):
        k.op("dve", lambda v: v.memset(Sst[:], 0.0), writes=[t_S])
        for gi in range((nchunks + GQ - 1) // GQ):
            b = ng % 2
            ng += 1
            g0 = gi * GQ * 64
            gl = min(GQ, nchunks - gi * GQ) * 64
            k.dma("sp", [(qg[b][:, :, :gl], qT[dh, :, g0:g0 + gl].rearrange("(c p) t -> p c t", p=128)),
                         (kg[b][:, :, :gl], kT[dh, :, g0:g0 + gl].rearrange("(c p) t -> p c t", p=128)),
                         (ktg[b][:, :gl // 64, :], ktok[dh, g0:g0 + gl, :].rearrange("(c j) k -> j c k", j=64)),
                         (vtg[b][:, :gl // 64, :], vtok[dh, g0:g0 + gl, :].rearrange("(c j) k -> j c k", j=64)),
                         (glg[b][:, :gl], gklo[dh, :, g0:g0 + gl])], t_in[b], writes=[t_in[b]])
            for ci in range(gl // 64):
                n = gi * GQ + ci
                c0 = n * 64
                lo = ci * 64
                k.op("pe", lambda p: p.matmul(PSB[0][0:64, 0:256], glg[b][:, lo:lo + 64], gkw[:, dh, :], start=True, stop=False),
                     reads=[t_in[b], t_c], writes=[tP[0]], inc=False)
                k.op("pe", lambda p: p.matmul(PSB[0][0:64, 0:256], ones1, gkb[:, dh, :], start=False, stop=True), reads=[t_c], acc=[tP[0]])
                k.op("act", lambda a: a.activation(la[:, :], PSB[0][0:64, 0:256], AF.Sigmoid), reads=[tP[0]], writes=[t_la, ser[0]])
                k.op("act", lambda a: a.activation(la[:, :], la[:, :], AF.Ln), writes=[t_la])
                k.op("dve", lambda v: v.tensor_scalar(la[:, :], la[:, :], 1.0 / 16, -1.0, ALU.mult, ALU.max), writes=[t_la])
                k.op("pe", lambda p: p.matmul(PSB[1][0:64, 0:256], LT[:, 0:64], la[:, :], start=True, stop=True),
                     reads=[t_la, t_c], writes=[tP[1]], inc=False)
                k.op("pe", lambda p: p.matmul(PSB[1][0:64, 256:512], ONES, la[:, :], start=True, stop=True), reads=[t_la], acc=[tP[1]])
                for c in range(2):
                    k.op("pe", lambda p, c=c: p.matmul(PSB[2][:, c * 128:c * 128 + 65], la[:, c * 128:(c + 1) * 128], LT, start=True, stop=True),
                         reads=[t_la, t_c], writes=[tP[2]] if c == 0 else (), acc=[tP[2]] if c else (), inc=(c == 1))
                k.op("act", lambda a: a.activation(cumt[:, :], PSB[1][0:64, 0:256], AF.Copy), reads=[tP[1]], writes=[t_cumt, ser[1]])
                k.op("dve", lambda v: v.tensor_tensor(e1[:, :], PSB[1][0:64, 256:512], cumt[:, :], ALU.subtract),
                     reads=[tP[1], t_cumt], writes=[t_e1, ser[1]])
                k.op("act", lambda a: a.activation(e1[:, :], e1[:, :], AF.Exp), writes=[t_e1])
                k.op("dve", lambda v: v.tensor_tensor(kend[:, :], ktg[b][:, ci, :], e1[:, :], ALU.mult), reads=[t_in[b], t_e1], writes=[t_kend])
                cview = PSB[2][:, 0:256].rearrange("p (c x) -> p c x", c=2)
                k.op("act", lambda a: a.activation(eT[:, :, :], cview[:, :, 0:64], AF.Exp), reads=[tP[2]], writes=[t_eT, ser[2]])
                k.op("act", lambda a: a.activation(einvT[:, :, :], cview[:, :, 0:64], AF.Exp, scale=-1.0), reads=[tP[2]], writes=[t_einv, ser[2]])
                k.op("act", lambda a: a.activation(dec[:, :], cview[:, :, 64], AF.Exp), reads=[tP[2]], writes=[t_dec, ser[2]])
                k.op("dve", lambda v: v.scalar_tensor_tensor(qdec[:, :, :], qg[b][:, :, lo:lo + 64], 0.0625, eT[:, :, :], ALU.mult, ALU.mult),
                     reads=[t_in[b], t_eT], writes=[t_qdec])
                k.op("dve", lambda v: v.tensor_tensor(kinv[:, :, :], kg[b][:, :, lo:lo + 64], einvT[:, :, :], ALU.mult),
                     reads=[t_in[b], t_einv], writes=[t_kinv])
                for c in range(2):
                    k.op("pe", lambda p, c=c: p.matmul(PSB[3][0:64, 0:64], kinv[:, c, :], qdec[:, c, :], start=(c == 0), stop=(c == 1)),
                         reads=[t_kinv, t_qdec] if c == 0 else (), writes=[tP[3]] if c == 0 else (), acc=[tP[3]] if c else (), inc=(c == 1))
                k.op("dve", lambda v: v.tensor_tensor(scT[:, :], PSB[3][0:64, 0:64], LT[:, 0:64], ALU.mult), reads=[tP[3], t_c], writes=[t_sc, ser[3]])
                k.op("pe", lambda p: p.matmul(PSB[4][0:64, :], scT[:, :], vtg[b][:, ci, :], start=True, stop=False),
                     reads=[t_sc, t_in[b]], writes=[tP[4]], inc=False)
                for c in range(2):
                    k.op("pe", lambda p, c=c: p.matmul(PSB[4][0:64, :], qdec[:, c, :], Sst[:, c, :], start=False, stop=(c == 1)),
                         reads=[t_qdec, t_S] if c == 0 else (), acc=[tP[4]], inc=(c == 1))
                ob = n % 2
                k.op("act", lambda a: a.activation(osb[ob][:, :], PSB[4][0:64, :], AF.Copy), reads=[tP[4]], writes=[t_osb[ob], ser[4]])
                k.dma("sp", [(O[dh, c0:c0 + 64, :], osb[ob][:, :])], t_osb[ob], reads=[t_osb[ob]], acc=[t_O])
                for c in range(2):
                    k.op("pe", lambda p, c=c: p.matmul(PSB[5 + c][:, :], kend[:, c * 128:(c + 1) * 128], vtg[b][:, ci, :], start=True, stop=True),
                         reads=[t_kend, t_in[b]], writes=[tP[5 + c]])
                    k.op("dve", lambda v, c=c: v.scalar_tensor_tensor(Sst[:, c, :], Sst[:, c, :], dec[:, c:c + 1], PSB[5 + c][:, :], ALU.mult, ALU.add),
                         reads=[tP[5 + c], t_dec], writes=[t_S, ser[5 + c]])
    o0 = [k.sb([128, 512], F32) for _ in range(2)]; o1 = [k.sb([128, 512], F32) for _ in range(2)]; gt = [k.sb([128, 512], F32) for _ in range(2)]
    t_o0 = k.toks(2); t_o1 = k.toks(2); t_gt = k.toks(2)
    junk = k.sb([128, 512], F32); t_junk = k.tok()
    st = k.sb([128, 4], F32); t_st = k.tok()
    t_y = k.tok()
    nn_ = 0
    ntile = (nchunks * 64) // 128
    for hl in range(2):
        for m in range(ntile):
            b = nn_ % 2
            nn_ += 1
            r0 = m * 128
            mir = (1 - m) if m < 2 else (2 + (31 - (m - 2)))
            k.dma("sp", [(o0[b][:, :], O[hl, r0:r0 + 128, :])], t_o0[b], reads=[t_O], writes=[t_o0[b]])
            k.dma("sp", [(o1[b][:, :], O[2 + hl, mir * 128:mir * 128 + 128, :])], t_o1[b], reads=[t_O], writes=[t_o1[b]])
            k.dma("sp", [(gt[b][:, :], gate[hl, r0:r0 + 128, :])], t_gt[b], writes=[t_gt[b]])
            k.op("pe", lambda p: p.matmul(PSB[0][:, :], J, o1[b][:, :], start=True, stop=True), reads=[t_o1[b], t_c], writes=[tP[0]])
            k.op("dve", lambda v: v.tensor_tensor(o0[b][:, :], o0[b][:, :], PSB[0][:, :], ALU.add), reads=[tP[0]], writes=[t_o0[b], ser[0]])
            k.op("act", lambda a: a.activation(junk[:, :], o0[b][:, :], AF.Square, accum_out=st[:, 0:1]), reads=[t_o0[b]], writes=[t_junk, t_st])
            k.op("act", lambda a: a.activation(st[:, 1:2], st[:, 0:1], AF.Sqrt, bias=1e-5, scale=1.0 / 512), writes=[t_st])
            k.op("dve", lambda v: v.reciprocal(st[:, 2:3], st[:, 1:2]), writes=[t_st])
            k.op("dve", lambda v: v.scalar_tensor_tensor(o0[b][:, :], o0[b][:, :], st[:, 2:3], nw[:, :], ALU.mult, ALU.mult),
                 reads=[t_st, t_c], writes=[t_o0[b]])
            k.op("act", lambda a: a.activation(gt[b][:, :], gt[b][:, :], AF.Silu), writes=[t_gt[b]])
            k.op("dve", lambda v: v.tensor_tensor(o0[b][:, :], o0[b][:, :], gt[b][:, :], ALU.mult), reads=[t_gt[b]], writes=[t_o0[b]])
            k.dma("sp", [(y[hl, r0:r0 + 128, :], o0[b][:, :])], t_o0[b], reads=[t_o0[b]], acc=[t_y])
    k.finish([t_y])
    return k


def gla_consts():
    c = np.zeros((128, 257), np.float32)
    j = np.arange(64)
    c[0:64, 0:64] = (j[:, None] <= j[None, :])
    c[0:64, 64] = 1.0
    c[0:64, 65:129] = 1.0
    c[:, 129:257] = np.eye(128, dtype=np.float32)[::-1]
    return c


def seg_rev_np(z, axis=0):
    idx = np.concatenate([np.arange(NCTX - 1, -1, -1), np.arange(TSEQ - 1, NCTX - 1, -1)])
    return np.take(z, idx, axis=axis)


def build_lru_only():
    k = K()
    xaT = k.inp("xaT", [512, T])
    gateT = k.inp("gateT", [512, T])
    prm_d = k.inp("prm", [128, 4, 16])
    gw_d = k.inp("gw", [128, 4, 4, 128])
    yT = k.outp("yT", [512, T])
    prm = k.sb([128, 4, 16], F32); t_prm = k.tok()
    gw = k.sb([128, 4, 4, 128], F32); t_gw = k.tok()
    k.dma("sp", [(prm[:], prm_d)], t_prm, writes=[t_prm])
    k.dma("sp", [(gw[:], gw_d)], t_gw, writes=[t_gw])
    P = Pool9(k)
    PS = [k.ps([128, 512], F32) for _ in range(8)]; tPS = k.toks(8)
    t_y = k.tok()
    emit_lru(k, P, PS, tPS, prm, t_prm, gw, t_gw, xaT, gateT, yT, t_y)
    k.finish([t_y])
    return k


def lru_core_params(q, conv_w, conv_b, ga_w, ga_b, gx_w, gx_b, lam):
    prm = np.zeros((128, 4, 16), np.float32)
    gw = np.zeros((128, 4, 4, 128), np.float32)
    for n in range(4):
        ch = slice((4 * q + n) * 128, (4 * q + n + 1) * 128)
        prm[:, n, 0:4] = conv_w[:, ch].T
        prm[:, n, 4] = conv_b[ch]
        for d in range(2):
            prm[:, n, 5 + 2 * d] = ga_b[d, ch]
            prm[:, n, 6 + 2 * d] = gx_b[d, ch]
            prm[:, n, 9 + d] = lam[d, ch]
            gw[:, n, 2 * d, :] = ga_w[d, 4 * q + n]
            gw[:, n, 2 * d + 1, :] = gx_w[d, 4 * q + n]
    return prm, gw


def fm_vec(v):
    return np.ascontiguousarray(np.asarray(v, np.float32).reshape(KC, 128).T)


def token_shards(x, ctx):
    out = []
    for b in range(2):
        seq = np.concatenate([ctx[b], x[b]], axis=0)
        for q in range(4):
            out.append(np.ascontiguousarray(seq[q * NTOK:(q + 1) * NTOK]))
    return out


def run_mods(c, c_ctx, ada_w, ada_b):
    cond = np.stack([c[0], c[1], c_ctx], 0).astype(np.float32)
    condT = np.ascontiguousarray(cond.T.reshape(KC, 128, 3).transpose(1, 0, 2))
    ins = []
    for core in range(8):
        sl = slice(core * 3072, (core + 1) * 3072)
        ins.append({"condT": condT, "aw": np.ascontiguousarray(ada_w[:, :, sl]),
                    "ab": np.ascontiguousarray(np.broadcast_to(ada_b[:, None, sl], (2, 3, 3072)))})
    res = run_bass_kernel_spmd(build_mods().nc, ins, core_ids=list(range(8)))
    return np.concatenate([res.results[core]["mod"] for core in range(8)], axis=2)


def inproj_vecs(mod_l, gain, b, q):
    lat = mod_l[b]
    ctxm = mod_l[2]
    first = ctxm if q == 0 else lat
    vec = np.zeros((128, 8, KC), np.float32)
    vec[:, 0] = fm_vec(gain)
    vec[:, 1] = fm_vec(first[D:2 * D]); vec[:, 2] = fm_vec(first[0:D])
    vec[:, 3] = fm_vec(lat[D:2 * D]); vec[:, 4] = fm_vec(lat[0:D])
    return vec


GU_ORDER = np.concatenate([np.concatenate([np.arange(cc * 128, cc * 128 + 128), np.arange(512 + cc * 128, 512 + cc * 128 + 128)])
                           for cc in range(4)])


def moe_core_inputs(core, router_w_l, router_b_l, w_gu_l, b_gu_l, w_down_l, b_down_l):
    own = list(range(NEXP * core, NEXP * core + NEXP))
    perm = own + [e for e in range(32) if e not in own]
    rw = np.ascontiguousarray(router_w_l[:, perm])
    rb = np.ascontiguousarray(np.broadcast_to(router_b_l[perm][None, :], (128, 32)))
    wgu = np.ascontiguousarray(w_gu_l[own][:, :, GU_ORDER])
    bg = b_gu_l[own][:, GU_ORDER].reshape(NEXP, 8, 128)
    bgu = np.ascontiguousarray(bg.transpose(2, 0, 1))
    return {"rw": rw, "rb": rb, "wgu": wgu, "bgu": bgu, "wd": np.ascontiguousarray(w_down_l[own]),
            "bd": np.ascontiguousarray(b_down_l[own])}


def moe_vecs(mod_l, gain, b):
    vec = np.zeros((128, 8, KC), np.float32)
    vec[:, 0] = fm_vec(gain)
    for ci, row in enumerate([2, b]):
        vec[:, 1 + 2 * ci] = fm_vec(mod_l[row][4 * D:5 * D])
        vec[:, 2 + 2 * ci] = fm_vec(mod_l[row][3 * D:4 * D])
    return vec


def build_combine_inproj(ncols, nparts=4):
    k = K()
    parts = k.inp("parts", [nparts, NTOK, D])
    x1 = k.inp("x1", [NTOK, D])
    g_d = k.inp("g5", [128, 2, D])
    vec_d = k.inp("vec", [128, 8, KC])
    W = k.inp("w", [D, ncols])
    x2 = k.outp("x2", [NTOK, D])
    outT = k.outp("projT", [ncols, NTOK])
    g5 = k.sb([128, 2, D], F32); t_g = k.tok()
    k.dma("sp", [(g5[:, c, :], g_d[:, c, :]) for c in range(2)], t_g, writes=[t_g])
    t_x2 = k.tok()
    vec = k.sb([128, 8, KC], F32); t_vec = k.tok()
    AB = k.sb([128, 4, KC], F32); t_AB = k.tok()
    k.dma("sp", [(vec[:], vec_d)], t_vec, writes=[t_vec])
    make_AB(k, vec, t_vec, AB, t_AB, 0, 1, 2, 0)
    make_AB(k, vec, t_vec, AB, t_AB, 0, 3, 4, 2)
    fb = FrontBufs(k)
    emit_combine(k, parts, x1, g5, t_g, x2, t_x2, nparts=nparts, bufs=(fb.xt, fb.t_xt))
    hT = k.sb([128, KC, NTOK], BF16)
    t_hT = k.toks(NT)
    front_end(k, fb, x2, lambda i: AB[:, 0 if i < 2 else 2, :], lambda i: AB[:, 1 if i < 2 else 3, :], t_AB, hT, t_hT, t_src=t_x2)
    gb = GemmBufs(k, nwb=2)
    t_out = k.tok()
    gemm_f(k, gb, W, ncols, hT, t_hT, outT, t_out)
    k.finish([t_out, t_x2])
    return k


def build_combine_final(nparts=4):
    k = K()
    parts = k.inp("parts", [nparts, NTOK, D])
    x1 = k.inp("x1", [NTOK, D])
    g_d = k.inp("g5", [128, 2, D])
    gn_d = k.inp("g", [128, D])
    out = k.outp("o", [NTOK, D])
    x4 = k.dram("x4", [NTOK, D], F32).ap(); t_x4 = k.tok()
    g5 = k.sb([128, 2, D], F32); t_g = k.tok()
    k.dma("sp", [(g5[:, c, :], g_d[:, c, :]) for c in range(2)], t_g, writes=[t_g])
    gn = k.sb([128, D], F32); t_gn = k.tok()
    k.dma("sp", [(gn[:], gn_d)], t_gn, writes=[t_gn])
    xt = [k.sb([128, D], F32) for _ in range(2)]; t_x = k.toks(2)
    emit_combine(k, parts, x1, g5, t_g, x4, t_x4, nparts=nparts, bufs=(xt, t_x))
    emit_final_norm(k, x4, t_x4, gn, t_gn, out, xt, t_x)
    return k


def emit_final_norm(k, x, t_src, g, t_g, out, xt, t_x):
    junk = k.sb([128, D], BF16); t_j = k.tok()
    st = k.sb([128, NT, 3], F32); t_s = k.toks(NT)
    t_out = k.tok()
    for i in range(NT):
        rows = tile_rows(i)
        s = i % 2
        r0 = i * 128
        k.dma("sp", [(xt[s][:rows, 0:2048], x[r0:r0 + rows, 0:2048]), (xt[s][:rows, 2048:4096], x[r0:r0 + rows, 2048:4096])],
              t_x[s], reads=[t_src] if t_src is not None else (), writes=[t_x[s]])
        k.op("act", lambda a: a.activation(junk[:rows, :], xt[s][:rows, :], AF.Square, accum_out=st[:rows, i, 0:1]),
             reads=[t_x[s]], writes=[t_j, t_s[i]])
        k.op("act", lambda a: a.activation(st[:rows, i, 1:2], st[:rows, i, 0:1], AF.Sqrt, bias=EPS, scale=1.0 / D),
             reads=[t_s[i]], writes=[t_s[i]])
        k.op("dve", lambda v: v.reciprocal(st[:rows, i, 2:3], st[:rows, i, 1:2]), reads=[t_s[i]], writes=[t_s[i]])
        k.op("act", lambda a: a.activation(xt[s][:rows, :], xt[s][:rows, :], AF.Copy, scale=st[:rows, i, 2:3]),
             reads=[t_s[i]], writes=[t_x[s]])
        k.op("dve", lambda v: v.tensor_tensor(xt[s][:rows, :], xt[s][:rows, :], g[:rows, :], ALU.mult),
             reads=[t_g], writes=[t_x[s]])
        k.dma("sp", [(out[r0:r0 + rows, :], xt[s][:rows, :])], t_x[s], reads=[t_x[s]], acc=[t_out])
    k.finish([t_out])


CORES = list(range(8))
_PROG = {}


def prog(name, builder, *a):
    return builder(*a).nc


def launch(nc, ins):
    return run_bass_kernel_spmd(nc, ins, core_ids=CORES).results


def rep128(v):
    return np.ascontiguousarray(np.broadcast_to(np.asarray(v, np.float32)[None], (128,) + tuple(np.shape(v))))


def gate_rows(mod_l, b, q, chunk):
    lat = mod_l[b][chunk * D:(chunk + 1) * D]
    first = mod_l[2][chunk * D:(chunk + 1) * D] if q == 0 else lat
    return rep128(np.stack([first, lat], 0))


def col_major_index():
    r = np.arange(64)
    perm = (r[None, :] * 64 + np.arange(64)[:, None]).reshape(-1)
    return np.concatenate([np.arange(NCTX), NCTX + perm])


def assemble_seq(per_core, axis):
    return [np.concatenate([per_core[b * 4 + q] for q in range(4)], axis=axis) for b in range(2)]


def run_moe_layer(xs, mod_l, gain, rw, rb, wgu, bgu, wdn, bdn):
    seq = assemble_seq(xs, 0)
    eg_in = [moe_core_inputs(eg, rw, rb, wgu, bgu, wdn, bdn) for eg in range(4)]
    ins = []
    for b in range(2):
        vec = moe_vecs(mod_l, gain, b)
        for eg in range(4):
            d = dict(eg_in[eg])
            d["x1"] = seq[b]
            d["vec"] = vec
            ins.append(d)
    res = launch(prog("moe", build_moe), ins)
    parts = []
    for b in range(2):
        for q in range(4):
            parts.append(np.ascontiguousarray(np.stack([res[b * 4 + eg]["ypart"][q * NTOK:(q + 1) * NTOK] for eg in range(4)], 0)))
    return parts


def kernel(x, c, ctx, c_ctx, ada_w, ada_b, norm_mix, norm_ffn, norm_final,
           ev_w_in, ev_w_out, lru_conv_w, lru_conv_b, lru_gate_a_w, lru_gate_a_b,
           lru_gate_x_w, lru_gate_x_b, lru_lambda,
           rwkv_mu, rwkv_w0, rwkv_w2, rwkv_a0, rwkv_a2, rwkv_g2, rwkv_k_k, rwkv_k_a,
           rwkv_r_k, rwkv_ln_w, rwkv_ln_b,
           od_w_in, od_w_out, gla_gk_w2, gla_gk_b, gla_norm_w,
           router_w, router_b, exp_w_gu, exp_b_gu, exp_w_down, exp_b_down):
    A = lambda z: np.asarray(z, np.float32)
    x, ctx = A(x), A(ctx)
    norm_mix, norm_ffn = A(norm_mix), A(norm_ffn)
    mod = run_mods(A(c), A(c_ctx), A(ada_w), A(ada_b))
    xs = token_shards(x, ctx)

    w_in = np.ascontiguousarray(A(ev_w_in)[0])
    res = launch(prog("inproj", build_inproj, EVEN_IN),
                 [{"x": xs[cc], "vec": inproj_vecs(mod[0], norm_mix[0], cc // 4, cc % 4), "w": w_in} for cc in CORES])
    PT = assemble_seq([r["projT"] for r in res], 1)
    del res
    cst = even_consts()
    ins = []
    for b in range(2):
        for q in range(4):
            cs = slice(512 * q, 512 * q + 512)
            lprm, gw = lru_core_params(q, A(lru_conv_w)[0], A(lru_conv_b)[0], A(lru_gate_a_w)[0], A(lru_gate_a_b)[0],
                                       A(lru_gate_x_w)[0], A(lru_gate_x_b)[0], A(lru_lambda)[0])
            rprm, mulo, w2c, a2c, g2c = rwkv_core_params(q, A(rwkv_mu)[0], A(rwkv_w0)[0], A(rwkv_w2)[0], A(rwkv_a0)[0], A(rwkv_a2)[0],
                                                         A(rwkv_g2)[0], A(rwkv_k_k)[0], A(rwkv_k_a)[0], A(rwkv_r_k)[0],
                                                         A(rwkv_ln_w)[0], A(rwkv_ln_b)[0])
            P_ = PT[b]
            rkvT = np.ascontiguousarray(np.stack([P_[4096 + 2048 * i:4096 + 2048 * (i + 1)][cs] for i in range(3)], 0))
            ins.append({"gateT": np.ascontiguousarray(P_[0:2048][cs]), "xaT": np.ascontiguousarray(P_[2048:4096][cs]), "rkvT": rkvT,
                        "loT": np.ascontiguousarray(P_[10240:10976]), "lprm": lprm, "gw": gw, "rprm": rprm, "mulo": mulo,
                        "w2": w2c, "a2": a2c, "g2": g2c, "cst": cst})
    del PT
    res = launch(prog("even", build_even_mixer), ins)
    del ins
    YT = []
    for b in range(2):
        yt = np.empty((D, TSEQ), np.float32)
        for q in range(4):
            r = res[b * 4 + q]["yT"]
            yt[512 * q:512 * q + 512] = r[0:512]
            yt[2048 + 512 * q:2048 + 512 * q + 512] = r[512:1024]
        YT.append(yt)
    del res
    w_out = np.ascontiguousarray(A(ev_w_out)[0])
    res = launch(prog("outproj", build_outproj),
                 [{"yT": np.ascontiguousarray(YT[cc // 4][:, (cc % 4) * NTOK:(cc % 4 + 1) * NTOK]), "x": xs[cc],
                   "g2": gate_rows(mod[0], cc // 4, cc % 4, 2), "w": w_out} for cc in CORES])
    x1 = [r["x1"] for r in res]
    del res, YT
    parts = run_moe_layer(x1, mod[0], norm_ffn[0], A(router_w)[0], A(router_b)[0], A(exp_w_gu)[0], A(exp_b_gu)[0],
                          A(exp_w_down)[0], A(exp_b_down)[0])

    w_in1 = np.ascontiguousarray(A(od_w_in)[0])
    res = launch(prog("cinproj", build_combine_inproj, ODD_IN),
                 [{"parts": parts[cc], "x1": x1[cc], "g5": gate_rows(mod[0], cc // 4, cc % 4, 5),
                   "vec": inproj_vecs(mod[1], norm_mix[1], cc // 4, cc % 4), "w": w_in1} for cc in CORES])
    del parts
    x2 = [r["x2"] for r in res]
    PT = assemble_seq([r["projT"] for r in res], 1)
    del res
    cm = col_major_index()
    gcst = gla_consts()
    nw = rep128(A(gla_norm_w)[0])
    gk_w2, gk_b = A(gla_gk_w2)[0], A(gla_gk_b)[0]
    ins = []
    for b in range(2):
        Pp = PT[b][:, cm]
        for hp in range(4):
            d = {k_: [] for k_ in ("qT", "kT", "ktok", "vtok", "gklo")}
            gkw = np.zeros((16, 4, 256), np.float32)
            gkb = np.zeros((1, 4, 256), np.float32)
            for dd in range(2):
                for hl in range(2):
                    h = 2 * hp + hl
                    qs = Pp[h * 256:(h + 1) * 256]
                    ks = Pp[2048 + h * 256:2048 + (h + 1) * 256]
                    vs = Pp[4096 + h * 512:4096 + (h + 1) * 512]
                    gl = Pp[12288 + dd * 16:12288 + (dd + 1) * 16]
                    if dd == 1:
                        qs, ks, vs, gl = (seg_rev_np(z, axis=1) for z in (qs, ks, vs, gl))
                    d["qT"].append(qs); d["kT"].append(ks); d["ktok"].append(ks.T); d["vtok"].append(vs.T); d["gklo"].append(gl)
                    gkw[:, dd * 2 + hl, :] = gk_w2[dd][:, h * 256:(h + 1) * 256]
                    gkb[0, dd * 2 + hl, :] = gk_b[dd][h * 256:(h + 1) * 256]
            o = {k_: np.ascontiguousarray(np.stack(v_, 0)) for k_, v_ in d.items()}
            o["gkw"] = gkw; o["gkb"] = gkb
            o["gate"] = np.ascontiguousarray(np.stack([Pp[8192 + (2 * hp + hl) * 512:8192 + (2 * hp + hl + 1) * 512].T for hl in range(2)], 0))
            o["nw"] = nw; o["cst"] = gcst
            ins.append(o)
    del PT
    res = launch(prog("gla", build_gla), ins)
    del ins
    YT = []
    for b in range(2):
        yp = np.concatenate([res[b * 4 + hp]["y"][hl] for hp in range(4) for hl in range(2)], axis=1)
        yn = np.empty_like(yp)
        yn[cm] = yp
        YT.append(np.ascontiguousarray(yn.T))
    del res
    w_out1 = np.ascontiguousarray(A(od_w_out)[0])
    res = launch(prog("outproj", build_outproj),
                 [{"yT": np.ascontiguousarray(YT[cc // 4][:, (cc % 4) * NTOK:(cc % 4 + 1) * NTOK]), "x": x2[cc],
                   "g2": gate_rows(mod[1], cc // 4, cc % 4, 2), "w": w_out1} for cc in CORES])
    x3 = [r["x1"] for r in res]
    del res, YT
    parts = run_moe_layer(x3, mod[1], norm_ffn[1], A(router_w)[1], A(router_b)[1], A(exp_w_gu)[1], A(exp_b_gu)[1],
                          A(exp_w_down)[1], A(exp_b_down)[1])
    gfin = rep128(A(norm_final))
    res = launch(prog("cfinal", build_combine_final),
                 [{"parts": parts[cc], "x1": x3[cc], "g5": gate_rows(mod[1], cc // 4, cc % 4, 5), "g": gfin} for cc in CORES])
    out = np.zeros((2, 4096, D), np.float32)
    for b in range(2):
        seq = np.concatenate([res[b * 4 + q]["o"] for q in range(4)], axis=0)
        out[b] = seq[NCTX:]
    return out
```
